# Optimizing a Trainium2 kernel written in Bass

```python
import math
import jax, jax.numpy as jnp
from jax import lax
import numpy as np

D_MODEL = 1024
BATCH = 16
SEQ = 2048
DEPTH = 4

GRID_W = 64
CTX_LEN = 256
HEAD_DIM = 64
A_Q_HEADS = 16
A_KV_HEADS = 4
A_GROUP = A_Q_HEADS // A_KV_HEADS
WINDOW = 128
BLOCK = 128
B_HEADS = D_MODEL // (2 * HEAD_DIM)
N_EXPERTS = 16
N_GROUPS = 4
GROUP_SIZE = N_EXPERTS // N_GROUPS
TOP_K = 2
GROUP_SCORE_K = 2
D_EXPERT = D_MODEL // 2
ROPE_THETA = 10000.0
N_A_LAYERS = (DEPTH + 1) // 2
N_B_LAYERS = DEPTH // 2
DEEPNORM_ALPHA = (2 * DEPTH) ** 0.25
DEEPNORM_BETA = (8 * DEPTH) ** -0.25
LN_EPS = 1e-6
SUBLN_EPS = 1e-5
NEG_INF = -1e30
ATTN_SCALE = HEAD_DIM ** -0.5

kernel_name = "hybrid_swa_diffattn_sharedrouter_moe_dit"

F32 = jnp.float32


def layer_norm(x, g, b):
    xf = x.astype(F32)
    mu = xf.mean(-1, keepdims=True)
    var = jnp.square(xf - mu).mean(-1, keepdims=True)
    return ((xf - mu) * lax.rsqrt(var + LN_EPS) * g.astype(F32) + b.astype(F32)).astype(x.dtype)


def rms_norm(x, g):
    xf = x.astype(F32)
    return (xf * lax.rsqrt(jnp.square(xf).mean(-1, keepdims=True) + SUBLN_EPS) * g.astype(F32)).astype(x.dtype)


def axial_rope_tables(n_tokens):
    n_rows = n_tokens // GRID_W
    rows = jnp.broadcast_to(jnp.arange(n_rows)[:, None], (n_rows, GRID_W)).reshape(-1)
    cols = jnp.broadcast_to(jnp.arange(GRID_W)[None, :], (n_rows, GRID_W)).reshape(-1)
    quarter = HEAD_DIM // 4
    inv_freq = ROPE_THETA ** (-jnp.arange(quarter, dtype=F32) / quarter)
    ang_r = rows.astype(F32)[:, None] * inv_freq
    ang_c = cols.astype(F32)[:, None] * inv_freq
    return (jnp.cos(ang_r), jnp.sin(ang_r), jnp.cos(ang_c), jnp.sin(ang_c))


def _rotate(x, cos, sin):
    x1, x2 = jnp.split(x, 2, axis=-1)
    return jnp.concatenate([x1 * cos - x2 * sin, x2 * cos + x1 * sin], axis=-1)


def apply_axial_rope(x, tables):
    cos_r, sin_r, cos_c, sin_c = tables
    shape = (x.shape[1],) + (1,) * (x.ndim - 3) + (cos_r.shape[-1],)
    xr, xc = jnp.split(x.astype(F32), 2, axis=-1)
    out = jnp.concatenate([_rotate(xr, cos_r.reshape(shape), sin_r.reshape(shape)),
                           _rotate(xc, cos_c.reshape(shape), sin_c.reshape(shape))], axis=-1)
    return out.astype(x.dtype)


def sink_attend(q, parts, sink):
    scores = []
    for k, v, mask in parts:
        s = jnp.einsum('bqhgd,bkhd->bhgqk', q, k).astype(F32) * ATTN_SCALE
        if mask is not None:
            s = jnp.where(mask, s, NEG_INF)
        scores.append(s)
    snk = sink.astype(F32)[None, :, :, None, None]
    m = snk
    for s in scores:
        m = jnp.maximum(m, s.max(-1, keepdims=True))
    probs = [jnp.exp(s - m) for s in scores]
    denom = jnp.exp(snk - m)
    for p in probs:
        denom = denom + p.sum(-1, keepdims=True)
    out = None
    for p, (k, v, mask) in zip(probs, parts):
        o = jnp.einsum('bhgqk,bkhd->bqhgd', (p / denom).astype(v.dtype), v)
        out = o if out is None else out + o
    return out


def window_attention_latent(q, k, v, kc, vc, sink):
    S = q.shape[1]
    nb = S // BLOCK
    span = BLOCK + 2 * WINDOW
    kp = jnp.pad(k, ((0, 0), (WINDOW, WINDOW), (0, 0), (0, 0)))
    vp = jnp.pad(v, ((0, 0), (WINDOW, WINDOW), (0, 0), (0, 0)))
    r = jnp.arange(span)
    qi = jnp.arange(BLOCK)
    band = jnp.abs((r - WINDOW)[None, :] - qi[:, None]) <= WINDOW

    def block(n):
        start = n * BLOCK
        qb = lax.dynamic_slice_in_dim(q, start, BLOCK, axis=1)
        kb = lax.dynamic_slice_in_dim(kp, start, span, axis=1)
        vb = lax.dynamic_slice_in_dim(vp, start, span, axis=1)
        key_pos = start - WINDOW + r
        mask = band & ((key_pos >= 0) & (key_pos < S))[None, :]
        return sink_attend(qb, [(kb, vb, mask), (kc, vc, None)], sink)

    out = lax.map(block, jnp.arange(nb))
    return jnp.moveaxis(out, 0, 1).reshape(q.shape)


def window_gqa_mixer(hl, hc, w_qkv, w_o, sink, tables, with_ctx_out):
    B, S, _ = hl.shape
    C = hc.shape[1]
    q_dim = A_Q_HEADS * HEAD_DIM
    kv_dim = A_KV_HEADS * HEAD_DIM
    ql, kl, vl = jnp.split(hl @ w_qkv, [q_dim, q_dim + kv_dim], axis=-1)
    ql = apply_axial_rope(ql.reshape(B, S, A_KV_HEADS, A_GROUP, HEAD_DIM), tables)
    kl = apply_axial_rope(kl.reshape(B, S, A_KV_HEADS, HEAD_DIM), tables)
    vl = vl.reshape(B, S, A_KV_HEADS, HEAD_DIM)
    kc, vc = jnp.split(hc @ w_qkv[:, q_dim:], 2, axis=-1)
    kc = kc.reshape(B, C, A_KV_HEADS, HEAD_DIM)
    vc = vc.reshape(B, C, A_KV_HEADS, HEAD_DIM)
    sink_hg = sink.reshape(A_KV_HEADS, A_GROUP)
    out_l = window_attention_latent(ql, kl, vl, kc, vc, sink_hg).reshape(B, S, q_dim) @ w_o
    if not with_ctx_out:
        return out_l, None
    qc = (hc @ w_qkv[:, :q_dim]).reshape(B, C, A_KV_HEADS, A_GROUP, HEAD_DIM)
    out_c = sink_attend(qc, [(kc, vc, None)], sink_hg).reshape(B, C, q_dim) @ w_o
    return out_l, out_c


def diff_attend(q, k, v, lam):
    s = jnp.einsum('bqhmd,bkhmd->bhmqk', q, k).astype(F32) * ATTN_SCALE
    p = jax.nn.softmax(s, axis=-1)
    a = p[:, :, 0] - lam * p[:, :, 1]
    return jnp.einsum('bhqk,bkhe->bqhe', a.astype(v.dtype), v)


def diff_mixer(hl, hc, w_qkv, w_o, lam_params, subln_g, lam_init, tables, with_ctx_out):
    B, S, _ = hl.shape
    C = hc.shape[1]
    dq = B_HEADS * 2 * HEAD_DIM
    ql, kl, vl = jnp.split(hl @ w_qkv, [dq, 2 * dq], axis=-1)
    ql = apply_axial_rope(ql.reshape(B, S, B_HEADS, 2, HEAD_DIM), tables)
    kl = apply_axial_rope(kl.reshape(B, S, B_HEADS, 2, HEAD_DIM), tables)
    vl = vl.reshape(B, S, B_HEADS, 2 * HEAD_DIM)
    kc, vc = jnp.split(hc @ w_qkv[:, dq:], 2, axis=-1)
    kc = kc.reshape(B, C, B_HEADS, 2, HEAD_DIM)
    vc = vc.reshape(B, C, B_HEADS, 2 * HEAD_DIM)
    lp = lam_params.astype(F32)
    lam = jnp.exp(jnp.sum(lp[0] * lp[1])) - jnp.exp(jnp.sum(lp[2] * lp[3])) + lam_init
    k_all = jnp.concatenate([kl, kc], axis=1)
    v_all = jnp.concatenate([vl, vc], axis=1)

    def block(n):
        qb = lax.dynamic_slice_in_dim(ql, n * BLOCK, BLOCK, axis=1)
        return diff_attend(qb, k_all, v_all, lam)

    out_l = jnp.moveaxis(lax.map(block, jnp.arange(S // BLOCK)), 0, 1).reshape(B, S, B_HEADS, 2 * HEAD_DIM)

    def finish(o):
        n = o.shape[1]
        return (rms_norm(o, subln_g) * (1.0 - lam_init)).reshape(B, n, dq) @ w_o

    if not with_ctx_out:
        return finish(out_l), None
    qc = (hc @ w_qkv[:, :dq]).reshape(B, C, B_HEADS, 2, HEAD_DIM)
    out_c = diff_attend(qc, kc, vc, lam)
    return finish(out_l), finish(out_c)


def moe_ffn(h, w_router, router_bias, w_gate, w_up, w_down):
    N = h.shape[0]
    s = jax.nn.sigmoid(h.astype(F32) @ w_router.astype(F32))
    biased = s + router_bias.astype(F32)
    bg = biased.reshape(N, N_GROUPS, GROUP_SIZE)
    gscore = lax.top_k(bg, GROUP_SCORE_K)[0].sum(-1)
    gsel = jnp.argmax(gscore, axis=-1)
    in_group = jnp.take_along_axis(bg, gsel[:, None, None], axis=1)[:, 0]
    _, loc = lax.top_k(in_group, TOP_K)
    eid = gsel[:, None] * GROUP_SIZE + loc
    s_sel = jnp.take_along_axis(s, eid, axis=-1)
    w = s_sel / s_sel.sum(-1, keepdims=True)
    gates = (jax.nn.one_hot(eid, N_EXPERTS, dtype=F32) * w[..., None]).sum(1)
    out = jnp.zeros_like(h)
    for e in range(N_EXPERTS):
        y = (jax.nn.silu(h @ w_gate[e]) * (h @ w_up[e])) @ w_down[e]
        out = out + gates[:, e:e + 1].astype(h.dtype) * y
    return out


def setup_inputs(seed: int = 0) -> dict:
    key = jax.random.key(seed)
    ks = jax.random.split(key, 22)

    def nrm(k, shape, scale):
        return jax.random.normal(k, shape, F32) * scale

    D = D_MODEL
    qa = A_Q_HEADS * HEAD_DIM
    kva = A_KV_HEADS * HEAD_DIM
    dq = B_HEADS * 2 * HEAD_DIM
    return {
        "x": nrm(ks[0], (BATCH, SEQ, D), 1.0),
        "c": nrm(ks[1], (BATCH, D), 1.0),
        "ctx": nrm(ks[2], (BATCH, CTX_LEN, D), 1.0),
        "c_ctx": nrm(ks[3], (D,), 1.0),
        "w_ada": nrm(ks[4], (DEPTH, D, 6 * D), 0.5 * D ** -0.5),
        "b_ada": nrm(ks[5], (DEPTH, 6 * D), 0.02),
        "wqkv_a": nrm(ks[6], (N_A_LAYERS, D, qa + 2 * kva), D ** -0.5),
        "wo_a": nrm(ks[7], (N_A_LAYERS, qa, D), qa ** -0.5 * DEEPNORM_BETA),
        "sink_a": nrm(ks[8], (N_A_LAYERS, A_Q_HEADS), 0.5),
        "wqkv_b": nrm(ks[9], (N_B_LAYERS, D, 3 * dq), D ** -0.5),
        "wo_b": nrm(ks[10], (N_B_LAYERS, dq, D), dq ** -0.5 * DEEPNORM_BETA),
        "lambda_b": nrm(ks[11], (N_B_LAYERS, 4, HEAD_DIM), 0.1),
        "subln_b": 1.0 + nrm(ks[12], (N_B_LAYERS, 2 * HEAD_DIM), 0.02),
        "ln_attn_g": 1.0 + nrm(ks[13], (DEPTH, D), 0.02),
        "ln_attn_b": nrm(ks[14], (DEPTH, D), 0.02),
        "ln_ffn_g": 1.0 + nrm(ks[15], (DEPTH, D), 0.02),
        "ln_ffn_b": nrm(ks[16], (DEPTH, D), 0.02),
        "w_router": nrm(ks[17], (D, N_EXPERTS), D ** -0.5),
        "router_bias": nrm(ks[18], (N_EXPERTS,), 0.01),
        "w_gate": nrm(ks[19], (DEPTH, N_EXPERTS, D, D_EXPERT), D ** -0.5),
        "w_up": nrm(ks[20], (DEPTH, N_EXPERTS, D, D_EXPERT), D ** -0.5),
        "w_down": nrm(ks[21], (DEPTH, N_EXPERTS, D_EXPERT, D), D_EXPERT ** -0.5 * DEEPNORM_BETA),
    }


def reference(x, c, ctx, c_ctx, w_ada, b_ada, wqkv_a, wo_a, sink_a, wqkv_b, wo_b, lambda_b, subln_b,
              ln_attn_g, ln_attn_b, ln_ffn_g, ln_ffn_b, w_router, router_bias, w_gate, w_up, w_down):
    B, S, D = x.shape
    C = ctx.shape[1]
    tables = axial_rope_tables(S)
    xl, xc = x, ctx
    silu_c = jax.nn.silu(c)
    silu_cc = jax.nn.silu(c_ctx)
    for i in range(DEPTH):
        last = i == DEPTH - 1
        mod_l = (silu_c @ w_ada[i] + b_ada[i])[:, None, :]
        mod_c = silu_cc @ w_ada[i] + b_ada[i]
        sh1_l, sc1_l, g1_l, sh2_l, sc2_l, g2_l = jnp.split(mod_l, 6, axis=-1)
        sh1_c, sc1_c, g1_c, sh2_c, sc2_c, g2_c = jnp.split(mod_c, 6, axis=-1)

        hl = xl * (1.0 + sc1_l) + sh1_l
        hc = xc * (1.0 + sc1_c) + sh1_c
        j = i // 2
        if i % 2 == 0:
            al, ac = window_gqa_mixer(hl, hc, wqkv_a[j], wo_a[j], sink_a[j], tables, not last)
        else:
            lam_init = 0.8 - 0.6 * math.exp(-0.3 * i)
            al, ac = diff_mixer(hl, hc, wqkv_b[j], wo_b[j], lambda_b[j], subln_b[j], lam_init, tables, not last)
        xl = layer_norm(DEEPNORM_ALPHA * xl + g1_l * al, ln_attn_g[i], ln_attn_b[i])

        hl2 = (xl * (1.0 + sc2_l) + sh2_l).reshape(B * S, D)
        if last:
            yl = moe_ffn(hl2, w_router, router_bias, w_gate[i], w_up[i], w_down[i]).reshape(B, S, D)
        else:
            xc = layer_norm(DEEPNORM_ALPHA * xc + g1_c * ac, ln_attn_g[i], ln_attn_b[i])
            hc2 = (xc * (1.0 + sc2_c) + sh2_c).reshape(B * C, D)
            y = moe_ffn(jnp.concatenate([hl2, hc2], axis=0), w_router, router_bias, w_gate[i], w_up[i], w_down[i])
            yl = y[:B * S].reshape(B, S, D)
            yc = y[B * S:].reshape(B, C, D)
            xc = layer_norm(DEEPNORM_ALPHA * xc + g2_c * yc, ln_ffn_g[i], ln_ffn_b[i])
        xl = layer_norm(DEEPNORM_ALPHA * xl + g2_l * yl, ln_ffn_g[i], ln_ffn_b[i])
    return xl
```

```python
import math
from contextlib import ExitStack, contextmanager

import numpy as np
import concourse.bass as bass
import concourse.mybir as mybir
from concourse.bass_utils import run_bass_kernel_spmd

F32 = mybir.dt.float32
BF16 = mybir.dt.bfloat16
AF = mybir.ActivationFunctionType
ALU = mybir.AluOpType
AX = mybir.AxisListType

DEPTH = 4
D = 1024
S = 2048
C = 256
TOK = S + C
NT = TOK // 128
NTL = S // 128
NB = 2
NCORES = 8
E = 16
DE = 512
ALPHA = (2 * DEPTH) ** 0.25
LN_EPS = 1e-6
SUBLN_EPS = 1e-5
SCALE = 0.125
NEG = -30000.0

SEM_ROLL = 30000
SAME_ENGINE_SYNC = ("act", "dve", "pool")
import os
DBG_CUT = int(os.environ.get("DBG_CUT", "9"))
SCOPES = bool(int(os.environ.get("DBG_SCOPES", "0")))
N_DMA_SLOTS = {"sp": 16, "pool": 12}


class Ctx:
    def __init__(self, nc, same_engine_sync=SAME_ENGINE_SYNC):
        self.nc = nc
        self.es = ExitStack()
        self.eng = {"pe": nc.tensor, "act": nc.scalar, "dve": nc.vector,
                    "pool": nc.gpsimd, "sp": nc.sync}
        self.same_engine_sync = set(same_engine_sync)
        self.sem = {}
        self.cnt = {}
        self.nsem = 0
        self.uid = 0
        for e in ("pe", "act", "dve", "pool"):
            self._new_sem(e)
        self.known = {e: {} for e in self.eng}
        self.last_w = {}
        self.readers = {}
        self.dma_sems = {}
        self.dma_val = {}
        self.dma_i = {}
        for q, n in N_DMA_SLOTS.items():
            self.dma_sems[q] = [self.es.enter_context(nc.semaphore(f"dq_{q}_{i}")) for i in range(n)]
            self.dma_val[q] = [0] * n
            self.dma_i[q] = 0
        self.out_events = []
        self.ps = [self.es.enter_context(nc.psum_tensor(f"psb{i}", [128, 512], F32)) for i in range(8)]

    def _new_sem(self, e):
        self.nsem += 1
        self.sem[e] = self.es.enter_context(self.nc.semaphore(f"s_{e}_{self.nsem}"))
        self.cnt[e] = 0

    def sbuf(self, name, shape, dt):
        self.uid += 1
        return self.es.enter_context(self.nc.sbuf_tensor(f"{name}_{self.uid}", list(shape), dt))

    @contextmanager
    def phase(self, name=None):
        st = ExitStack()
        ctx = self
        if name is not None and SCOPES:
            st.enter_context(self.nc.named_scope(name))

        class PH:
            def sbuf(self_, name, shape, dt):
                ctx.uid += 1
                return st.enter_context(ctx.nc.sbuf_tensor(f"{name}_{ctx.uid}", list(shape), dt))

        try:
            yield PH()
        finally:
            self.barrier()
            st.close()

    def _deps(self, reads, writes):
        evs = []
        for k in list(reads) + list(writes):
            ev = self.last_w.get(k)
            if ev is not None:
                evs.append(ev)
        for k in writes:
            evs.extend(self.readers.get(k, ()))
        return evs

    def _emit_waits(self, engine, evs):
        best = {}
        for (sem, val, src) in evs:
            if src == engine and engine not in self.same_engine_sync:
                continue
            key = id(sem)
            if self.known[engine].get(key, 0) >= val:
                continue
            if key not in best or best[key][1] < val:
                best[key] = (sem, val)
        for key, (sem, val) in best.items():
            self.eng[engine].wait_ge(sem, val)
            self.known[engine][key] = val

    def _record(self, ev, reads, writes):
        for k in writes:
            self.last_w[k] = ev
            self.readers[k] = []
        for k in reads:
            self.readers.setdefault(k, []).append(ev)

    @staticmethod
    def _excl(reads, writes):
        r = [k for k in reads if not (isinstance(k, str) and k.startswith("ps"))]
        w = list(writes) + [k for k in reads if isinstance(k, str) and k.startswith("ps")]
        return r, w

    def op(self, engine, reads, writes, emit):
        reads, writes = self._excl(reads, writes)
        self._emit_waits(engine, self._deps(reads, writes))
        ins = emit(self.eng[engine])
        if self.cnt[engine] >= SEM_ROLL:
            self._new_sem(engine)
        self.cnt[engine] += 1
        ins.then_inc(self.sem[engine], 1)
        ev = (self.sem[engine], self.cnt[engine], engine)
        self._record(ev, reads, writes)
        return ev

    def dma(self, queue, out_ap, in_ap, reads, writes, is_output=False):
        slot = self.dma_i[queue] % len(self.dma_sems[queue])
        self.dma_i[queue] += 1
        sem = self.dma_sems[queue][slot]
        evs = self._deps(reads, writes)
        if self.dma_val[queue][slot] > 0:
            evs.append((sem, self.dma_val[queue][slot], "dma"))
        self._emit_waits(queue, evs)
        outs = out_ap if isinstance(out_ap, (list, tuple)) else [out_ap]
        ins_ = in_ap if isinstance(in_ap, (list, tuple)) else [in_ap]
        for o, i in zip(outs, ins_):
            self.eng[queue].dma_start(out=o, in_=i).then_inc(sem, 16)
            self.dma_val[queue][slot] += 16
        ev = (sem, self.dma_val[queue][slot], "dma")
        self._record(ev, reads, writes)
        if is_output:
            self.out_events.append(ev)
        return ev

    def _all_events(self):
        evs = []
        for e in ("pe", "act", "dve", "pool"):
            if self.cnt[e] > 0:
                evs.append((self.sem[e], self.cnt[e], "bar"))
        for q in self.dma_sems:
            for s, v in zip(self.dma_sems[q], self.dma_val[q]):
                if v > 0:
                    evs.append((s, v, "dma"))
        return evs

    def barrier(self):
        evs = self._all_events()
        for e in ("pe", "act", "dve", "pool", "sp"):
            self._emit_waits(e, evs)
        self.last_w.clear()
        self.readers.clear()

    def finish(self):
        self._emit_waits("sp", list(self.out_events) + self._all_events())
        self.es.close()


class Rot:
    def __init__(self, alloc, name, n, shape, dt):
        self.tiles = [alloc.sbuf(f"{name}{i}", shape, dt) for i in range(n)]
        self.keys = [f"{name}#{id(self)}#{i}" for i in range(n)]
        self.i = -1

    def next(self):
        self.i = (self.i + 1) % len(self.tiles)
        return self.tiles[self.i], self.keys[self.i]


class PsRot:
    def __init__(self, c, banks):
        self.c = c
        self.banks = list(banks)
        self.i = -1

    def next(self):
        self.i = (self.i + 1) % len(self.banks)
        b = self.banks[self.i]
        return self.c.ps[b], f"ps{b}"


def build(n_layers=DEPTH, debug=False, stop_after=None):
    nc = bass.Bass("TRN2", target_bir_lowering=False)

    def din(name, shape, dt=F32):
        return nc.dram_tensor(name, list(shape), dt, kind="ExternalInput").ap()

    x = din("x", [NB, S, D])
    ctxin = din("ctx", [NB, C, D])
    c3 = din("c3", [3, D])
    w_ada = din("w_ada", [DEPTH, D, 6 * D])
    b_ada = din("b_ada", [DEPTH, 6 * D])
    wqkv_a = din("wqkv_a", [2, D, 1536])
    wo_a = din("wo_a", [2, D, D])
    sink_a = din("sink_a", [2, 16])
    wqkv_b = din("wqkv_b", [2, D, 3072])
    wo_b = din("wo_b", [2, D, D])
    lambda_b = din("lambda_b", [2, 4, 64])
    subln_b = din("subln_b", [2, 128])
    ln_attn_g = din("ln_attn_g", [DEPTH, D])
    ln_attn_b = din("ln_attn_b", [DEPTH, D])
    ln_ffn_g = din("ln_ffn_g", [DEPTH, D])
    ln_ffn_b = din("ln_ffn_b", [DEPTH, D])
    w_router = din("w_router", [D, E])
    router_bias = din("router_bias", [E])
    w_gate = din("w_gate", [DEPTH, E, D, DE])
    w_up = din("w_up", [DEPTH, E, D, DE])
    w_down = din("w_down", [DEPTH, E, DE, D])
    k_ident = din("k_ident", [128, 128])
    k_rt = din("k_rt", [128, 128])
    k_cos = din("k_cos", [128, S])
    k_sin = din("k_sin", [128, S])
    k_maskp = din("k_maskp", [128, 512])
    k_maskn = din("k_maskn", [128, 512])

    out = nc.dram_tensor("out", [NB, S, D], F32, kind="ExternalOutput").ap()
    skind = "ExternalOutput" if debug else "Internal"
    xa = nc.dram_tensor("xa", [NB, TOK, D], F32, kind=skind).ap()
    xb = nc.dram_tensor("xb", [NB, TOK, D], F32, kind=skind).ap()
    osc = nc.dram_tensor("osc", [NB, TOK, D], BF16, kind="Internal").ap()
    gsc = nc.dram_tensor("gsc", [DEPTH, 3, 2, 128, D], F32, kind="Internal").ap()
    if debug:
        dbg_gates = nc.dram_tensor("dbg_gates", [NB, TOK, E], F32, kind="ExternalOutput").ap()
        dbg_mod = nc.dram_tensor("dbg_mod", [128, DEPTH * 4 * 8 * 3], F32, kind="ExternalOutput").ap()
        dbg_o = nc.dram_tensor("dbg_o", [NB, TOK, D], F32, kind="ExternalOutput").ap()

    c = Ctx(nc)
    ps = c.ps

    identf = c.sbuf("identf", [128, 128], F32)
    identb = c.sbuf("identb", [128, 128], BF16)
    rtb = c.sbuf("rtb", [128, 128], BF16)
    modS = c.sbuf("modS", [128, DEPTH * 4 * 8 * 3], F32)
    h2T = c.sbuf("h2T", [128, 8, TOK], BF16)
    gates = c.sbuf("gates", [128, NT, E], F32)
    wrb = c.sbuf("wrb", [128, 8, E], BF16)
    rbias = c.sbuf("rbias", [128, E], F32)

    def mod_ap(l, kind, j, cls):
        o = ((l * 4 + kind) * 8 + j) * 3 + cls
        return modS[:, o:o + 1]

    c.dma("sp", identf[:], k_ident[:, :], [], ["identf"])
    c.dma("pool", identb[:], k_ident[:, :], [], ["identb"])
    c.dma("pool", rtb[:], k_rt[:, :], [], ["rtb"])
    c.dma("pool", wrb[:], w_router.rearrange("(k p) e -> p k e", p=128), [], ["wrb"])
    c.dma("sp", rbias[:], router_bias.partition_broadcast(128), [], ["rbias"])

    def ada_prologue():
        with c.phase("ada") as ph:
            c3s = ph.sbuf("c3s", [3, D], F32)
            silT = ph.sbuf("silT", [128, 8, 3], F32)
            silbc = [ph.sbuf(f"silbc{i}", [128, 8, 128], BF16) for i in range(3)]
            lnrows = ph.sbuf("lnrows", [64, 128], F32)
            lnT = ph.sbuf("lnT", [128, 64], F32)
            c.dma("sp", c3s[:], c3[:, :], [], ["c3s"])
            c.dma("sp", [lnrows[0:32, :], lnrows[32:64, :]],
                  [ln_attn_g.rearrange("l (j p) -> (l j) p", p=128),
                   ln_attn_b.rearrange("l (j p) -> (l j) p", p=128)], [], ["lnrows"])

            def e_ct(pe):
                for k in range(8):
                    ins = pe.matmul(ps[0][:, k * 3:(k + 1) * 3], c3s[0:3, k * 128:(k + 1) * 128],
                                    identf[0:3, 0:3], start=(k == 0), stop=True, skip_group_check=True)
                return ins
            c.op("pe", ["c3s", "identf"], ["ps0"], e_ct)
            c.op("act", ["ps0"], ["silT"],
                 lambda a: a.activation(out=silT[:].rearrange("p k c -> p (k c)"), in_=ps[0][:, 0:24], func=AF.Silu))
            for cls in range(3):
                c.op("dve", ["silT"], [f"silbc{cls}"],
                     lambda v, cls=cls: v.tensor_copy(out=silbc[cls][:],
                                                      in_=silT[:, :, cls:cls + 1].to_broadcast([128, 8, 128])))
            c.op("pe", ["lnrows", "identf"], ["ps1"],
                 lambda pe: pe.matmul(ps[1][:, 0:64], lnrows[0:64, :], identf[0:64, 0:64], start=True, stop=True))
            c.op("dve", ["ps1"], ["lnT"], lambda v: v.tensor_copy(out=lnT[:], in_=ps[1][:, 0:64]))

            wa_rot = Rot(ph, "wa", 2, [128, 8, 512], F32)
            wab_rot = Rot(ph, "wab", 2, [128, 8, 512], BF16)
            gst_rot = Rot(ph, "gst", 2, [128, 512], F32)
            gps = PsRot(c, [4, 5, 6])
            bt = ph.sbuf("bt", [48, 128], F32)
            bT = ph.sbuf("bT", [128, 48], F32)
            gbias = ph.sbuf("gbias", [128, 2, D], F32)
            mraw = ph.sbuf("mraw", [128, 32, 3], F32)
            tmp83 = ph.sbuf("tmp83", [128, 8, 3], F32)
            for l in range(n_layers):
                c.dma("sp", bt[:], b_ada[l].rearrange("(j p) -> j p", p=128), [], ["bt"])
                c.dma("sp", [gbias[:, 0, :], gbias[:, 1, :]],
                      [b_ada[l, 2048:3072].partition_broadcast(128),
                       b_ada[l, 5120:6144].partition_broadcast(128)], [], ["gbias"])
                c.op("pe", ["bt", "identf"], ["ps2"],
                     lambda pe: pe.matmul(ps[2][:, 0:48], bt[0:48, :], identf[0:48, 0:48], start=True, stop=True))
                c.op("act", ["ps2"], ["bT"], lambda a: a.copy(out=bT[:], in_=ps[2][:, 0:48]))
                first3 = True
                for cc in range(12):
                    kind = cc // 2
                    half = cc % 2
                    if kind in (2, 5):
                        wa, wak = wab_rot.next()
                        c.dma("pool", wa[:], w_ada[l][:, cc * 512:(cc + 1) * 512].rearrange("(k p) f -> p k f", p=128),
                              [], [wak])
                    else:
                        wa, wak = wa_rot.next()
                        c.dma("sp", wa[:], w_ada[l][:, cc * 512:(cc + 1) * 512].rearrange("(k p) f -> p k f", p=128),
                              [], [wak])
                    if kind in (2, 5):
                        which = 0 if kind == 2 else 1
                        for cls in range(3):
                            pst, psk = gps.next()

                            def e_g(pe, pst=pst, cls=cls, wa=wa):
                                for k in range(8):
                                    ins = pe.matmul(pst[:, :], silbc[cls][:, k, :], wa[:, k, :],
                                                    start=(k == 0), stop=(k == 7))
                                return ins
                            c.op("pe", [wak, f"silbc{cls}"], [psk], e_g)
                            gst, gstk = gst_rot.next()
                            c.op("dve", [psk, "gbias"], [gstk],
                                 lambda v, pst=pst, gst=gst, which=which, half=half: v.tensor_tensor(
                                     out=gst[:], in0=pst[:, :], in1=gbias[:, which, half * 512:(half + 1) * 512],
                                     op=ALU.add))
                            c.dma("sp", gsc[l, cls, which][:, half * 512:(half + 1) * 512], gst[:],
                                  [gstk], [("gsc", l, cls, which, half)])
                    else:
                        ks = {1: 0, 0: 1, 4: 2, 3: 3}[kind]

                        def e_m(pe, wa=wa, ks=ks, half=half, first=first3):
                            for fs in range(4):
                                slot = ks * 8 + half * 4 + fs
                                for k in range(8):
                                    ins = pe.matmul(ps[3][:, slot * 3:(slot + 1) * 3],
                                                    wa[:, k, fs * 128:(fs + 1) * 128], silT[:, k, :],
                                                    start=(first and fs == 0 and k == 0), stop=(k == 7),
                                                    skip_group_check=True)
                            return ins
                        c.op("pe", [wak, "silT"], ["ps3"], e_m)
                        first3 = False
                ps3v = ps[3][:, 0:96].rearrange("p (s c) -> p s c", c=3)
                for ks, a0 in ((0, 8), (1, 0), (2, 32), (3, 24)):
                    c.op("dve", ["ps3", "bT"], ["mraw"],
                         lambda v, ks=ks, a0=a0: v.tensor_tensor(
                             out=mraw[:, ks * 8:(ks + 1) * 8, :], in0=ps3v[:, ks * 8:(ks + 1) * 8, :],
                             in1=bT[:, a0:a0 + 8].unsqueeze(2).to_broadcast([128, 8, 3]), op=ALU.add))
                base = l * 96
                mv = modS[:, base:base + 96].rearrange("p (s c) -> p s c", c=3)
                lng = lnT[:, l * 8:(l + 1) * 8].unsqueeze(2).to_broadcast([128, 8, 3])
                lnb = lnT[:, 32 + l * 8:32 + (l + 1) * 8].unsqueeze(2).to_broadcast([128, 8, 3])
                c.op("dve", ["mraw"], ["modS"], lambda v: v.tensor_scalar_add(out=mv[:, 0:8, :], in0=mraw[:, 0:8, :], scalar1=1.0))
                c.op("dve", ["mraw"], ["modS"], lambda v: v.tensor_copy(out=mv[:, 8:16, :], in_=mraw[:, 8:16, :]))
                c.op("dve", ["mraw"], ["tmp83"], lambda v: v.tensor_scalar_add(out=tmp83[:], in0=mraw[:, 16:24, :], scalar1=1.0))
                c.op("dve", ["tmp83", "lnT"], ["modS"], lambda v: v.tensor_tensor(out=mv[:, 16:24, :], in0=tmp83[:], in1=lng, op=ALU.mult))
                c.op("dve", ["tmp83", "lnT"], ["tmp83"], lambda v: v.tensor_tensor(out=tmp83[:], in0=tmp83[:], in1=lnb, op=ALU.mult))
                c.op("dve", ["tmp83", "mraw"], ["modS"], lambda v: v.tensor_tensor(out=mv[:, 24:32, :], in0=tmp83[:], in1=mraw[:, 24:32, :], op=ALU.add))
            if debug:
                c.dma("sp", dbg_mod[:, :], modS[:], ["modS"], ["dbg_mod"], is_output=True)

    def xin_ap(l, b, t):
        if l == 0:
            if t < NTL:
                return x[b, t * 128:(t + 1) * 128, :], ("x", b, t)
            return ctxin[b, (t - NTL) * 128:(t - NTL + 1) * 128, :], ("ctx", b, t)
        return xa[b, t * 128:(t + 1) * 128, :], ("xa", b, t)

    def cls_of(b, t):
        return b if t < NTL else 2

    def make_hT(ph_rots, l, b, tiles, kind_s, kind_b, dst, dst_key, dst_col0, src_loader):
        tps = ph_rots["tps"]
        n = len(tiles)
        srcs = [src_loader(t) for t in tiles]
        for j in range(8):
            pst, psk = tps.next()
            pv = pst[:].bitcast(BF16)

            def e_t(pe, j=j, pv=pv):
                for i in range(n):
                    ins = pe.transpose(out=pv[:, i * 128:(i + 1) * 128], in_=srcs[i][0][:, j * 128:(j + 1) * 128],
                                       identity=identb[:])
                return ins
            c.op("pe", [s[1] for s in srcs] + ["identb"], [psk], e_t)
            cls = cls_of(b, tiles[0])
            c.op("act", [psk, "modS"], [dst_key],
                 lambda a, j=j, pv=pv, cls=cls: a.activation(
                     out=dst[:, j, dst_col0:dst_col0 + n * 128], in_=pv[:, 0:n * 128], func=AF.Identity,
                     bias=mod_ap(l, kind_b, j, cls), scale=mod_ap(l, kind_s, j, cls)))

    def layer_norm(ph_rots, r, rk, gb, gbk, xo, xok, xnb=None, xnbk=None):
        st, stk = ph_rots["stats"].next()
        xn, xnk = ph_rots["xn"].next()
        c.op("dve", [rk], [stk], lambda v: v.bn_stats(out=st[:, 0:6], in_=r[:, 0:512]))
        c.op("dve", [rk], [stk], lambda v: v.bn_stats(out=st[:, 6:12], in_=r[:, 512:1024]))
        c.op("dve", [stk], [stk], lambda v: v.bn_aggr(out=st[:, 12:14], in_=st[:, 0:12]))
        c.op("dve", [stk], [stk], lambda v: v.tensor_scalar_add(out=st[:, 14:15], in0=st[:, 13:14], scalar1=LN_EPS))
        c.op("act", [stk], [stk], lambda a: a.activation(out=st[:, 14:15], in_=st[:, 14:15], func=AF.Ln))
        c.op("act", [stk], [stk], lambda a: a.activation(out=st[:, 14:15], in_=st[:, 14:15], func=AF.Exp, scale=-0.5))
        c.op("dve", [stk], [stk], lambda v: v.scalar_tensor_tensor(out=st[:, 15:16], in0=st[:, 12:13], scalar=-1.0,
                                                                   in1=st[:, 14:15], op0=ALU.mult, op1=ALU.mult))
        c.op("act", [rk, stk], [xnk], lambda a: a.activation(out=xn[:], in_=r[:], func=AF.Identity,
                                                             bias=st[:, 15:16], scale=st[:, 14:15]))
        if xnb is not None:
            c.op("act", [rk, stk], [xnbk], lambda a: a.activation(out=xnb[:], in_=r[:], func=AF.Identity,
                                                                  bias=st[:, 15:16], scale=st[:, 14:15]))
        c.op("pool", [xnk, gbk], [xnk], lambda g: g.tensor_tensor(out=xn[:], in0=xn[:], in1=gb[:, 0, :], op=ALU.mult))
        c.op("pool", [xnk, gbk], [xok], lambda g: g.tensor_tensor(out=xo[:], in0=xn[:], in1=gb[:, 1, :], op=ALU.add))

    def qkv_project(ph, rots, l, b, last, wq, wqk, nq, wk, wkk, nk, wv, wvk, vcols, QT, KT, VA, cosT, sinT):
        xf_rot, xb_rot, hT_rot, raw_rot, t1_rot, t2_rot = (rots[k] for k in ("xf", "xbf", "hT", "raw", "t1", "t2"))
        mm = rots["mm"]
        vheads = VA.shape[2]
        vd = VA.shape[3] - 1
        groups = [list(range(g * 4, g * 4 + 4)) for g in range(4)] + [[16, 17]]
        for tiles in groups:
            is_ctx = tiles[0] >= NTL
            T = len(tiles) * 128
            col0 = tiles[0] * 128
            loaded = {}

            def loader(t):
                if t in loaded:
                    return loaded[t]
                xf, xfk = xf_rot.next()
                ap, dk = xin_ap(l, b, t)
                c.dma("sp", xf[:], ap, [dk], [xfk])
                xbt, xbk = xb_rot.next()
                c.op("pool", [xfk], [xbk], lambda g, xf=xf, xbt=xbt: g.tensor_copy(out=xbt[:], in_=xf[:]))
                loaded[t] = (xbt, xbk)
                return loaded[t]
            hT, hTk = hT_rot.next()
            make_hT(rots, l, b, tiles, 0, 1, hT, hTk, 0, loader)
            jobs = []
            if not (is_ctx and last):
                jobs += [("q", i) for i in range(nq)]
            jobs += [("k", i) for i in range(nk)]
            def emit_proj(kind, i):
                w, wkey = (wq, wqk) if kind == "q" else (wk, wkk)
                dstT, dkey = (QT, "QT") if kind == "q" else (KT, "KT")
                pst, psk = mm.next()

                def e_p(pe):
                    for k in range(8):
                        ins = pe.matmul(pst[:, 0:T], w[:, k, i * 128:(i + 1) * 128], hT[:, k, 0:T],
                                        start=(k == 0), stop=(k == 7))
                    return ins
                c.op("pe", [wkey, hTk], [psk], e_p)
                if is_ctx:
                    c.op("act", [psk], [(dkey, i, tiles[0])],
                         lambda a: a.copy(out=dstT[:, i, col0:col0 + T], in_=pst[:, 0:T]))
                    return None
                raw, rawk = raw_rot.next()
                c.op("act", [psk], [rawk], lambda a: a.copy(out=raw[:, 0:T], in_=pst[:, 0:T]))
                return (dstT, dkey, i, raw, rawk)

            def emit_rope(st):
                dstT, dkey, i, raw, rawk = st
                pst2, psk2 = mm.next()
                c.op("pe", [rawk, "rtb"], [psk2],
                     lambda pe: pe.matmul(pst2[:, 0:T], rtb[:], raw[:, 0:T], start=True, stop=True))
                t1, t1k = t1_rot.next()
                t2, t2k = t2_rot.next()
                c.op("dve", [rawk, "cos"], [t1k],
                     lambda g: g.tensor_tensor(out=t1[:, 0:T], in0=raw[:, 0:T], in1=cosT[:, col0:col0 + T], op=ALU.mult))
                c.op("dve", [psk2, "sin"], [t2k],
                     lambda v: v.tensor_tensor(out=t2[:, 0:T], in0=pst2[:, 0:T], in1=sinT[:, col0:col0 + T], op=ALU.mult))
                c.op("pool", [t1k, t2k], [(dkey, i, tiles[0])],
                     lambda g: g.tensor_tensor(out=dstT[:, i, col0:col0 + T], in0=t1[:, 0:T], in1=t2[:, 0:T], op=ALU.add))

            prev = None
            for job in jobs + [None]:
                cur = emit_proj(*job) if job is not None else None
                if prev is not None:
                    emit_rope(prev)
                prev = cur
            for si, t in enumerate(tiles):
                pst, psk = mm.next()

                def e_v(pe, pst=pst, hT=hT, si=si):
                    for k in range(8):
                        ins = pe.matmul(pst[:, 0:vcols], hT[:, k, si * 128:(si + 1) * 128], wv[:, k, 0:vcols],
                                        start=(k == 0), stop=(k == 7))
                    return ins
                c.op("pe", [wvk, hTk], [psk], e_v)
                c.op("act", [psk], [("VA", t)],
                     lambda a, pst=pst, t=t: a.copy(out=VA[:, t, :, 0:vd],
                                                    in_=pst[:, 0:vcols].rearrange("p (h d) -> p h d", d=vd)))

    def std_rots(ph, raw_cols=512):
        return {
            "xf": Rot(ph, "xf", 3, [128, D], F32),
            "xbf": Rot(ph, "xbf", 5, [128, D], BF16),
            "hT": Rot(ph, "hT", 2, [128, 8, 512], BF16),
            "raw": Rot(ph, "raw", 3, [128, 512], BF16),
            "t1": Rot(ph, "t1", 2, [128, 512], F32),
            "t2": Rot(ph, "t2", 2, [128, 512], F32),
            "tps": PsRot(c, [0, 1]),
            "mm": PsRot(c, [2, 3, 4, 5]),
        }

    def phase_attn_A(l, b, last):
        jj = l // 2
        with c.phase(f"attnA_{l}_{b}") as ph:
            rots = std_rots(ph)
            wq = ph.sbuf("wq", [128, 8, 1024], BF16)
            wk = ph.sbuf("wk", [128, 8, 4, 2, 64], BF16)
            wv = ph.sbuf("wv", [128, 8, 256], BF16)
            cosT = ph.sbuf("cosT", [128, S], F32)
            sinT = ph.sbuf("sinT", [128, S], F32)
            maskp = ph.sbuf("maskp", [128, 512], BF16)
            maskn = ph.sbuf("maskn", [128, 512], BF16)
            esink = ph.sbuf("esink", [128, 16], F32)
            QT = ph.sbuf("QT", [128, 8, TOK], BF16)
            KT = ph.sbuf("KT", [128, 4, TOK], BF16)
            VA = ph.sbuf("VA", [128, NT, 4, 65], BF16)
            wsrc = wqkv_a[jj]
            for k in range(8):
                c.dma("pool", wq[:, k, :], wsrc[k * 128:(k + 1) * 128, 0:1024], [], ["wq"])
            ksrc = wsrc[:, 1024:1280].rearrange("(k p) (g d) -> p k g d", p=128, d=64)
            for k in range(8):
                c.dma("pool", [wk[:, k, :, 0, :], wk[:, k, :, 1, :]], [ksrc[:, k], ksrc[:, k]], [], ["wk"])
            c.dma("pool", wv[:], wsrc[:, 1280:1536].rearrange("(k p) f -> p k f", p=128), [], ["wv"])
            c.dma("sp", cosT[:], k_cos[:, :], [], ["cos"])
            c.dma("sp", sinT[:], k_sin[:, :], [], ["sin"])
            c.dma("pool", maskp[:], k_maskp[:, :], [], ["maskp"])
            c.dma("pool", maskn[:], k_maskn[:, :], [], ["maskn"])
            c.dma("sp", esink[:], sink_a[jj].partition_broadcast(128), [], ["esink"])
            c.op("act", ["esink"], ["esink"], lambda a: a.activation(out=esink[:], in_=esink[:], func=AF.Exp))
            c.op("pool", [], [("VA", t) for t in range(NT)], lambda g: g.memset(VA[:, :, :, 64:65], 1.0))
            wkv = wk[:].rearrange("p k g r d -> p k (g r d)")
            qkv_project(ph, rots, l, b, last, wq, "wq", 8, wkv, "wk", 4, wv, "wv", 256, QT, KT, VA, cosT, sinT)

            if stop_after is not None and stop_after[0] == "qkv":
                return
            spsx = PsRot(c, [0, 2, 6])
            spsy = PsRot(c, [1, 3, 7])
            ops_ = PsRot(c, [4, 5])
            pT_rot = Rot(ph, "pT", 4, [128, 512], BF16)
            ot_rot = Rot(ph, "ot", 2, [128, 16, 64], BF16)
            dn_rot = Rot(ph, "dn", 2, [128, 8], F32)
            qtiles = list(range(NTL)) + ([] if last else [16, 17])
            pcol = {0: 0, 2: 128, 1: 256, 3: 384}
            LA = 2
            its = []
            for qt in qtiles:
                if qt < NTL:
                    kts = ([(qt - 1, maskp, "maskp")] if qt > 0 else []) + [(qt, None, None)] + \
                          ([(qt + 1, maskn, "maskn")] if qt < NTL - 1 else []) + [(16, None, None), (17, None, None)]
                else:
                    kts = [(16, None, None), (17, None, None)]
                for g in range(4):
                    for ki, (kt, msk, mskk) in enumerate(kts):
                        its.append((qt, g, ki, len(kts), kt, msk, mskk))
            state = {}

            def emit_s(it):
                qt, g, ki, nk, kt, msk, mskk = it
                sx, sxk = spsx.next()
                sy, syk = spsy.next()

                def e_s(pe):
                    if msk is not None:
                        pe.matmul(sx[:, 0:256], identb[:], msk[:, 0:256], start=True, stop=False, skip_group_check=True)
                        pe.matmul(sy[:, 0:256], identb[:], msk[:, 0:256], start=True, stop=False, skip_group_check=True)
                    for hd in range(4):
                        cch = 2 * g + hd // 2
                        base = 64 * (hd % 2)
                        dst = sx if hd % 2 == 0 else sy
                        o = 128 * (hd // 2)
                        ins = pe.matmul(dst[:, o:o + 128],
                                        KT[base:base + 64, g, kt * 128:(kt + 1) * 128],
                                        QT[base:base + 64, cch, qt * 128:(qt + 1) * 128],
                                        start=(msk is None and hd < 2), stop=(hd >= 2), skip_group_check=True)
                    return ins
                rk = [("KT", g, (kt // 4) * 4 if kt < 16 else 16), ("QT", 2 * g, (qt // 4) * 4 if qt < 16 else 16),
                      ("QT", 2 * g + 1, (qt // 4) * 4 if qt < 16 else 16), "identb"] + ([mskk] if mskk else [])
                c.op("pe", rk, [sxk, syk], e_s)
                pT, pTk = pT_rot.next()
                c.op("act", [sxk], [(pTk, 0)],
                     lambda a: a.activation(out=pT[:, 0:256], in_=sx[:, 0:256], func=AF.Exp, scale=SCALE))
                c.op("act", [syk], [(pTk, 1)],
                     lambda a: a.activation(out=pT[:, 256:512], in_=sy[:, 0:256], func=AF.Exp, scale=SCALE))
                return (pT, pTk)

            def emit_pv(it, pt):
                qt, g, ki, nk, kt, msk, mskk = it
                pT, pTk = pt
                if g == 0 and ki == 0:
                    state["ot"] = ot_rot.next()
                if ki == 0:
                    state["op"] = ops_.next()
                ot, otk = state["ot"]
                opst, opsk = state["op"]

                def e_o(pe):
                    for hd in range(4):
                        ins = pe.matmul(opst[:, hd * 128:hd * 128 + 65], pT[:, pcol[hd]:pcol[hd] + 128],
                                        VA[:, kt, g, :], start=(ki == 0 and hd == 0), stop=(ki == nk - 1),
                                        skip_group_check=True)
                    return ins
                c.op("pe", [(pTk, 0), (pTk, 1), ("VA", kt)], [opsk], e_o)
                if ki != nk - 1:
                    return
                dn, dnk = dn_rot.next()
                ov = opst[:, :].rearrange("p (h d) -> p h d", d=128)
                c.op("dve", [opsk, "esink"], [dnk],
                     lambda v: v.tensor_tensor(out=dn[:, 0:4], in0=ov[:, :, 64], in1=esink[:, 4 * g:4 * g + 4], op=ALU.add))
                c.op("dve", [dnk], [dnk], lambda v: v.reciprocal(out=dn[:, 4:8], in_=dn[:, 0:4]))
                c.op("dve", [opsk, dnk], [otk],
                     lambda v: v.tensor_tensor(
                         out=ot[:, 4 * g:4 * g + 4, :], in0=ov[:, :, 0:64],
                         in1=dn[:, 4:8].unsqueeze(2).to_broadcast([128, 4, 64]), op=ALU.mult))
                if g == 3:
                    c.dma("pool", osc[b, qt * 128:(qt + 1) * 128, :], ot[:].rearrange("p h d -> p (h d)"), [otk], [("osc", b, qt)])

            pend = []
            for idx in range(len(its) + LA):
                if idx < len(its):
                    pend.append(emit_s(its[idx]))
                if idx - LA >= 0:
                    emit_pv(its[idx - LA], pend[idx - LA])

    def phase_attn_B(l, b, last, hh):
        jj = l // 2
        lam_init = 0.8 - 0.6 * math.exp(-0.3 * l)
        with c.phase(f"attnB_{l}_{b}_{hh}") as ph:
            rots = std_rots(ph)
            wq = ph.sbuf("wq", [128, 8, 512], BF16)
            wk = ph.sbuf("wk", [128, 8, 512], BF16)
            wv = ph.sbuf("wv", [128, 8, 512], BF16)
            cosT = ph.sbuf("cosT", [128, S], F32)
            sinT = ph.sbuf("sinT", [128, S], F32)
            QT = ph.sbuf("QT", [128, 4, TOK], BF16)
            KT = ph.sbuf("KT", [128, 4, TOK], BF16)
            VA = ph.sbuf("VA", [128, NT, 4, 129], BF16)
            lamt = ph.sbuf("lamt", [128, 4, 64], F32)
            lsm = ph.sbuf("lsm", [128, 8], F32)
            gsub = ph.sbuf("gsub", [128, 128], F32)
            wsrc = wqkv_b[jj]
            for wt, key, c0 in ((wq, "wq", hh * 512), (wk, "wk", 1024 + hh * 512), (wv, "wv", 2048 + hh * 512)):
                for k in range(8):
                    c.dma("pool", wt[:, k, :], wsrc[k * 128:(k + 1) * 128, c0:c0 + 512], [], [key])
            c.dma("sp", cosT[:], k_cos[:, :], [], ["cos"])
            c.dma("sp", sinT[:], k_sin[:, :], [], ["sin"])
            c.dma("sp", lamt[:].rearrange("p a d -> p (a d)"),
                  lambda_b[jj].rearrange("a d -> (a d)").partition_broadcast(128), [], ["lamt"])
            c.dma("sp", gsub[:], subln_b[jj].partition_broadcast(128), [], ["gsub"])
            c.op("dve", ["lamt"], ["lamt"], lambda v: v.tensor_tensor(out=lamt[:, 0, :], in0=lamt[:, 0, :], in1=lamt[:, 1, :], op=ALU.mult))
            c.op("dve", ["lamt"], ["lamt"], lambda v: v.tensor_tensor(out=lamt[:, 2, :], in0=lamt[:, 2, :], in1=lamt[:, 3, :], op=ALU.mult))
            c.op("dve", ["lamt"], ["lsm"], lambda v: v.reduce_sum(out=lsm[:, 0:1], in_=lamt[:, 0, :], axis=AX.X))
            c.op("dve", ["lamt"], ["lsm"], lambda v: v.reduce_sum(out=lsm[:, 1:2], in_=lamt[:, 2, :], axis=AX.X))
            c.op("act", ["lsm"], ["lsm"], lambda a: a.activation(out=lsm[:, 2:4], in_=lsm[:, 0:2], func=AF.Exp))
            c.op("dve", ["lsm"], ["lsm"], lambda v: v.tensor_tensor(out=lsm[:, 4:5], in0=lsm[:, 3:4], in1=lsm[:, 2:3], op=ALU.subtract))
            c.op("dve", ["lsm"], ["lsm"], lambda v: v.tensor_scalar_add(out=lsm[:, 4:5], in0=lsm[:, 4:5], scalar1=-lam_init))
            c.op("dve", ["gsub"], ["gsub"], lambda v: v.tensor_scalar_mul(out=gsub[:], in0=gsub[:], scalar1=(1.0 - lam_init)))
            c.op("pool", [], [("VA", t) for t in range(NT)], lambda g: g.memset(VA[:, :, :, 128:129], 1.0))
            qkv_project(ph, rots, l, b, last, wq, "wq", 4, wk, "wk", 4, wv, "wv", 512, QT, KT, VA, cosT, sinT)

            spsx = PsRot(c, [0, 2])
            spsy = PsRot(c, [1, 3])
            pT_rot = Rot(ph, "pT", 4, [128, 2, 512], BF16)
            ot_rot = Rot(ph, "ot", 2, [128, 4, 512], BF16)
            sm_rot = Rot(ph, "sm", 2, [128, 16], F32)
            of_rot = Rot(ph, "of", 2, [128, 4, 128], F32)
            jk_rot = Rot(ph, "jk", 2, [128, 128], F32)
            qgroups = [list(range(g * 4, g * 4 + 4)) for g in range(4)] + ([] if last else [[16, 17]])
            LA = 2
            for qg in qgroups:
                nq = len(qg)
                Tq = nq * 128
                q0 = qg[0] * 128
                kts = list(range(NT)) if qg[0] < NTL else [16, 17]
                nk = len(kts)
                ot, otk = ot_rot.next()
                gq = qg[0]
                its = [(h, ki, kt) for h in range(4) for ki, kt in enumerate(kts)]
                pend = []

                def emit_s(it):
                    h, ki, kt = it
                    sx, sxk = spsx.next()
                    sy, syk = spsy.next()

                    def e_s(pe):
                        pe.matmul(sx[:, 0:Tq], KT[0:64, h, kt * 128:(kt + 1) * 128], QT[0:64, h, q0:q0 + Tq], start=True, stop=True)
                        return pe.matmul(sy[:, 0:Tq], KT[64:128, h, kt * 128:(kt + 1) * 128], QT[64:128, h, q0:q0 + Tq], start=True, stop=True)
                    c.op("pe", [("KT", h, (kt // 4) * 4 if kt < 16 else 16), ("QT", h, gq)], [sxk, syk], e_s)
                    pT, pTk = pT_rot.next()
                    c.op("act", [sxk], [(pTk, 0)],
                         lambda a: a.activation(out=pT[:, 0, 0:Tq], in_=sx[:, 0:Tq], func=AF.Exp, scale=SCALE))
                    c.op("act", [syk], [(pTk, 1)],
                         lambda a: a.activation(out=pT[:, 1, 0:Tq], in_=sy[:, 0:Tq], func=AF.Exp, scale=SCALE))
                    return (pT, pTk)

                def emit_pv(it, pt):
                    h, ki, kt = it
                    pT, pTk = pt

                    def e_o(pe):
                        for qi in range(nq):
                            for m in range(2):
                                ins = pe.matmul(ps[4 + qi][:, m * 256:m * 256 + 129], pT[:, m, qi * 128:(qi + 1) * 128], VA[:, kt, h, :],
                                                start=(ki == 0 and m == 0), stop=(ki == nk - 1), skip_group_check=True)
                        return ins
                    c.op("pe", [(pTk, 0), (pTk, 1), ("VA", kt)], [f"ps{4 + qi}" for qi in range(nq)], e_o)
                    if ki != nk - 1:
                        return
                    sm, smk = sm_rot.next()
                    of, ofk = of_rot.next()
                    for qi in range(nq):
                        pk = f"ps{4 + qi}"
                        pb = ps[4 + qi]
                        c.op("dve", [pk], [smk], lambda v, qi=qi, pb=pb: v.reciprocal(out=sm[:, qi:qi + 1], in_=pb[:, 128:129]))
                        c.op("dve", [pk, smk], [smk], lambda v, qi=qi, pb=pb: v.reciprocal(out=sm[:, 4 + qi:5 + qi], in_=pb[:, 384:385]))
                        c.op("dve", ["lsm", smk], [smk],
                             lambda v, qi=qi: v.tensor_scalar_mul(out=sm[:, 4 + qi:5 + qi], in0=sm[:, 4 + qi:5 + qi], scalar1=lsm[:, 4:5]))
                        c.op("dve", [pk, smk], [ofk],
                             lambda v, qi=qi, pb=pb: v.tensor_scalar_mul(out=of[:, qi, :], in0=pb[:, 0:128], scalar1=sm[:, qi:qi + 1]))
                        c.op("dve", [pk, smk, ofk], [ofk],
                             lambda v, qi=qi, pb=pb: v.scalar_tensor_tensor(out=of[:, qi, :], in0=pb[:, 256:384], scalar=sm[:, 4 + qi:5 + qi], in1=of[:, qi, :], op0=ALU.mult, op1=ALU.add))
                        jk, jkk = jk_rot.next()
                        c.op("act", [ofk], [jkk, smk],
                             lambda a, qi=qi, jk=jk: a.activation(out=jk[:], in_=of[:, qi, :], func=AF.Square, accum_out=sm[:, 8 + qi:9 + qi]))
                    c.op("dve", [smk], [smk], lambda v: v.tensor_scalar(out=sm[:, 8:8 + nq], in0=sm[:, 8:8 + nq], scalar1=1.0 / 128.0, scalar2=SUBLN_EPS, op0=ALU.mult, op1=ALU.add))
                    c.op("act", [smk], [smk], lambda a: a.activation(out=sm[:, 12:12 + nq], in_=sm[:, 8:8 + nq], func=AF.Ln))
                    c.op("act", [smk], [smk], lambda a: a.activation(out=sm[:, 12:12 + nq], in_=sm[:, 12:12 + nq], func=AF.Exp, scale=-0.5))
                    for qi in range(nq):
                        c.op("dve", [ofk, smk, "gsub"], [otk],
                             lambda v, qi=qi: v.scalar_tensor_tensor(
                                 out=ot[:, qi, h * 128:(h + 1) * 128], in0=of[:, qi, :], scalar=sm[:, 12 + qi:13 + qi], in1=gsub[:], op0=ALU.mult, op1=ALU.mult))

                for idx in range(len(its) + LA):
                    if idx < len(its):
                        pend.append(emit_s(its[idx]))
                    if idx - LA >= 0:
                        emit_pv(its[idx - LA], pend[idx - LA])
                for qi, t in enumerate(qg):
                    c.dma("pool", osc[b, t * 128:(t + 1) * 128, hh * 512:(hh + 1) * 512], ot[:, qi, :], [otk], [("osc", b, t, hh)])

    def phase_oproj(l, b, last):
        typ_a = (l % 2 == 0)
        jj = l // 2
        with c.phase(f"oproj_{l}_{b}") as ph:
            wo = ph.sbuf("wo", [128, 8, D], BF16)
            gb1 = ph.sbuf("gb1", [128, 2, D], F32)
            g1l = ph.sbuf("g1l", [128, D], F32)
            g1c = ph.sbuf("g1c", [128, D], F32)
            wsrc = (wo_a if typ_a else wo_b)[jj]
            for k in range(8):
                c.dma("pool", wo[:, k, :], wsrc[k * 128:(k + 1) * 128, :], [], ["wo"])
            c.dma("sp", [gb1[:, 0, :], gb1[:, 1, :]],
                  [ln_attn_g[l].partition_broadcast(128), ln_attn_b[l].partition_broadcast(128)], [], ["gb1"])
            c.dma("sp", g1l[:], gsc[l, b, 0], [("gsc", l, b, 0, 0), ("gsc", l, b, 0, 1)], ["g1l"])
            c.dma("sp", g1c[:], gsc[l, 2, 0], [("gsc", l, 2, 0, 0), ("gsc", l, 2, 0, 1)], ["g1c"])
            rots = {"stats": Rot(ph, "st", 3, [128, 16], F32), "xn": Rot(ph, "xn", 2, [128, D], F32),
                    "tps": PsRot(c, [0, 1])}
            ob_rot = Rot(ph, "ob", 3, [128, D], BF16)
            xf_rot = Rot(ph, "xf", 3, [128, D], F32)
            oT_rot = Rot(ph, "oT", 2, [128, 8, 128], BF16)
            r_rot = Rot(ph, "r", 2, [128, D], F32)
            x1_rot = Rot(ph, "x1", 2, [128, D], F32)
            xnb_rot = Rot(ph, "xnb", 3, [128, D], BF16)
            sg_rot = Rot(ph, "sg", 2, [128, 64], F32)
            alps = PsRot(c, [2, 3, 4, 5])
            rps = PsRot(c, [6, 7])
            tiles = list(range(NTL)) + ([] if last else [16, 17])
            loads = {}
            st1 = {}

            def load(t):
                ob, obk = ob_rot.next()
                c.dma("sp", ob[:], osc[b, t * 128:(t + 1) * 128, :], [], [obk])
                xf, xfk = xf_rot.next()
                ap, dk = xin_ap(l, b, t)
                c.dma("sp", xf[:], ap, [dk], [xfk])
                loads[t] = (ob, obk, xf, xfk)

            def stage1(t):
                g1 = g1l if t < NTL else g1c
                g1k = "g1l" if t < NTL else "g1c"
                ob, obk, xf, xfk = loads.pop(t)
                pst, psk = rots["tps"].next()
                pv = pst[:].bitcast(BF16)

                def e_t(pe):
                    for j in range(8):
                        ins = pe.transpose(out=pv[:, j * 128:(j + 1) * 128], in_=ob[:, j * 128:(j + 1) * 128], identity=identb[:])
                    return ins
                c.op("pe", [obk, "identb"], [psk], e_t)
                oT, oTk = oT_rot.next()
                c.op("act", [psk], [oTk], lambda a: a.copy(out=oT[:].rearrange("p j t -> p (j t)"), in_=pv[:, :]))
                r, rk = r_rot.next()
                for half in range(2):
                    apst, apsk = alps.next()

                    def e_a(pe, apst=apst, half=half):
                        for k in range(8):
                            ins = pe.matmul(apst[:, :], oT[:, k, :], wo[:, k, half * 512:(half + 1) * 512], start=(k == 0), stop=(k == 7))
                        return ins
                    c.op("pe", [oTk, "wo"], [apsk], e_a)
                    c.op("dve", [apsk, g1k], [(rk, half)],
                         lambda v, apst=apst, half=half: v.tensor_tensor(out=r[:, half * 512:(half + 1) * 512], in0=apst[:, :], in1=g1[:, half * 512:(half + 1) * 512], op=ALU.mult))
                c.op("dve", [(rk, 0), (rk, 1), xfk], [rk],
                     lambda g: g.scalar_tensor_tensor(out=r[:], in0=xf[:], scalar=ALPHA, in1=r[:], op0=ALU.mult, op1=ALU.add))
                x1, x1k = x1_rot.next()
                xnb, xnbk = xnb_rot.next()
                layer_norm(rots, r, rk, gb1, "gb1", x1, x1k, xnb, xnbk)
                c.dma("pool", xb[b, t * 128:(t + 1) * 128, :], x1[:], [x1k], [("xb", b, t)])
                st1[t] = (xnb, xnbk)

            h2ps = PsRot(c, [6, 7])

            def stage2(t0):
                cls = cls_of(b, t0)
                pair = [st1.pop(t0), st1.pop(t0 + 1)]
                for jh in range(2):
                    pst, psk = h2ps.next()
                    pv = pst[:].bitcast(BF16)

                    def e_t2(pe):
                        for jj_ in range(4):
                            j = jh * 4 + jj_
                            for i in range(2):
                                ins = pe.transpose(out=pv[:, jj_ * 256 + i * 128:jj_ * 256 + (i + 1) * 128],
                                                   in_=pair[i][0][:, j * 128:(j + 1) * 128], identity=identb[:])
                        return ins
                    c.op("pe", [pair[0][1], pair[1][1], "identb"], [psk], e_t2)
                    for jj_ in range(4):
                        j = jh * 4 + jj_
                        c.op("act", [psk, "modS"], [("h2T", t0, j)],
                             lambda a, j=j, jj_=jj_, pv=pv: a.activation(
                                 out=h2T[:, j, t0 * 128:(t0 + 2) * 128], in_=pv[:, jj_ * 256:(jj_ + 1) * 256], func=AF.Identity,
                                 bias=mod_ap(l, 3, j, cls), scale=mod_ap(l, 2, j, cls)))

            n = len(tiles)
            for i in range(min(2, n)):
                load(tiles[i])
            for i in range(n):
                stage1(tiles[i])
                if i + 2 < n:
                    load(tiles[i + 2])
                if i >= 2 and i % 2 == 0:
                    stage2(tiles[i - 2])
            for t0 in sorted(k for k in list(st1.keys()) if k % 2 == 0):
                stage2(t0)
            nt = n
            rp = ps[2]

            def e_r(pe):
                for ti, t in enumerate(tiles):
                    for k in range(8):
                        ins = pe.matmul(rp[:, ti * E:(ti + 1) * E], h2T[:, k, t * 128:(t + 1) * 128], wrb[:, k, :],
                                        start=(ti == 0 and k == 0), stop=(k == 7), skip_group_check=True)
                return ins
            c.op("pe", [("h2T", t0, j) for t0 in tiles[::2] for j in range(8)] + ["wrb"], ["ps2"], e_r)
            router_topk(ph, rp, "ps2", tiles)
            if debug:
                c.dma("sp", dbg_gates[b].rearrange("(t p) e -> p t e", p=128), gates[:], [("gates", t) for t in tiles], [("dbg_gates", b)], is_output=True)

    def router_topk(ph, rp, rpk, tiles):
        G = len(tiles)
        W = G * E
        sgs = ph.sbuf("rt_s", [128, NT * E], F32)
        sgb = ph.sbuf("rt_b", [128, NT * E], F32)
        scr = ph.sbuf("rt_x", [128, 8, NT * 4], F32)
        red = ph.sbuf("rt_r", [128, 2, NT], F32)
        k = "rt"
        s_ = sgs[:, 0:W]
        b_all = sgb[:, 0:W]
        Bv = b_all.rearrange("p (g i) -> p g i", i=4)
        n4 = G * 4

        def sc(i):
            return scr[:, i, 0:n4]
        c.op("act", [rpk], [k], lambda a: a.activation(out=s_, in_=rp[:, 0:W], func=AF.Exp, scale=-1.0))
        c.op("dve", [k], [k], lambda v: v.tensor_scalar_add(out=s_, in0=s_, scalar1=1.0))
        c.op("dve", [k], [k], lambda v: v.reciprocal(out=s_, in_=s_))
        c.op("dve", [k, "rbias"], [k], lambda v: v.tensor_tensor(
            out=b_all.rearrange("p (t e) -> p t e", e=E), in0=s_.rearrange("p (t e) -> p t e", e=E),
            in1=rbias[:].unsqueeze(1).to_broadcast([128, G, E]), op=ALU.add))
        a0, a1, a2, a3 = (Bv[:, :, i] for i in range(4))
        P_, Q_, R_, S_, M1, M2, GS = sc(0), sc(1), sc(2), sc(3), sc(4), sc(5), sc(6)
        steps = [
            (P_, a0, a1, ALU.max), (Q_, a0, a1, ALU.min), (R_, a2, a3, ALU.max), (S_, a2, a3, ALU.min),
            (M1, P_, R_, ALU.max),
            (M2, P_, R_, ALU.min),
            (Q_, Q_, S_, ALU.max),
            (M2, M2, Q_, ALU.max),
            (GS, M1, M2, ALU.add),
        ]
        for (o, i0, i1, op_) in steps:
            c.op("dve", [k], [k], lambda v, o=o, i0=i0, i1=i1, op_=op_: v.tensor_tensor(out=o, in0=i0, in1=i1, op=op_))
        gs3 = GS.rearrange("p (t g) -> p t g", g=4)
        c.op("dve", [k], [k], lambda v: v.tensor_reduce(out=red[:, 0, 0:G], in_=gs3, axis=AX.X, op=ALU.max))
        c.op("dve", [k], [k], lambda v: v.tensor_tensor(out=gs3, in0=gs3, in1=red[:, 0, 0:G].unsqueeze(2).to_broadcast([128, G, 4]), op=ALU.is_ge))
        c.op("dve", [k], [k], lambda v: v.tensor_tensor(out=Bv, in0=Bv, in1=M2.unsqueeze(2).to_broadcast([128, n4, 4]), op=ALU.is_ge))
        c.op("dve", [k], [k], lambda v: v.tensor_tensor(out=Bv, in0=Bv, in1=GS.unsqueeze(2).to_broadcast([128, n4, 4]), op=ALU.mult))
        c.op("dve", [k], [k], lambda v: v.tensor_tensor(out=b_all, in0=b_all, in1=s_, op=ALU.mult))
        b3 = b_all.rearrange("p (t e) -> p t e", e=E)
        c.op("dve", [k], [k], lambda v: v.tensor_reduce(out=red[:, 0, 0:G], in_=b3, axis=AX.X, op=ALU.add))
        c.op("dve", [k], [k], lambda v: v.reciprocal(out=red[:, 1, 0:G], in_=red[:, 0, 0:G]))
        t0 = tiles[0]
        c.op("dve", [k], [("gates", t) for t in tiles],
             lambda v: v.tensor_tensor(out=gates[:, t0:t0 + G, :], in0=b3,
                                       in1=red[:, 1, 0:G].unsqueeze(2).to_broadcast([128, G, E]), op=ALU.mult))

    def phase_moe(l, b, last):
        ntile = NTL if last else NT
        ntok = ntile * 128
        with c.phase() as pho:
            acc = pho.sbuf("acc", [128, NT, D], F32)
            with c.phase(f"moe_{l}_{b}") as ph:
                wg_rot = Rot(ph, "wg", 2, [128, 8, DE], BF16)
                wu_rot = Rot(ph, "wu", 2, [128, 8, DE], BF16)
                wd_rot = Rot(ph, "wd", 2, [128, 4, D], BF16)
                sg_rot = Rot(ph, "sgl", 2, [128, 512], BF16)
                act_rot = Rot(ph, "act", 2, [128, 4, 512], BF16)
                gps = PsRot(c, [0, 1])
                ups = PsRot(c, [2, 3])
                yps = PsRot(c, [4, 5, 6, 7])
                ttiles = []
                t0 = 0
                while t0 < ntile:
                    n = min(4, ntile - t0)
                    ttiles.append((t0, n))
                    t0 += n
                for e in range(E):
                    wg, wgk = wg_rot.next()
                    wu, wuk = wu_rot.next()
                    wd, wdk = wd_rot.next()
                    c.dma("pool", wg[:], w_gate[l, e].rearrange("(k p) f -> p k f", p=128), [], [wgk])
                    c.dma("pool", wu[:], w_up[l, e].rearrange("(k p) f -> p k f", p=128), [], [wuk])
                    c.dma("pool", wd[:], w_down[l, e].rearrange("(k p) f -> p k f", p=128), [], [wdk])
                    for (t0, n) in ttiles:
                        T = n * 128
                        c0 = t0 * 128
                        hk = [("h2T", t) for t in range(t0, t0 + n)]
                        at, atk = act_rot.next()
                        for fc in range(4):
                            gp, gpk = gps.next()
                            up, upk = ups.next()

                            def e_g(pe, gp=gp, wg=wg, fc=fc, c0=c0, T=T):
                                for k in range(8):
                                    ins = pe.matmul(gp[:, 0:T], wg[:, k, fc * 128:(fc + 1) * 128], h2T[:, k, c0:c0 + T], start=(k == 0), stop=(k == 7))
                                return ins

                            def e_u(pe, up=up, wu=wu, fc=fc, c0=c0, T=T):
                                for k in range(8):
                                    ins = pe.matmul(up[:, 0:T], wu[:, k, fc * 128:(fc + 1) * 128], h2T[:, k, c0:c0 + T], start=(k == 0), stop=(k == 7))
                                return ins
                            c.op("pe", [wgk] + hk, [gpk], e_g)
                            c.op("pe", [wuk] + hk, [upk], e_u)
                            sgl, sglk = sg_rot.next()
                            c.op("act", [gpk], [sglk], lambda a, gp=gp, sgl=sgl, T=T: a.activation(out=sgl[:, 0:T], in_=gp[:, 0:T], func=AF.Silu))
                            c.op("dve", [upk, sglk], [(atk, fc)],
                                 lambda v, up=up, sgl=sgl, at=at, fc=fc, T=T: v.tensor_tensor(out=at[:, fc, 0:T], in0=up[:, 0:T], in1=sgl[:, 0:T], op=ALU.mult))
                        for si in range(n):
                            t = t0 + si
                            for half in range(2):
                                yp, ypk = yps.next()

                                def e_y(pe, yp=yp, at=at, si=si, half=half, wd=wd):
                                    for fc in range(4):
                                        ins = pe.matmul(yp[:, :], at[:, fc, si * 128:(si + 1) * 128], wd[:, fc, half * 512:(half + 1) * 512], start=(fc == 0), stop=(fc == 3))
                                    return ins
                                c.op("pe", [(atk, fc) for fc in range(4)] + [wdk], [ypk], e_y)
                                av = acc[:, t, half * 512:(half + 1) * 512]
                                if e == 0:
                                    c.op("dve", [ypk, ("gates", t)], [("acc", t, half)],
                                         lambda v, yp=yp, av=av, t=t, e=e: v.tensor_scalar_mul(out=av, in0=yp[:, :], scalar1=gates[:, t, e:e + 1]))
                                else:
                                    c.op("dve", [ypk, ("gates", t)], [("acc", t, half)],
                                         lambda v, yp=yp, av=av, t=t, e=e: v.scalar_tensor_tensor(out=av, in0=yp[:, :], scalar=gates[:, t, e:e + 1], in1=av, op0=ALU.mult, op1=ALU.add))
            with c.phase(f"ln2_{l}_{b}") as ph:
                gb2 = ph.sbuf("gb2", [128, 2, D], F32)
                g2l = ph.sbuf("g2l", [128, D], F32)
                g2c = ph.sbuf("g2c", [128, D], F32)
                c.dma("sp", [gb2[:, 0, :], gb2[:, 1, :]],
                      [ln_ffn_g[l].partition_broadcast(128), ln_ffn_b[l].partition_broadcast(128)], [], ["gb2"])
                c.dma("sp", g2l[:], gsc[l, b, 1], [], ["g2l"])
                c.dma("sp", g2c[:], gsc[l, 2, 1], [], ["g2c"])
                rots = {"stats": Rot(ph, "st", 3, [128, 16], F32), "xn": Rot(ph, "xn", 2, [128, D], F32)}
                xf_rot = Rot(ph, "xf", 3, [128, D], F32)
                x2_rot = Rot(ph, "x2", 2, [128, D], F32)
                lds = {}

                def load2(t):
                    xf, xfk = xf_rot.next()
                    c.dma("sp", xf[:], xb[b, t * 128:(t + 1) * 128, :], [], [xfk])
                    lds[t] = (xf, xfk)
                for t in range(min(2, ntile)):
                    load2(t)
                for t in range(ntile):
                    g2 = g2l if t < NTL else g2c
                    g2k = "g2l" if t < NTL else "g2c"
                    xf, xfk = lds.pop(t)
                    rk = ("accr", t)
                    c.op("dve", [g2k], [rk], lambda v, t=t, g2=g2: v.tensor_tensor(out=acc[:, t, :], in0=acc[:, t, :], in1=g2[:], op=ALU.mult))
                    c.op("dve", [xfk], [rk], lambda g, t=t, xf=xf: g.scalar_tensor_tensor(out=acc[:, t, :], in0=xf[:], scalar=ALPHA, in1=acc[:, t, :], op0=ALU.mult, op1=ALU.add))
                    if t + 2 < ntile:
                        load2(t + 2)
                    x2, x2k = x2_rot.next()
                    layer_norm(rots, acc[:, t, :], rk, gb2, "gb2", x2, x2k)
                    if last:
                        c.dma("pool", out[b, t * 128:(t + 1) * 128, :], x2[:], [x2k], [("out", b, t)], is_output=True)
                    else:
                        c.dma("pool", xa[b, t * 128:(t + 1) * 128, :], x2[:], [x2k], [("xa", b, t)], is_output=debug)

    ada_prologue()
    done = stop_after is not None and stop_after[0] == "ada"
    if done:
        n_layers = 0
    for l in range(n_layers):
        last = (l == DEPTH - 1)
        for b in range(NB):
            if l % 2 == 0:
                phase_attn_A(l, b, last)
            else:
                for hh in range(2):
                    phase_attn_B(l, b, last, hh)
            if stop_after is not None and stop_after[0] in ("attn", "qkv") and stop_after[1:] == (l, b):
                done = True
                break
            phase_oproj(l, b, last)
            if stop_after == ("oproj", l, b):
                done = True
                break
            phase_moe(l, b, last)
            if stop_after == ("moe", l, b):
                done = True
                break
        if done:
            break
    c.finish()
    return nc


def _consts():
    ident = np.eye(128, dtype=np.float32)
    R = np.zeros((64, 64), np.float32)
    for d in range(64):
        if (d % 32) < 16:
            R[d, d + 16] = -1.0
        else:
            R[d, d - 16] = 1.0
    RT = np.zeros((128, 128), np.float32)
    RT[:64, :64] = R.T
    RT[64:, 64:] = R.T
    t = np.arange(S)
    rows = (t // 64).astype(np.float64)
    cols = (t % 64).astype(np.float64)
    inv = 10000.0 ** (-np.arange(16, dtype=np.float64) / 16.0)
    cos = np.zeros((128, S), np.float32)
    sin = np.zeros((128, S), np.float32)
    for p in range(128):
        d = p % 64
        pos = rows if d < 32 else cols
        ang = pos * np.float64(np.float32(inv[d % 16]))
        ang = (pos.astype(np.float32) * np.float32(inv[d % 16])).astype(np.float64)
        cos[p] = np.cos(ang)
        sin[p] = np.sin(ang)
    kk = np.arange(128)[:, None]
    qq = np.arange(128)[None, :]
    mp = np.where(kk >= qq, 0.0, NEG).astype(np.float32)
    mn = np.where(kk <= qq, 0.0, NEG).astype(np.float32)
    return {"k_ident": ident, "k_rt": RT, "k_cos": cos, "k_sin": sin,
            "k_maskp": np.tile(mp, (1, 4)), "k_maskn": np.tile(mn, (1, 4))}


_NC_CACHE = {}


def make_in_maps(inputs):
    consts = _consts()
    shared = {k: np.ascontiguousarray(inputs[k], dtype=np.float32) for k in (
        "w_ada", "b_ada", "wqkv_a", "wo_a", "sink_a", "wqkv_b", "wo_b", "lambda_b", "subln_b",
        "ln_attn_g", "ln_attn_b", "ln_ffn_g", "ln_ffn_b", "w_router", "router_bias", "w_gate", "w_up", "w_down")}
    shared.update(consts)
    in_maps = []
    for i in range(NCORES):
        m = dict(shared)
        m["x"] = np.ascontiguousarray(inputs["x"][NB * i:NB * (i + 1)], dtype=np.float32)
        m["ctx"] = np.ascontiguousarray(inputs["ctx"][NB * i:NB * (i + 1)], dtype=np.float32)
        m["c3"] = np.ascontiguousarray(
            np.concatenate([inputs["c"][NB * i:NB * (i + 1)], inputs["c_ctx"][None, :]], axis=0), dtype=np.float32)
        in_maps.append(m)
    return in_maps


def kernel(**inputs):
    if "nc" not in _NC_CACHE:
        _NC_CACHE["nc"] = build()
    nc = _NC_CACHE["nc"]
    in_maps = make_in_maps(inputs)
    res = run_bass_kernel_spmd(nc, in_maps, core_ids=list(range(NCORES)))
    return np.concatenate([np.asarray(r["out"]) for r in res.results], axis=0).astype(np.float32)
```

```python
import math
from contextlib import ExitStack, contextmanager

import numpy as np
import concourse.bass as bass
import concourse.mybir as mybir
from concourse.bass_utils import run_bass_kernel_spmd

F32 = mybir.dt.float32
BF16 = mybir.dt.bfloat16
AF = mybir.ActivationFunctionType
ALU = mybir.AluOpType
AX = mybir.AxisListType

DEPTH = 4
D = 1024
S = 2048
C = 256
TOK = S + C
NT = TOK // 128
NTL = S // 128
NB = 2
NCORES = 8
E = 16
DE = 512
ALPHA = (2 * DEPTH) ** 0.25
LN_EPS = 1e-6
SUBLN_EPS = 1e-5
SCALE = 0.125
NEG = -30000.0

SEM_ROLL = 30000
SAME_ENGINE_SYNC = ("act", "dve", "pool")
import os
DBG_CUT = int(os.environ.get("DBG_CUT", "9"))
SCOPES = bool(int(os.environ.get("DBG_SCOPES", "0")))
N_DMA_SLOTS = {"sp": 16, "pool": 12}


class Ctx:
    def __init__(self, nc, same_engine_sync=SAME_ENGINE_SYNC):
        self.nc = nc
        self.es = ExitStack()
        self.eng = {"pe": nc.tensor, "act": nc.scalar, "dve": nc.vector,
                    "pool": nc.gpsimd, "sp": nc.sync}
        self.same_engine_sync = set(same_engine_sync)
        self.sem = {}
        self.cnt = {}
        self.nsem = 0
        self.uid = 0
        for e in ("pe", "act", "dve", "pool"):
            self._new_sem(e)
        self.known = {e: {} for e in self.eng}
        self.last_w = {}
        self.readers = {}
        self.dma_sems = {}
        self.dma_val = {}
        self.dma_i = {}
        for q, n in N_DMA_SLOTS.items():
            self.dma_sems[q] = [self.es.enter_context(nc.semaphore(f"dq_{q}_{i}")) for i in range(n)]
            self.dma_val[q] = [0] * n
            self.dma_i[q] = 0
        self.out_events = []
        self.ps = [self.es.enter_context(nc.psum_tensor(f"psb{i}", [128, 512], F32)) for i in range(8)]

    def _new_sem(self, e):
        self.nsem += 1
        self.sem[e] = self.es.enter_context(self.nc.semaphore(f"s_{e}_{self.nsem}"))
        self.cnt[e] = 0

    def sbuf(self, name, shape, dt):
        self.uid += 1
        return self.es.enter_context(self.nc.sbuf_tensor(f"{name}_{self.uid}", list(shape), dt))

    @contextmanager
    def phase(self, name=None):
        st = ExitStack()
        ctx = self
        if name is not None and SCOPES:
            st.enter_context(self.nc.named_scope(name))

        class PH:
            def sbuf(self_, name, shape, dt):
                ctx.uid += 1
                return st.enter_context(ctx.nc.sbuf_tensor(f"{name}_{ctx.uid}", list(shape), dt))

        try:
            yield PH()
        finally:
            self.barrier()
            st.close()

    def _deps(self, reads, writes):
        evs = []
        for k in list(reads) + list(writes):
            ev = self.last_w.get(k)
            if ev is not None:
                evs.append(ev)
        for k in writes:
            evs.extend(self.readers.get(k, ()))
        return evs

    def _emit_waits(self, engine, evs):
        best = {}
        for (sem, val, src) in evs:
            if src == engine and engine not in self.same_engine_sync:
                continue
            key = id(sem)
            if self.known[engine].get(key, 0) >= val:
                continue
            if key not in best or best[key][1] < val:
                best[key] = (sem, val)
        for key, (sem, val) in best.items():
            self.eng[engine].wait_ge(sem, val)
            self.known[engine][key] = val

    def _record(self, ev, reads, writes):
        for k in writes:
            self.last_w[k] = ev
            self.readers[k] = []
        for k in reads:
            self.readers.setdefault(k, []).append(ev)

    @staticmethod
    def _excl(reads, writes):
        r = [k for k in reads if not (isinstance(k, str) and k.startswith("ps"))]
        w = list(writes) + [k for k in reads if isinstance(k, str) and k.startswith("ps")]
        return r, w

    def op(self, engine, reads, writes, emit):
        reads, writes = self._excl(reads, writes)
        self._emit_waits(engine, self._deps(reads, writes))
        ins = emit(self.eng[engine])
        if self.cnt[engine] >= SEM_ROLL:
            self._new_sem(engine)
        self.cnt[engine] += 1
        ins.then_inc(self.sem[engine], 1)
        ev = (self.sem[engine], self.cnt[engine], engine)
        self._record(ev, reads, writes)
        return ev

    def dma(self, queue, out_ap, in_ap, reads, writes, is_output=False):
        slot = self.dma_i[queue] % len(self.dma_sems[queue])
        self.dma_i[queue] += 1
        sem = self.dma_sems[queue][slot]
        evs = self._deps(reads, writes)
        if self.dma_val[queue][slot] > 0:
            evs.append((sem, self.dma_val[queue][slot], "dma"))
        self._emit_waits(queue, evs)
        outs = out_ap if isinstance(out_ap, (list, tuple)) else [out_ap]
        ins_ = in_ap if isinstance(in_ap, (list, tuple)) else [in_ap]
        for o, i in zip(outs, ins_):
            self.eng[queue].dma_start(out=o, in_=i).then_inc(sem, 16)
            self.dma_val[queue][slot] += 16
        ev = (sem, self.dma_val[queue][slot], "dma")
        self._record(ev, reads, writes)
        if is_output:
            self.out_events.append(ev)
        return ev

    def _all_events(self):
        evs = []
        for e in ("pe", "act", "dve", "pool"):
            if self.cnt[e] > 0:
                evs.append((self.sem[e], self.cnt[e], "bar"))
        for q in self.dma_sems:
            for s, v in zip(self.dma_sems[q], self.dma_val[q]):
                if v > 0:
                    evs.append((s, v, "dma"))
        return evs

    def barrier(self):
        evs = self._all_events()
        for e in ("pe", "act", "dve", "pool", "sp"):
            self._emit_waits(e, evs)
        self.last_w.clear()
        self.readers.clear()

    def finish(self):
        self._emit_waits("sp", list(self.out_events) + self._all_events())
        self.es.close()


class Rot:
    def __init__(self, alloc, name, n, shape, dt):
        self.tiles = [alloc.sbuf(f"{name}{i}", shape, dt) for i in range(n)]
        self.keys = [f"{name}#{id(self)}#{i}" for i in range(n)]
        self.i = -1

    def next(self):
        self.i = (self.i + 1) % len(self.tiles)
        return self.tiles[self.i], self.keys[self.i]


class PsRot:
    def __init__(self, c, banks):
        self.c = c
        self.banks = list(banks)
        self.i = -1

    def next(self):
        self.i = (self.i + 1) % len(self.banks)
        b = self.banks[self.i]
        return self.c.ps[b], f"ps{b}"


def build(n_layers=DEPTH, debug=False, stop_after=None):
    nc = bass.Bass("TRN2", target_bir_lowering=False)

    def din(name, shape, dt=F32):
        return nc.dram_tensor(name, list(shape), dt, kind="ExternalInput").ap()

    x = din("x", [NB, S, D])
    ctxin = din("ctx", [NB, C, D])
    c3 = din("c3", [3, D])
    w_ada = din("w_ada", [DEPTH, D, 6 * D])
    b_ada = din("b_ada", [DEPTH, 6 * D])
    wqkv_a = din("wqkv_a", [2, D, 1536])
    wo_a = din("wo_a", [2, D, D])
    sink_a = din("sink_a", [2, 16])
    wqkv_b = din("wqkv_b", [2, D, 3072])
    wo_b = din("wo_b", [2, D, D])
    lambda_b = din("lambda_b", [2, 4, 64])
    subln_b = din("subln_b", [2, 128])
    ln_attn_g = din("ln_attn_g", [DEPTH, D])
    ln_attn_b = din("ln_attn_b", [DEPTH, D])
    ln_ffn_g = din("ln_ffn_g", [DEPTH, D])
    ln_ffn_b = din("ln_ffn_b", [DEPTH, D])
    w_router = din("w_router", [D, E])
    router_bias = din("router_bias", [E])
    w_gate = din("w_gate", [DEPTH, E, D, DE])
    w_up = din("w_up", [DEPTH, E, D, DE])
    w_down = din("w_down", [DEPTH, E, DE, D])
    k_ident = din("k_ident", [128, 128])
    k_rt = din("k_rt", [128, 128])
    k_cos = din("k_cos", [128, S])
    k_sin = din("k_sin", [128, S])
    k_maskp = din("k_maskp", [128, 512])
    k_maskn = din("k_maskn", [128, 512])

    out = nc.dram_tensor("out", [NB, S, D], F32, kind="ExternalOutput").ap()
    skind = "ExternalOutput" if debug else "Internal"
    xa = nc.dram_tensor("xa", [NB, TOK, D], F32, kind=skind).ap()
    xb = nc.dram_tensor("xb", [NB, TOK, D], F32, kind=skind).ap()
    osc = nc.dram_tensor("osc", [NB, TOK, D], BF16, kind="Internal").ap()
    gsc = nc.dram_tensor("gsc", [DEPTH, 3, 2, 128, D], F32, kind="Internal").ap()
    if debug:
        dbg_gates = nc.dram_tensor("dbg_gates", [NB, TOK, E], F32, kind="ExternalOutput").ap()
        dbg_mod = nc.dram_tensor("dbg_mod", [128, DEPTH * 4 * 8 * 3], F32, kind="ExternalOutput").ap()
        dbg_o = nc.dram_tensor("dbg_o", [NB, TOK, D], F32, kind="ExternalOutput").ap()

    c = Ctx(nc)
    ps = c.ps

    identf = c.sbuf("identf", [128, 128], F32)
    identb = c.sbuf("identb", [128, 128], BF16)
    rtb = c.sbuf("rtb", [128, 128], BF16)
    modS = c.sbuf("modS", [128, DEPTH * 4 * 8 * 3], F32)
    h2T = c.sbuf("h2T", [128, 8, TOK], BF16)
    gates = c.sbuf("gates", [128, NT, E], F32)
    wrb = c.sbuf("wrb", [128, 8, E], BF16)
    rbias = c.sbuf("rbias", [128, E], F32)

    def mod_ap(l, kind, j, cls):
        o = ((l * 4 + kind) * 8 + j) * 3 + cls
        return modS[:, o:o + 1]

    c.dma("sp", identf[:], k_ident[:, :], [], ["identf"])
    c.dma("pool", identb[:], k_ident[:, :], [], ["identb"])
    c.dma("pool", rtb[:], k_rt[:, :], [], ["rtb"])
    c.dma("pool", wrb[:], w_router.rearrange("(k p) e -> p k e", p=128), [], ["wrb"])
    c.dma("sp", rbias[:], router_bias.partition_broadcast(128), [], ["rbias"])

    def ada_prologue():
        with c.phase("ada") as ph:
            c3s = ph.sbuf("c3s", [3, D], F32)
            silT = ph.sbuf("silT", [128, 8, 3], F32)
            silbc = [ph.sbuf(f"silbc{i}", [128, 8, 128], BF16) for i in range(3)]
            lnrows = ph.sbuf("lnrows", [64, 128], F32)
            lnT = ph.sbuf("lnT", [128, 64], F32)
            c.dma("sp", c3s[:], c3[:, :], [], ["c3s"])
            c.dma("sp", [lnrows[0:32, :], lnrows[32:64, :]],
                  [ln_attn_g.rearrange("l (j p) -> (l j) p", p=128),
                   ln_attn_b.rearrange("l (j p) -> (l j) p", p=128)], [], ["lnrows"])

            def e_ct(pe):
                for k in range(8):
                    ins = pe.matmul(ps[0][:, k * 3:(k + 1) * 3], c3s[0:3, k * 128:(k + 1) * 128],
                                    identf[0:3, 0:3], start=(k == 0), stop=True, skip_group_check=True)
                return ins
            c.op("pe", ["c3s", "identf"], ["ps0"], e_ct)
            c.op("act", ["ps0"], ["silT"],
                 lambda a: a.activation(out=silT[:].rearrange("p k c -> p (k c)"), in_=ps[0][:, 0:24], func=AF.Silu))
            for cls in range(3):
                c.op("dve", ["silT"], [f"silbc{cls}"],
                     lambda v, cls=cls: v.tensor_copy(out=silbc[cls][:],
                                                      in_=silT[:, :, cls:cls + 1].to_broadcast([128, 8, 128])))
            c.op("pe", ["lnrows", "identf"], ["ps1"],
                 lambda pe: pe.matmul(ps[1][:, 0:64], lnrows[0:64, :], identf[0:64, 0:64], start=True, stop=True))
            c.op("dve", ["ps1"], ["lnT"], lambda v: v.tensor_copy(out=lnT[:], in_=ps[1][:, 0:64]))

            wa_rot = Rot(ph, "wa", 2, [128, 8, 512], F32)
            wab_rot = Rot(ph, "wab", 2, [128, 8, 512], BF16)
            gst_rot = Rot(ph, "gst", 2, [128, 512], F32)
            gps = PsRot(c, [4, 5, 6])
            bt = ph.sbuf("bt", [48, 128], F32)
            bT = ph.sbuf("bT", [128, 48], F32)
            gbias = ph.sbuf("gbias", [128, 2, D], F32)
            mraw = ph.sbuf("mraw", [128, 32, 3], F32)
            tmp83 = ph.sbuf("tmp83", [128, 8, 3], F32)
            for l in range(n_layers):
                c.dma("sp", bt[:], b_ada[l].rearrange("(j p) -> j p", p=128), [], ["bt"])
                c.dma("sp", [gbias[:, 0, :], gbias[:, 1, :]],
                      [b_ada[l, 2048:3072].partition_broadcast(128),
                       b_ada[l, 5120:6144].partition_broadcast(128)], [], ["gbias"])
                c.op("pe", ["bt", "identf"], ["ps2"],
                     lambda pe: pe.matmul(ps[2][:, 0:48], bt[0:48, :], identf[0:48, 0:48], start=True, stop=True))
                c.op("act", ["ps2"], ["bT"], lambda a: a.copy(out=bT[:], in_=ps[2][:, 0:48]))
                first3 = True
                for cc in range(12):
                    kind = cc // 2
                    half = cc % 2
                    if kind in (2, 5):
                        wa, wak = wab_rot.next()
                        c.dma("pool", wa[:], w_ada[l][:, cc * 512:(cc + 1) * 512].rearrange("(k p) f -> p k f", p=128),
                              [], [wak])
                    else:
                        wa, wak = wa_rot.next()
                        c.dma("sp", wa[:], w_ada[l][:, cc * 512:(cc + 1) * 512].rearrange("(k p) f -> p k f", p=128),
                              [], [wak])
                    if kind in (2, 5):
                        which = 0 if kind == 2 else 1
                        for cls in range(3):
                            pst, psk = gps.next()

                            def e_g(pe, pst=pst, cls=cls, wa=wa):
                                for k in range(8):
                                    ins = pe.matmul(pst[:, :], silbc[cls][:, k, :], wa[:, k, :],
                                                    start=(k == 0), stop=(k == 7))
                                return ins
                            c.op("pe", [wak, f"silbc{cls}"], [psk], e_g)
                            gst, gstk = gst_rot.next()
                            c.op("dve", [psk, "gbias"], [gstk],
                                 lambda v, pst=pst, gst=gst, which=which, half=half: v.tensor_tensor(
                                     out=gst[:], in0=pst[:, :], in1=gbias[:, which, half * 512:(half + 1) * 512],
                                     op=ALU.add))
                            c.dma("sp", gsc[l, cls, which][:, half * 512:(half + 1) * 512], gst[:],
                                  [gstk], [("gsc", l, cls, which, half)])
                    else:
                        ks = {1: 0, 0: 1, 4: 2, 3: 3}[kind]

                        def e_m(pe, wa=wa, ks=ks, half=half, first=first3):
                            for fs in range(4):
                                slot = ks * 8 + half * 4 + fs
                                for k in range(8):
                                    ins = pe.matmul(ps[3][:, slot * 3:(slot + 1) * 3],
                                                    wa[:, k, fs * 128:(fs + 1) * 128], silT[:, k, :],
                                                    start=(first and fs == 0 and k == 0), stop=(k == 7),
                                                    skip_group_check=True)
                            return ins
                        c.op("pe", [wak, "silT"], ["ps3"], e_m)
                        first3 = False
                ps3v = ps[3][:, 0:96].rearrange("p (s c) -> p s c", c=3)
                for ks, a0 in ((0, 8), (1, 0), (2, 32), (3, 24)):
                    c.op("dve", ["ps3", "bT"], ["mraw"],
                         lambda v, ks=ks, a0=a0: v.tensor_tensor(
                             out=mraw[:, ks * 8:(ks + 1) * 8, :], in0=ps3v[:, ks * 8:(ks + 1) * 8, :],
                             in1=bT[:, a0:a0 + 8].unsqueeze(2).to_broadcast([128, 8, 3]), op=ALU.add))
                base = l * 96
                mv = modS[:, base:base + 96].rearrange("p (s c) -> p s c", c=3)
                lng = lnT[:, l * 8:(l + 1) * 8].unsqueeze(2).to_broadcast([128, 8, 3])
                lnb = lnT[:, 32 + l * 8:32 + (l + 1) * 8].unsqueeze(2).to_broadcast([128, 8, 3])
                c.op("dve", ["mraw"], ["modS"], lambda v: v.tensor_scalar_add(out=mv[:, 0:8, :], in0=mraw[:, 0:8, :], scalar1=1.0))
                c.op("dve", ["mraw"], ["modS"], lambda v: v.tensor_copy(out=mv[:, 8:16, :], in_=mraw[:, 8:16, :]))
                c.op("dve", ["mraw"], ["tmp83"], lambda v: v.tensor_scalar_add(out=tmp83[:], in0=mraw[:, 16:24, :], scalar1=1.0))
                c.op("dve", ["tmp83", "lnT"], ["modS"], lambda v: v.tensor_tensor(out=mv[:, 16:24, :], in0=tmp83[:], in1=lng, op=ALU.mult))
                c.op("dve", ["tmp83", "lnT"], ["tmp83"], lambda v: v.tensor_tensor(out=tmp83[:], in0=tmp83[:], in1=lnb, op=ALU.mult))
                c.op("dve", ["tmp83", "mraw"], ["modS"], lambda v: v.tensor_tensor(out=mv[:, 24:32, :], in0=tmp83[:], in1=mraw[:, 24:32, :], op=ALU.add))
            if debug:
                c.dma("sp", dbg_mod[:, :], modS[:], ["modS"], ["dbg_mod"], is_output=True)

    def xin_ap(l, b, t):
        if l == 0:
            if t < NTL:
                return x[b, t * 128:(t + 1) * 128, :], ("x", b, t)
            return ctxin[b, (t - NTL) * 128:(t - NTL + 1) * 128, :], ("ctx", b, t)
        return xa[b, t * 128:(t + 1) * 128, :], ("xa", b, t)

    def cls_of(b, t):
        return b if t < NTL else 2

    def make_hT(ph_rots, l, b, tiles, kind_s, kind_b, dst, dst_key, dst_col0, src_loader):
        tps = ph_rots["tps"]
        n = len(tiles)
        srcs = [src_loader(t) for t in tiles]
        for j in range(8):
            pst, psk = tps.next()
            pv = pst[:].bitcast(BF16)

            def e_t(pe, j=j, pv=pv):
                for i in range(n):
                    ins = pe.transpose(out=pv[:, i * 128:(i + 1) * 128], in_=srcs[i][0][:, j * 128:(j + 1) * 128],
                                       identity=identb[:])
                return ins
            c.op("pe", [s[1] for s in srcs] + ["identb"], [psk], e_t)
            cls = cls_of(b, tiles[0])
            c.op("act", [psk, "modS"], [dst_key],
                 lambda a, j=j, pv=pv, cls=cls: a.activation(
                     out=dst[:, j, dst_col0:dst_col0 + n * 128], in_=pv[:, 0:n * 128], func=AF.Identity,
                     bias=mod_ap(l, kind_b, j, cls), scale=mod_ap(l, kind_s, j, cls)))

    def ln_stats(ph_rots, r, rk):
        st, stk = ph_rots["stats"].next()
        c.op("dve", [rk], [stk], lambda v: v.bn_stats(out=st[:, 0:6], in_=r[:, 0:512]))
        c.op("dve", [rk], [stk], lambda v: v.bn_stats(out=st[:, 6:12], in_=r[:, 512:1024]))
        c.op("dve", [stk], [stk], lambda v: v.bn_aggr(out=st[:, 12:14], in_=st[:, 0:12]))
        c.op("dve", [stk], [stk], lambda v: v.tensor_scalar_add(out=st[:, 14:15], in0=st[:, 13:14], scalar1=LN_EPS))
        return st, stk

    def ln_apply(ph_rots, stt, r, rk, gb, gbk, xo, xok, xnb=None, xnbk=None):
        st, stk = stt
        xn, xnk = ph_rots["xn"].next()
        c.op("act", [stk], [stk], lambda a: a.activation(out=st[:, 14:15], in_=st[:, 14:15], func=AF.Ln))
        c.op("act", [stk], [stk], lambda a: a.activation(out=st[:, 14:15], in_=st[:, 14:15], func=AF.Exp, scale=-0.5))
        c.op("dve", [stk], [stk], lambda v: v.scalar_tensor_tensor(out=st[:, 15:16], in0=st[:, 12:13], scalar=-1.0,
                                                                   in1=st[:, 14:15], op0=ALU.mult, op1=ALU.mult))
        c.op("act", [rk, stk], [xnk], lambda a: a.activation(out=xn[:], in_=r[:], func=AF.Identity,
                                                             bias=st[:, 15:16], scale=st[:, 14:15]))
        if xnb is not None:
            c.op("act", [rk, stk], [xnbk], lambda a: a.activation(out=xnb[:], in_=r[:], func=AF.Identity,
                                                                  bias=st[:, 15:16], scale=st[:, 14:15]))
        c.op("pool", [xnk, gbk], [xnk], lambda g: g.tensor_tensor(out=xn[:], in0=xn[:], in1=gb[:, 0, :], op=ALU.mult))
        c.op("pool", [xnk, gbk], [xok], lambda g: g.tensor_tensor(out=xo[:], in0=xn[:], in1=gb[:, 1, :], op=ALU.add))

    def layer_norm(ph_rots, r, rk, gb, gbk, xo, xok, xnb=None, xnbk=None):
        stt = ln_stats(ph_rots, r, rk)
        ln_apply(ph_rots, stt, r, rk, gb, gbk, xo, xok, xnb, xnbk)

    def qkv_project(ph, rots, l, b, last, wq, wqk, nq, wk, wkk, nk, wv, wvk, vcols, QT, KT, VA, cosT, sinT):
        xf_rot, xb_rot, hT_rot, raw_rot, t1_rot, t2_rot = (rots[k] for k in ("xf", "xbf", "hT", "raw", "t1", "t2"))
        mm = rots["mm"]
        vheads = VA.shape[2]
        vd = VA.shape[3] - 1
        groups = [list(range(g * 4, g * 4 + 4)) for g in range(4)] + [[16, 17]]
        for tiles in groups:
            is_ctx = tiles[0] >= NTL
            T = len(tiles) * 128
            col0 = tiles[0] * 128
            loaded = {}

            def loader(t):
                if t in loaded:
                    return loaded[t]
                xf, xfk = xf_rot.next()
                ap, dk = xin_ap(l, b, t)
                c.dma("sp", xf[:], ap, [dk], [xfk])
                xbt, xbk = xb_rot.next()
                c.op("pool", [xfk], [xbk], lambda g, xf=xf, xbt=xbt: g.tensor_copy(out=xbt[:], in_=xf[:]))
                loaded[t] = (xbt, xbk)
                return loaded[t]
            hT, hTk = hT_rot.next()
            make_hT(rots, l, b, tiles, 0, 1, hT, hTk, 0, loader)
            jobs = []
            if not (is_ctx and last):
                jobs += [("q", i) for i in range(nq)]
            jobs += [("k", i) for i in range(nk)]
            def emit_proj(kind, i):
                w, wkey = (wq, wqk) if kind == "q" else (wk, wkk)
                dstT, dkey = (QT, "QT") if kind == "q" else (KT, "KT")
                pst, psk = mm.next()

                def e_p(pe):
                    for k in range(8):
                        ins = pe.matmul(pst[:, 0:T], w[:, k, i * 128:(i + 1) * 128], hT[:, k, 0:T],
                                        start=(k == 0), stop=(k == 7))
                    return ins
                c.op("pe", [wkey, hTk], [psk], e_p)
                if is_ctx:
                    c.op("act", [psk], [(dkey, i, tiles[0])],
                         lambda a: a.copy(out=dstT[:, i, col0:col0 + T], in_=pst[:, 0:T]))
                    return None
                raw, rawk = raw_rot.next()
                c.op("act", [psk], [rawk], lambda a: a.copy(out=raw[:, 0:T], in_=pst[:, 0:T]))
                return (dstT, dkey, i, raw, rawk)

            def emit_rope(st):
                dstT, dkey, i, raw, rawk = st
                pst2, psk2 = mm.next()
                c.op("pe", [rawk, "rtb"], [psk2],
                     lambda pe: pe.matmul(pst2[:, 0:T], rtb[:], raw[:, 0:T], start=True, stop=True))
                t1, t1k = t1_rot.next()
                t2, t2k = t2_rot.next()
                c.op("dve", [rawk, "cos"], [t1k],
                     lambda g: g.tensor_tensor(out=t1[:, 0:T], in0=raw[:, 0:T], in1=cosT[:, col0:col0 + T], op=ALU.mult))
                c.op("dve", [psk2, "sin"], [t2k],
                     lambda v: v.tensor_tensor(out=t2[:, 0:T], in0=pst2[:, 0:T], in1=sinT[:, col0:col0 + T], op=ALU.mult))
                c.op("pool", [t1k, t2k], [(dkey, i, tiles[0])],
                     lambda g: g.tensor_tensor(out=dstT[:, i, col0:col0 + T], in0=t1[:, 0:T], in1=t2[:, 0:T], op=ALU.add))

            prev = None
            for job in jobs + [None]:
                cur = emit_proj(*job) if job is not None else None
                if prev is not None:
                    emit_rope(prev)
                prev = cur
            for si, t in enumerate(tiles):
                pst, psk = mm.next()

                def e_v(pe, pst=pst, hT=hT, si=si):
                    for k in range(8):
                        ins = pe.matmul(pst[:, 0:vcols], hT[:, k, si * 128:(si + 1) * 128], wv[:, k, 0:vcols],
                                        start=(k == 0), stop=(k == 7))
                    return ins
                c.op("pe", [wvk, hTk], [psk], e_v)
                c.op("act", [psk], [("VA", t)],
                     lambda a, pst=pst, t=t: a.copy(out=VA[:, t, :, 0:vd],
                                                    in_=pst[:, 0:vcols].rearrange("p (h d) -> p h d", d=vd)))

    def std_rots(ph, raw_cols=512):
        return {
            "xf": Rot(ph, "xf", 3, [128, D], F32),
            "xbf": Rot(ph, "xbf", 5, [128, D], BF16),
            "hT": Rot(ph, "hT", 2, [128, 8, 512], BF16),
            "raw": Rot(ph, "raw", 3, [128, 512], BF16),
            "t1": Rot(ph, "t1", 2, [128, 512], F32),
            "t2": Rot(ph, "t2", 2, [128, 512], F32),
            "tps": PsRot(c, [0, 1]),
            "mm": PsRot(c, [2, 3, 4, 5]),
        }

    def phase_attn_A(l, b, last):
        jj = l // 2
        with c.phase(f"attnA_{l}_{b}") as ph:
            rots = std_rots(ph)
            wq = ph.sbuf("wq", [128, 8, 1024], BF16)
            wk = ph.sbuf("wk", [128, 8, 4, 2, 64], BF16)
            wv = ph.sbuf("wv", [128, 8, 256], BF16)
            cosT = ph.sbuf("cosT", [128, S], F32)
            sinT = ph.sbuf("sinT", [128, S], F32)
            maskp = ph.sbuf("maskp", [128, 512], BF16)
            maskn = ph.sbuf("maskn", [128, 512], BF16)
            esink = ph.sbuf("esink", [128, 16], F32)
            QT = ph.sbuf("QT", [128, 8, TOK], BF16)
            KT = ph.sbuf("KT", [128, 4, TOK], BF16)
            VA = ph.sbuf("VA", [128, NT, 4, 65], BF16)
            wsrc = wqkv_a[jj]
            for k in range(8):
                c.dma("pool", wq[:, k, :], wsrc[k * 128:(k + 1) * 128, 0:1024], [], ["wq"])
            ksrc = wsrc[:, 1024:1280].rearrange("(k p) (g d) -> p k g d", p=128, d=64)
            for k in range(8):
                c.dma("pool", [wk[:, k, :, 0, :], wk[:, k, :, 1, :]], [ksrc[:, k], ksrc[:, k]], [], ["wk"])
            c.dma("pool", wv[:], wsrc[:, 1280:1536].rearrange("(k p) f -> p k f", p=128), [], ["wv"])
            c.dma("sp", cosT[:], k_cos[:, :], [], ["cos"])
            c.dma("sp", sinT[:], k_sin[:, :], [], ["sin"])
            c.dma("pool", maskp[:], k_maskp[:, :], [], ["maskp"])
            c.dma("pool", maskn[:], k_maskn[:, :], [], ["maskn"])
            c.dma("sp", esink[:], sink_a[jj].partition_broadcast(128), [], ["esink"])
            c.op("act", ["esink"], ["esink"], lambda a: a.activation(out=esink[:], in_=esink[:], func=AF.Exp))
            c.op("pool", [], [("VA", t) for t in range(NT)], lambda g: g.memset(VA[:, :, :, 64:65], 1.0))
            wkv = wk[:].rearrange("p k g r d -> p k (g r d)")
            qkv_project(ph, rots, l, b, last, wq, "wq", 8, wkv, "wk", 4, wv, "wv", 256, QT, KT, VA, cosT, sinT)

            if stop_after is not None and stop_after[0] == "qkv":
                return
            spsx = PsRot(c, [0, 2, 6])
            spsy = PsRot(c, [1, 3, 7])
            ops_ = PsRot(c, [4, 5])
            pT_rot = Rot(ph, "pT", 4, [128, 512], BF16)
            ot_rot = Rot(ph, "ot", 2, [128, 16, 64], BF16)
            dn_rot = Rot(ph, "dn", 2, [128, 8], F32)
            qtiles = list(range(NTL)) + ([] if last else [16, 17])
            pcol = {0: 0, 2: 128, 1: 256, 3: 384}
            LA = 2
            its = []
            for qt in qtiles:
                if qt < NTL:
                    kts = ([(qt - 1, maskp, "maskp")] if qt > 0 else []) + [(qt, None, None)] + \
                          ([(qt + 1, maskn, "maskn")] if qt < NTL - 1 else []) + [(16, None, None), (17, None, None)]
                else:
                    kts = [(16, None, None), (17, None, None)]
                for g in range(4):
                    for ki, (kt, msk, mskk) in enumerate(kts):
                        its.append((qt, g, ki, len(kts), kt, msk, mskk))
            state = {}

            def emit_s(it):
                qt, g, ki, nk, kt, msk, mskk = it
                sx, sxk = spsx.next()
                sy, syk = spsy.next()

                def e_s(pe):
                    if msk is not None:
                        pe.matmul(sx[:, 0:256], identb[:], msk[:, 0:256], start=True, stop=False, skip_group_check=True)
                        pe.matmul(sy[:, 0:256], identb[:], msk[:, 0:256], start=True, stop=False, skip_group_check=True)
                    for hd in range(4):
                        cch = 2 * g + hd // 2
                        base = 64 * (hd % 2)
                        dst = sx if hd % 2 == 0 else sy
                        o = 128 * (hd // 2)
                        ins = pe.matmul(dst[:, o:o + 128],
                                        KT[base:base + 64, g, kt * 128:(kt + 1) * 128],
                                        QT[base:base + 64, cch, qt * 128:(qt + 1) * 128],
                                        start=(msk is None and hd < 2), stop=(hd >= 2), skip_group_check=True)
                    return ins
                rk = [("KT", g, (kt // 4) * 4 if kt < 16 else 16), ("QT", 2 * g, (qt // 4) * 4 if qt < 16 else 16),
                      ("QT", 2 * g + 1, (qt // 4) * 4 if qt < 16 else 16), "identb"] + ([mskk] if mskk else [])
                c.op("pe", rk, [sxk, syk], e_s)
                pT, pTk = pT_rot.next()
                c.op("act", [sxk], [(pTk, 0)],
                     lambda a: a.activation(out=pT[:, 0:256], in_=sx[:, 0:256], func=AF.Exp, scale=SCALE))
                c.op("act", [syk], [(pTk, 1)],
                     lambda a: a.activation(out=pT[:, 256:512], in_=sy[:, 0:256], func=AF.Exp, scale=SCALE))
                return (pT, pTk)

            def emit_pv(it, pt):
                qt, g, ki, nk, kt, msk, mskk = it
                pT, pTk = pt
                if g == 0 and ki == 0:
                    state["ot"] = ot_rot.next()
                if ki == 0:
                    state["op"] = ops_.next()
                ot, otk = state["ot"]
                opst, opsk = state["op"]

                def e_o(pe):
                    for hd in range(4):
                        ins = pe.matmul(opst[:, hd * 128:hd * 128 + 65], pT[:, pcol[hd]:pcol[hd] + 128],
                                        VA[:, kt, g, :], start=(ki == 0 and hd == 0), stop=(ki == nk - 1),
                                        skip_group_check=True)
                    return ins
                c.op("pe", [(pTk, 0), (pTk, 1), ("VA", kt)], [opsk], e_o)
                if ki != nk - 1:
                    return
                dn, dnk = dn_rot.next()
                ov = opst[:, :].rearrange("p (h d) -> p h d", d=128)
                c.op("dve", [opsk, "esink"], [dnk],
                     lambda v: v.tensor_tensor(out=dn[:, 0:4], in0=ov[:, :, 64], in1=esink[:, 4 * g:4 * g + 4], op=ALU.add))
                c.op("dve", [dnk], [dnk], lambda v: v.reciprocal(out=dn[:, 4:8], in_=dn[:, 0:4]))
                c.op("dve", [opsk, dnk], [otk],
                     lambda v: v.tensor_tensor(
                         out=ot[:, 4 * g:4 * g + 4, :], in0=ov[:, :, 0:64],
                         in1=dn[:, 4:8].unsqueeze(2).to_broadcast([128, 4, 64]), op=ALU.mult))
                if g == 3:
                    c.dma("pool", osc[b, qt * 128:(qt + 1) * 128, :], ot[:].rearrange("p h d -> p (h d)"), [otk], [("osc", b, qt)])

            pend = []
            for idx in range(len(its) + LA):
                if idx < len(its):
                    pend.append(emit_s(its[idx]))
                if idx - LA >= 0:
                    emit_pv(its[idx - LA], pend[idx - LA])

    def phase_attn_B(l, b, last, hh):
        jj = l // 2
        lam_init = 0.8 - 0.6 * math.exp(-0.3 * l)
        with c.phase(f"attnB_{l}_{b}_{hh}") as ph:
            rots = std_rots(ph)
            wq = ph.sbuf("wq", [128, 8, 512], BF16)
            wk = ph.sbuf("wk", [128, 8, 512], BF16)
            wv = ph.sbuf("wv", [128, 8, 512], BF16)
            cosT = ph.sbuf("cosT", [128, S], F32)
            sinT = ph.sbuf("sinT", [128, S], F32)
            QT = ph.sbuf("QT", [128, 4, TOK], BF16)
            KT = ph.sbuf("KT", [128, 4, TOK], BF16)
            VA = ph.sbuf("VA", [128, NT, 4, 129], BF16)
            lamt = ph.sbuf("lamt", [128, 4, 64], F32)
            lsm = ph.sbuf("lsm", [128, 8], F32)
            gsub = ph.sbuf("gsub", [128, 128], F32)
            wsrc = wqkv_b[jj]
            for wt, key, c0 in ((wq, "wq", hh * 512), (wk, "wk", 1024 + hh * 512), (wv, "wv", 2048 + hh * 512)):
                for k in range(8):
                    c.dma("pool", wt[:, k, :], wsrc[k * 128:(k + 1) * 128, c0:c0 + 512], [], [key])
            c.dma("sp", cosT[:], k_cos[:, :], [], ["cos"])
            c.dma("sp", sinT[:], k_sin[:, :], [], ["sin"])
            c.dma("sp", lamt[:].rearrange("p a d -> p (a d)"),
                  lambda_b[jj].rearrange("a d -> (a d)").partition_broadcast(128), [], ["lamt"])
            c.dma("sp", gsub[:], subln_b[jj].partition_broadcast(128), [], ["gsub"])
            c.op("dve", ["lamt"], ["lamt"], lambda v: v.tensor_tensor(out=lamt[:, 0, :], in0=lamt[:, 0, :], in1=lamt[:, 1, :], op=ALU.mult))
            c.op("dve", ["lamt"], ["lamt"], lambda v: v.tensor_tensor(out=lamt[:, 2, :], in0=lamt[:, 2, :], in1=lamt[:, 3, :], op=ALU.mult))
            c.op("dve", ["lamt"], ["lsm"], lambda v: v.reduce_sum(out=lsm[:, 0:1], in_=lamt[:, 0, :], axis=AX.X))
            c.op("dve", ["lamt"], ["lsm"], lambda v: v.reduce_sum(out=lsm[:, 1:2], in_=lamt[:, 2, :], axis=AX.X))
            c.op("act", ["lsm"], ["lsm"], lambda a: a.activation(out=lsm[:, 2:4], in_=lsm[:, 0:2], func=AF.Exp))
            c.op("dve", ["lsm"], ["lsm"], lambda v: v.tensor_tensor(out=lsm[:, 4:5], in0=lsm[:, 3:4], in1=lsm[:, 2:3], op=ALU.subtract))
            c.op("dve", ["lsm"], ["lsm"], lambda v: v.tensor_scalar_add(out=lsm[:, 4:5], in0=lsm[:, 4:5], scalar1=-lam_init))
            c.op("dve", ["gsub"], ["gsub"], lambda v: v.tensor_scalar_mul(out=gsub[:], in0=gsub[:], scalar1=(1.0 - lam_init)))
            c.op("pool", [], [("VA", t) for t in range(NT)], lambda g: g.memset(VA[:, :, :, 128:129], 1.0))
            qkv_project(ph, rots, l, b, last, wq, "wq", 4, wk, "wk", 4, wv, "wv", 512, QT, KT, VA, cosT, sinT)

            spsx = PsRot(c, [0, 2])
            spsy = PsRot(c, [1, 3])
            pT_rot = Rot(ph, "pT", 4, [128, 2, 512], BF16)
            ot_rot = Rot(ph, "ot", 2, [128, 4, 512], BF16)
            sm_rot = Rot(ph, "sm", 2, [128, 16], F32)
            of_rot = Rot(ph, "of", 2, [128, 4, 128], F32)
            jk_rot = Rot(ph, "jk", 2, [128, 128], F32)
            qgroups = [list(range(g * 4, g * 4 + 4)) for g in range(4)] + ([] if last else [[16, 17]])
            LA = 2
            for qg in qgroups:
                nq = len(qg)
                Tq = nq * 128
                q0 = qg[0] * 128
                kts = list(range(NT)) if qg[0] < NTL else [16, 17]
                nk = len(kts)
                ot, otk = ot_rot.next()
                gq = qg[0]
                its = [(h, ki, kt) for h in range(4) for ki, kt in enumerate(kts)]
                pend = []

                def emit_s(it):
                    h, ki, kt = it
                    sx, sxk = spsx.next()
                    sy, syk = spsy.next()

                    def e_s(pe):
                        pe.matmul(sx[:, 0:Tq], KT[0:64, h, kt * 128:(kt + 1) * 128], QT[0:64, h, q0:q0 + Tq], start=True, stop=True)
                        return pe.matmul(sy[:, 0:Tq], KT[64:128, h, kt * 128:(kt + 1) * 128], QT[64:128, h, q0:q0 + Tq], start=True, stop=True)
                    c.op("pe", [("KT", h, (kt // 4) * 4 if kt < 16 else 16), ("QT", h, gq)], [sxk, syk], e_s)
                    pT, pTk = pT_rot.next()
                    c.op("act", [sxk], [(pTk, 0)],
                         lambda a: a.activation(out=pT[:, 0, 0:Tq], in_=sx[:, 0:Tq], func=AF.Exp, scale=SCALE))
                    c.op("act", [syk], [(pTk, 1)],
                         lambda a: a.activation(out=pT[:, 1, 0:Tq], in_=sy[:, 0:Tq], func=AF.Exp, scale=SCALE))
                    return (pT, pTk)

                def emit_pv(it, pt):
                    h, ki, kt = it
                    pT, pTk = pt

                    def e_o(pe):
                        for qi in range(nq):
                            for m in range(2):
                                ins = pe.matmul(ps[4 + qi][:, m * 256:m * 256 + 129], pT[:, m, qi * 128:(qi + 1) * 128], VA[:, kt, h, :],
                                                start=(ki == 0 and m == 0), stop=(ki == nk - 1), skip_group_check=True)
                        return ins
                    c.op("pe", [(pTk, 0), (pTk, 1), ("VA", kt)], [f"ps{4 + qi}" for qi in range(nq)], e_o)
                    if ki != nk - 1:
                        return
                    sm, smk = sm_rot.next()
                    of, ofk = of_rot.next()
                    for qi in range(nq):
                        pk = f"ps{4 + qi}"
                        pb = ps[4 + qi]
                        c.op("dve", [pk], [smk], lambda v, qi=qi, pb=pb: v.reciprocal(out=sm[:, qi:qi + 1], in_=pb[:, 128:129]))
                        c.op("dve", [pk, smk], [smk], lambda v, qi=qi, pb=pb: v.reciprocal(out=sm[:, 4 + qi:5 + qi], in_=pb[:, 384:385]))
                        c.op("dve", ["lsm", smk], [smk],
                             lambda v, qi=qi: v.tensor_scalar_mul(out=sm[:, 4 + qi:5 + qi], in0=sm[:, 4 + qi:5 + qi], scalar1=lsm[:, 4:5]))
                        c.op("dve", [pk, smk], [ofk],
                             lambda v, qi=qi, pb=pb: v.tensor_scalar_mul(out=of[:, qi, :], in0=pb[:, 0:128], scalar1=sm[:, qi:qi + 1]))
                        c.op("dve", [pk, smk, ofk], [ofk],
                             lambda v, qi=qi, pb=pb: v.scalar_tensor_tensor(out=of[:, qi, :], in0=pb[:, 256:384], scalar=sm[:, 4 + qi:5 + qi], in1=of[:, qi, :], op0=ALU.mult, op1=ALU.add))
                        jk, jkk = jk_rot.next()
                        c.op("act", [ofk], [jkk, smk],
                             lambda a, qi=qi, jk=jk: a.activation(out=jk[:], in_=of[:, qi, :], func=AF.Square, accum_out=sm[:, 8 + qi:9 + qi]))
                    c.op("dve", [smk], [smk], lambda v: v.tensor_scalar(out=sm[:, 8:8 + nq], in0=sm[:, 8:8 + nq], scalar1=1.0 / 128.0, scalar2=SUBLN_EPS, op0=ALU.mult, op1=ALU.add))
                    c.op("act", [smk], [smk], lambda a: a.activation(out=sm[:, 12:12 + nq], in_=sm[:, 8:8 + nq], func=AF.Ln))
                    c.op("act", [smk], [smk], lambda a: a.activation(out=sm[:, 12:12 + nq], in_=sm[:, 12:12 + nq], func=AF.Exp, scale=-0.5))
                    for qi in range(nq):
                        c.op("dve", [ofk, smk, "gsub"], [otk],
                             lambda v, qi=qi: v.scalar_tensor_tensor(
                                 out=ot[:, qi, h * 128:(h + 1) * 128], in0=of[:, qi, :], scalar=sm[:, 12 + qi:13 + qi], in1=gsub[:], op0=ALU.mult, op1=ALU.mult))

                for idx in range(len(its) + LA):
                    if idx < len(its):
                        pend.append(emit_s(its[idx]))
                    if idx - LA >= 0:
                        emit_pv(its[idx - LA], pend[idx - LA])
                for qi, t in enumerate(qg):
                    c.dma("pool", osc[b, t * 128:(t + 1) * 128, hh * 512:(hh + 1) * 512], ot[:, qi, :], [otk], [("osc", b, t, hh)])

    def phase_oproj(l, b, last):
        typ_a = (l % 2 == 0)
        jj = l // 2
        with c.phase(f"oproj_{l}_{b}") as ph:
            wo = ph.sbuf("wo", [128, 8, D], BF16)
            gb1 = ph.sbuf("gb1", [128, 2, D], F32)
            g1l = ph.sbuf("g1l", [128, D], F32)
            g1c = ph.sbuf("g1c", [128, D], F32)
            wsrc = (wo_a if typ_a else wo_b)[jj]
            for k in range(8):
                c.dma("pool", wo[:, k, :], wsrc[k * 128:(k + 1) * 128, :], [], ["wo"])
            c.dma("sp", [gb1[:, 0, :], gb1[:, 1, :]],
                  [ln_attn_g[l].partition_broadcast(128), ln_attn_b[l].partition_broadcast(128)], [], ["gb1"])
            c.dma("sp", g1l[:], gsc[l, b, 0], [("gsc", l, b, 0, 0), ("gsc", l, b, 0, 1)], ["g1l"])
            c.dma("sp", g1c[:], gsc[l, 2, 0], [("gsc", l, 2, 0, 0), ("gsc", l, 2, 0, 1)], ["g1c"])
            rots = {"stats": Rot(ph, "st", 4, [128, 16], F32), "xn": Rot(ph, "xn", 2, [128, D], F32),
                    "tps": PsRot(c, [0, 1])}
            ob_rot = Rot(ph, "ob", 3, [128, D], BF16)
            xf_rot = Rot(ph, "xf", 3, [128, D], F32)
            oT_rot = Rot(ph, "oT", 2, [128, 8, 128], BF16)
            r_rot = Rot(ph, "r", 3, [128, D], F32)
            x1_rot = Rot(ph, "x1", 2, [128, D], F32)
            xnb_rot = Rot(ph, "xnb", 5, [128, D], BF16)
            sg_rot = Rot(ph, "sg", 2, [128, 64], F32)
            alps = PsRot(c, [2, 3, 4, 5])
            rps = PsRot(c, [6, 7])
            tiles = list(range(NTL)) + ([] if last else [16, 17])
            loads = {}
            st1 = {}
            st1a = {}

            def load(t):
                ob, obk = ob_rot.next()
                c.dma("sp", ob[:], osc[b, t * 128:(t + 1) * 128, :], [], [obk])
                xf, xfk = xf_rot.next()
                ap, dk = xin_ap(l, b, t)
                c.dma("sp", xf[:], ap, [dk], [xfk])
                loads[t] = (ob, obk, xf, xfk)

            def stage1(t):
                g1 = g1l if t < NTL else g1c
                g1k = "g1l" if t < NTL else "g1c"
                ob, obk, xf, xfk = loads.pop(t)
                pst, psk = rots["tps"].next()
                pv = pst[:].bitcast(BF16)

                def e_t(pe):
                    for j in range(8):
                        ins = pe.transpose(out=pv[:, j * 128:(j + 1) * 128], in_=ob[:, j * 128:(j + 1) * 128], identity=identb[:])
                    return ins
                c.op("pe", [obk, "identb"], [psk], e_t)
                oT, oTk = oT_rot.next()
                c.op("act", [psk], [oTk], lambda a: a.copy(out=oT[:].rearrange("p j t -> p (j t)"), in_=pv[:, :]))
                r, rk = r_rot.next()
                for half in range(2):
                    apst, apsk = alps.next()

                    def e_a(pe, apst=apst, half=half):
                        for k in range(8):
                            ins = pe.matmul(apst[:, :], oT[:, k, :], wo[:, k, half * 512:(half + 1) * 512], start=(k == 0), stop=(k == 7))
                        return ins
                    c.op("pe", [oTk, "wo"], [apsk], e_a)
                    c.op("dve", [apsk, g1k], [(rk, half)],
                         lambda v, apst=apst, half=half: v.tensor_tensor(out=r[:, half * 512:(half + 1) * 512], in0=apst[:, :], in1=g1[:, half * 512:(half + 1) * 512], op=ALU.mult))
                c.op("dve", [(rk, 0), (rk, 1), xfk], [rk],
                     lambda g: g.scalar_tensor_tensor(out=r[:], in0=xf[:], scalar=ALPHA, in1=r[:], op0=ALU.mult, op1=ALU.add))
                st1a[t] = (ln_stats(rots, r, rk), r, rk)

            def stage1b(t):
                stt, r, rk = st1a.pop(t)
                x1, x1k = x1_rot.next()
                xnb, xnbk = xnb_rot.next()
                ln_apply(rots, stt, r, rk, gb1, "gb1", x1, x1k, xnb, xnbk)
                c.dma("pool", xb[b, t * 128:(t + 1) * 128, :], x1[:], [x1k], [("xb", b, t)])
                st1[t] = (xnb, xnbk)

            h2ps = PsRot(c, [6, 7])

            def stage2(t0):
                cls = cls_of(b, t0)
                pair = [st1.pop(t0), st1.pop(t0 + 1)]
                for jh in range(2):
                    pst, psk = h2ps.next()
                    pv = pst[:].bitcast(BF16)

                    def e_t2(pe):
                        for jj_ in range(4):
                            j = jh * 4 + jj_
                            for i in range(2):
                                ins = pe.transpose(out=pv[:, jj_ * 256 + i * 128:jj_ * 256 + (i + 1) * 128],
                                                   in_=pair[i][0][:, j * 128:(j + 1) * 128], identity=identb[:])
                        return ins
                    c.op("pe", [pair[0][1], pair[1][1], "identb"], [psk], e_t2)
                    for jj_ in range(4):
                        j = jh * 4 + jj_
                        c.op("act", [psk, "modS"], [("h2T", t0, j)],
                             lambda a, j=j, jj_=jj_, pv=pv: a.activation(
                                 out=h2T[:, j, t0 * 128:(t0 + 2) * 128], in_=pv[:, jj_ * 256:(jj_ + 1) * 256], func=AF.Identity,
                                 bias=mod_ap(l, 3, j, cls), scale=mod_ap(l, 2, j, cls)))

            n = len(tiles)
            for i in range(min(2, n)):
                load(tiles[i])
            for i in range(n + 1):
                if i < n:
                    stage1(tiles[i])
                    if i + 2 < n:
                        load(tiles[i + 2])
                if i >= 1:
                    stage1b(tiles[i - 1])
                    if (i - 1) % 2 == 1 and i - 1 >= 3:
                        stage2(tiles[i - 4])
            for t0 in sorted(k for k in list(st1.keys()) if k % 2 == 0):
                stage2(t0)
            nt = n
            rp = ps[2]

            def e_r(pe):
                for ti, t in enumerate(tiles):
                    for k in range(8):
                        ins = pe.matmul(rp[:, ti * E:(ti + 1) * E], h2T[:, k, t * 128:(t + 1) * 128], wrb[:, k, :],
                                        start=(ti == 0 and k == 0), stop=(k == 7), skip_group_check=True)
                return ins
            c.op("pe", [("h2T", t0, j) for t0 in tiles[::2] for j in range(8)] + ["wrb"], ["ps2"], e_r)
            router_topk(ph, rp, "ps2", tiles)
            if debug:
                c.dma("sp", dbg_gates[b].rearrange("(t p) e -> p t e", p=128), gates[:], [("gates", t) for t in tiles], [("dbg_gates", b)], is_output=True)

    def router_topk(ph, rp, rpk, tiles):
        G = len(tiles)
        W = G * E
        sgs = ph.sbuf("rt_s", [128, NT * E], F32)
        sgb = ph.sbuf("rt_b", [128, NT * E], F32)
        scr = ph.sbuf("rt_x", [128, 8, NT * 4], F32)
        red = ph.sbuf("rt_r", [128, 2, NT], F32)
        k = "rt"
        s_ = sgs[:, 0:W]
        b_all = sgb[:, 0:W]
        Bv = b_all.rearrange("p (g i) -> p g i", i=4)
        n4 = G * 4

        def sc(i):
            return scr[:, i, 0:n4]
        c.op("act", [rpk], [k], lambda a: a.activation(out=s_, in_=rp[:, 0:W], func=AF.Exp, scale=-1.0))
        c.op("dve", [k], [k], lambda v: v.tensor_scalar_add(out=s_, in0=s_, scalar1=1.0))
        c.op("dve", [k], [k], lambda v: v.reciprocal(out=s_, in_=s_))
        c.op("dve", [k, "rbias"], [k], lambda v: v.tensor_tensor(
            out=b_all.rearrange("p (t e) -> p t e", e=E), in0=s_.rearrange("p (t e) -> p t e", e=E),
            in1=rbias[:].unsqueeze(1).to_broadcast([128, G, E]), op=ALU.add))
        a0, a1, a2, a3 = (Bv[:, :, i] for i in range(4))
        P_, Q_, R_, S_, M1, M2, GS = sc(0), sc(1), sc(2), sc(3), sc(4), sc(5), sc(6)
        steps = [
            (P_, a0, a1, ALU.max), (Q_, a0, a1, ALU.min), (R_, a2, a3, ALU.max), (S_, a2, a3, ALU.min),
            (M1, P_, R_, ALU.max),
            (M2, P_, R_, ALU.min),
            (Q_, Q_, S_, ALU.max),
            (M2, M2, Q_, ALU.max),
            (GS, M1, M2, ALU.add),
        ]
        for (o, i0, i1, op_) in steps:
            c.op("dve", [k], [k], lambda v, o=o, i0=i0, i1=i1, op_=op_: v.tensor_tensor(out=o, in0=i0, in1=i1, op=op_))
        gs3 = GS.rearrange("p (t g) -> p t g", g=4)
        c.op("dve", [k], [k], lambda v: v.tensor_reduce(out=red[:, 0, 0:G], in_=gs3, axis=AX.X, op=ALU.max))
        c.op("dve", [k], [k], lambda v: v.tensor_tensor(out=gs3, in0=gs3, in1=red[:, 0, 0:G].unsqueeze(2).to_broadcast([128, G, 4]), op=ALU.is_ge))
        c.op("dve", [k], [k], lambda v: v.tensor_tensor(out=Bv, in0=Bv, in1=M2.unsqueeze(2).to_broadcast([128, n4, 4]), op=ALU.is_ge))
        c.op("dve", [k], [k], lambda v: v.tensor_tensor(out=Bv, in0=Bv, in1=GS.unsqueeze(2).to_broadcast([128, n4, 4]), op=ALU.mult))
        c.op("dve", [k], [k], lambda v: v.tensor_tensor(out=b_all, in0=b_all, in1=s_, op=ALU.mult))
        b3 = b_all.rearrange("p (t e) -> p t e", e=E)
        c.op("dve", [k], [k], lambda v: v.tensor_reduce(out=red[:, 0, 0:G], in_=b3, axis=AX.X, op=ALU.add))
        c.op("dve", [k], [k], lambda v: v.reciprocal(out=red[:, 1, 0:G], in_=red[:, 0, 0:G]))
        t0 = tiles[0]
        c.op("dve", [k], [("gates", t) for t in tiles],
             lambda v: v.tensor_tensor(out=gates[:, t0:t0 + G, :], in0=b3,
                                       in1=red[:, 1, 0:G].unsqueeze(2).to_broadcast([128, G, E]), op=ALU.mult))

    def phase_moe(l, b, last):
        ntile = NTL if last else NT
        ntok = ntile * 128
        with c.phase() as pho:
            acc = pho.sbuf("acc", [128, NT, D], F32)
            with c.phase(f"moe_{l}_{b}") as ph:
                wg_rot = Rot(ph, "wg", 2, [128, 8, DE], BF16)
                wu_rot = Rot(ph, "wu", 2, [128, 8, DE], BF16)
                wd_rot = Rot(ph, "wd", 2, [128, 4, D], BF16)
                sg_rot = Rot(ph, "sgl", 2, [128, 512], BF16)
                act_rot = Rot(ph, "act", 2, [128, 4, 512], BF16)
                gps = PsRot(c, [0, 1])
                ups = PsRot(c, [2, 3])
                yps = PsRot(c, [4, 5, 6, 7])
                ttiles = []
                t0 = 0
                while t0 < ntile:
                    n = min(4, ntile - t0)
                    ttiles.append((t0, n))
                    t0 += n
                for e in range(E):
                    wg, wgk = wg_rot.next()
                    wu, wuk = wu_rot.next()
                    wd, wdk = wd_rot.next()
                    c.dma("pool", wg[:], w_gate[l, e].rearrange("(k p) f -> p k f", p=128), [], [wgk])
                    c.dma("pool", wu[:], w_up[l, e].rearrange("(k p) f -> p k f", p=128), [], [wuk])
                    c.dma("pool", wd[:], w_down[l, e].rearrange("(k p) f -> p k f", p=128), [], [wdk])
                    for (t0, n) in ttiles:
                        T = n * 128
                        c0 = t0 * 128
                        hk = [("h2T", t) for t in range(t0, t0 + n)]
                        at, atk = act_rot.next()
                        for fc in range(4):
                            gp, gpk = gps.next()
                            up, upk = ups.next()

                            def e_g(pe, gp=gp, wg=wg, fc=fc, c0=c0, T=T):
                                for k in range(8):
                                    ins = pe.matmul(gp[:, 0:T], wg[:, k, fc * 128:(fc + 1) * 128], h2T[:, k, c0:c0 + T], start=(k == 0), stop=(k == 7))
                                return ins

                            def e_u(pe, up=up, wu=wu, fc=fc, c0=c0, T=T):
                                for k in range(8):
                                    ins = pe.matmul(up[:, 0:T], wu[:, k, fc * 128:(fc + 1) * 128], h2T[:, k, c0:c0 + T], start=(k == 0), stop=(k == 7))
                                return ins
                            c.op("pe", [wgk] + hk, [gpk], e_g)
                            c.op("pe", [wuk] + hk, [upk], e_u)
                            sgl, sglk = sg_rot.next()
                            c.op("act", [gpk], [sglk], lambda a, gp=gp, sgl=sgl, T=T: a.activation(out=sgl[:, 0:T], in_=gp[:, 0:T], func=AF.Silu))
                            c.op("dve", [upk, sglk], [(atk, fc)],
                                 lambda v, up=up, sgl=sgl, at=at, fc=fc, T=T: v.tensor_tensor(out=at[:, fc, 0:T], in0=up[:, 0:T], in1=sgl[:, 0:T], op=ALU.mult))
                        for si in range(n):
                            t = t0 + si
                            for half in range(2):
                                yp, ypk = yps.next()

                                def e_y(pe, yp=yp, at=at, si=si, half=half, wd=wd):
                                    for fc in range(4):
                                        ins = pe.matmul(yp[:, :], at[:, fc, si * 128:(si + 1) * 128], wd[:, fc, half * 512:(half + 1) * 512], start=(fc == 0), stop=(fc == 3))
                                    return ins
                                c.op("pe", [(atk, fc) for fc in range(4)] + [wdk], [ypk], e_y)
                                av = acc[:, t, half * 512:(half + 1) * 512]
                                if e == 0:
                                    c.op("dve", [ypk, ("gates", t)], [("acc", t, half)],
                                         lambda v, yp=yp, av=av, t=t, e=e: v.tensor_scalar_mul(out=av, in0=yp[:, :], scalar1=gates[:, t, e:e + 1]))
                                else:
                                    c.op("dve", [ypk, ("gates", t)], [("acc", t, half)],
                                         lambda v, yp=yp, av=av, t=t, e=e: v.scalar_tensor_tensor(out=av, in0=yp[:, :], scalar=gates[:, t, e:e + 1], in1=av, op0=ALU.mult, op1=ALU.add))
            with c.phase(f"ln2_{l}_{b}") as ph:
                gb2 = ph.sbuf("gb2", [128, 2, D], F32)
                g2l = ph.sbuf("g2l", [128, D], F32)
                g2c = ph.sbuf("g2c", [128, D], F32)
                c.dma("sp", [gb2[:, 0, :], gb2[:, 1, :]],
                      [ln_ffn_g[l].partition_broadcast(128), ln_ffn_b[l].partition_broadcast(128)], [], ["gb2"])
                c.dma("sp", g2l[:], gsc[l, b, 1], [], ["g2l"])
                c.dma("sp", g2c[:], gsc[l, 2, 1], [], ["g2c"])
                rots = {"stats": Rot(ph, "st", 3, [128, 16], F32), "xn": Rot(ph, "xn", 2, [128, D], F32)}
                xf_rot = Rot(ph, "xf", 3, [128, D], F32)
                x2_rot = Rot(ph, "x2", 2, [128, D], F32)
                lds = {}

                def load2(t):
                    xf, xfk = xf_rot.next()
                    c.dma("sp", xf[:], xb[b, t * 128:(t + 1) * 128, :], [], [xfk])
                    lds[t] = (xf, xfk)
                for t in range(min(2, ntile)):
                    load2(t)
                for t in range(ntile):
                    g2 = g2l if t < NTL else g2c
                    g2k = "g2l" if t < NTL else "g2c"
                    xf, xfk = lds.pop(t)
                    rk = ("accr", t)
                    c.op("dve", [g2k], [rk], lambda v, t=t, g2=g2: v.tensor_tensor(out=acc[:, t, :], in0=acc[:, t, :], in1=g2[:], op=ALU.mult))
                    c.op("dve", [xfk], [rk], lambda g, t=t, xf=xf: g.scalar_tensor_tensor(out=acc[:, t, :], in0=xf[:], scalar=ALPHA, in1=acc[:, t, :], op0=ALU.mult, op1=ALU.add))
                    if t + 2 < ntile:
                        load2(t + 2)
                    x2, x2k = x2_rot.next()
                    layer_norm(rots, acc[:, t, :], rk, gb2, "gb2", x2, x2k)
                    if last:
                        c.dma("pool", out[b, t * 128:(t + 1) * 128, :], x2[:], [x2k], [("out", b, t)], is_output=True)
                    else:
                        c.dma("pool", xa[b, t * 128:(t + 1) * 128, :], x2[:], [x2k], [("xa", b, t)], is_output=debug)

    ada_prologue()
    done = stop_after is not None and stop_after[0] == "ada"
    if done:
        n_layers = 0
    for l in range(n_layers):
        last = (l == DEPTH - 1)
        for b in range(NB):
            if l % 2 == 0:
                phase_attn_A(l, b, last)
            else:
                for hh in range(2):
                    phase_attn_B(l, b, last, hh)
            if stop_after is not None and stop_after[0] in ("attn", "qkv") and stop_after[1:] == (l, b):
                done = True
                break
            phase_oproj(l, b, last)
            if stop_after == ("oproj", l, b):
                done = True
                break
            phase_moe(l, b, last)
            if stop_after == ("moe", l, b):
                done = True
                break
        if done:
            break
    c.finish()
    return nc


def _consts():
    ident = np.eye(128, dtype=np.float32)
    R = np.zeros((64, 64), np.float32)
    for d in range(64):
        if (d % 32) < 16:
            R[d, d + 16] = -1.0
        else:
            R[d, d - 16] = 1.0
    RT = np.zeros((128, 128), np.float32)
    RT[:64, :64] = R.T
    RT[64:, 64:] = R.T
    t = np.arange(S)
    rows = (t // 64).astype(np.float64)
    cols = (t % 64).astype(np.float64)
    inv = 10000.0 ** (-np.arange(16, dtype=np.float64) / 16.0)
    cos = np.zeros((128, S), np.float32)
    sin = np.zeros((128, S), np.float32)
    for p in range(128):
        d = p % 64
        pos = rows if d < 32 else cols
        ang = pos * np.float64(np.float32(inv[d % 16]))
        ang = (pos.astype(np.float32) * np.float32(inv[d % 16])).astype(np.float64)
        cos[p] = np.cos(ang)
        sin[p] = np.sin(ang)
    kk = np.arange(128)[:, None]
    qq = np.arange(128)[None, :]
    mp = np.where(kk >= qq, 0.0, NEG).astype(np.float32)
    mn = np.where(kk <= qq, 0.0, NEG).astype(np.float32)
    return {"k_ident": ident, "k_rt": RT, "k_cos": cos, "k_sin": sin,
            "k_maskp": np.tile(mp, (1, 4)), "k_maskn": np.tile(mn, (1, 4))}


_NC_CACHE = {}


def make_in_maps(inputs):
    consts = _consts()
    shared = {k: np.ascontiguousarray(inputs[k], dtype=np.float32) for k in (
        "w_ada", "b_ada", "wqkv_a", "wo_a", "sink_a", "wqkv_b", "wo_b", "lambda_b", "subln_b",
        "ln_attn_g", "ln_attn_b", "ln_ffn_g", "ln_ffn_b", "w_router", "router_bias", "w_gate", "w_up", "w_down")}
    shared.update(consts)
    in_maps = []
    for i in range(NCORES):
        m = dict(shared)
        m["x"] = np.ascontiguousarray(inputs["x"][NB * i:NB * (i + 1)], dtype=np.float32)
        m["ctx"] = np.ascontiguousarray(inputs["ctx"][NB * i:NB * (i + 1)], dtype=np.float32)
        m["c3"] = np.ascontiguousarray(
            np.concatenate([inputs["c"][NB * i:NB * (i + 1)], inputs["c_ctx"][None, :]], axis=0), dtype=np.float32)
        in_maps.append(m)
    return in_maps


def kernel(**inputs):
    if "nc" not in _NC_CACHE:
        _NC_CACHE["nc"] = build()
    nc = _NC_CACHE["nc"]
    in_maps = make_in_maps(inputs)
    res = run_bass_kernel_spmd(nc, in_maps, core_ids=list(range(NCORES)))
    return np.concatenate([np.asarray(r["out"]) for r in res.results], axis=0).astype(np.float32)
```

```python
import math
from contextlib import ExitStack, contextmanager

import numpy as np
import concourse.bass as bass
import concourse.mybir as mybir
from concourse.bass_utils import run_bass_kernel_spmd

F32 = mybir.dt.float32
BF16 = mybir.dt.bfloat16
AF = mybir.ActivationFunctionType
ALU = mybir.AluOpType
AX = mybir.AxisListType

DEPTH = 4
D = 1024
S = 2048
C = 256
TOK = S + C
NT = TOK // 128
NTL = S // 128
NB = 2
NCORES = 8
E = 16
DE = 512
ALPHA = (2 * DEPTH) ** 0.25
LN_EPS = 1e-6
SUBLN_EPS = 1e-5
SCALE = 0.125
NEG = -30000.0

SEM_ROLL = 30000
SAME_ENGINE_SYNC = ("act", "dve", "pool")
import os
DBG_CUT = int(os.environ.get("DBG_CUT", "9"))
SCOPES = bool(int(os.environ.get("DBG_SCOPES", "0")))
N_DMA_SLOTS = {"sp": 16, "pool": 12}


class Ctx:
    def __init__(self, nc, same_engine_sync=SAME_ENGINE_SYNC):
        self.nc = nc
        self.es = ExitStack()
        self.eng = {"pe": nc.tensor, "act": nc.scalar, "dve": nc.vector,
                    "pool": nc.gpsimd, "sp": nc.sync}
        self.same_engine_sync = set(same_engine_sync)
        self.sem = {}
        self.cnt = {}
        self.nsem = 0
        self.uid = 0
        for e in ("pe", "act", "dve", "pool"):
            self._new_sem(e)
        self.known = {e: {} for e in self.eng}
        self.last_w = {}
        self.readers = {}
        self.dma_sems = {}
        self.dma_val = {}
        self.dma_i = {}
        for q, n in N_DMA_SLOTS.items():
            self.dma_sems[q] = [self.es.enter_context(nc.semaphore(f"dq_{q}_{i}")) for i in range(n)]
            self.dma_val[q] = [0] * n
            self.dma_i[q] = 0
        self.out_events = []
        self.psall = self.es.enter_context(nc.psum_tensor("psall", [128, 8 * 512], F32))
        self.ps = [self.psall[:, i * 512:(i + 1) * 512] for i in range(8)]

    def _new_sem(self, e):
        self.nsem += 1
        self.sem[e] = self.es.enter_context(self.nc.semaphore(f"s_{e}_{self.nsem}"))
        self.cnt[e] = 0

    def sbuf(self, name, shape, dt):
        self.uid += 1
        return self.es.enter_context(self.nc.sbuf_tensor(f"{name}_{self.uid}", list(shape), dt))

    @contextmanager
    def phase(self, name=None):
        st = ExitStack()
        ctx = self
        if name is not None and SCOPES:
            st.enter_context(self.nc.named_scope(name))

        class PH:
            def sbuf(self_, name, shape, dt):
                ctx.uid += 1
                return st.enter_context(ctx.nc.sbuf_tensor(f"{name}_{ctx.uid}", list(shape), dt))

        try:
            yield PH()
        finally:
            self.barrier()
            st.close()

    def _deps(self, reads, writes):
        evs = []
        for k in list(reads) + list(writes):
            ev = self.last_w.get(k)
            if ev is not None:
                evs.append(ev)
        for k in writes:
            evs.extend(self.readers.get(k, ()))
        return evs

    def _emit_waits(self, engine, evs):
        best = {}
        for (sem, val, src) in evs:
            if src == engine and engine not in self.same_engine_sync:
                continue
            key = id(sem)
            if self.known[engine].get(key, 0) >= val:
                continue
            if key not in best or best[key][1] < val:
                best[key] = (sem, val)
        for key, (sem, val) in best.items():
            self.eng[engine].wait_ge(sem, val)
            self.known[engine][key] = val

    def _record(self, ev, reads, writes):
        for k in writes:
            self.last_w[k] = ev
            self.readers[k] = []
        for k in reads:
            self.readers.setdefault(k, []).append(ev)

    @staticmethod
    def _excl(reads, writes):
        r = [k for k in reads if not (isinstance(k, str) and k.startswith("ps"))]
        w = list(writes) + [k for k in reads if isinstance(k, str) and k.startswith("ps")]
        return r, w

    def op(self, engine, reads, writes, emit):
        reads, writes = self._excl(reads, writes)
        self._emit_waits(engine, self._deps(reads, writes))
        ins = emit(self.eng[engine])
        if self.cnt[engine] >= SEM_ROLL:
            self._new_sem(engine)
        self.cnt[engine] += 1
        ins.then_inc(self.sem[engine], 1)
        ev = (self.sem[engine], self.cnt[engine], engine)
        self._record(ev, reads, writes)
        return ev

    def dma(self, queue, out_ap, in_ap, reads, writes, is_output=False):
        slot = self.dma_i[queue] % len(self.dma_sems[queue])
        self.dma_i[queue] += 1
        sem = self.dma_sems[queue][slot]
        evs = self._deps(reads, writes)
        if self.dma_val[queue][slot] > 0:
            evs.append((sem, self.dma_val[queue][slot], "dma"))
        self._emit_waits(queue, evs)
        outs = out_ap if isinstance(out_ap, (list, tuple)) else [out_ap]
        ins_ = in_ap if isinstance(in_ap, (list, tuple)) else [in_ap]
        for o, i in zip(outs, ins_):
            self.eng[queue].dma_start(out=o, in_=i).then_inc(sem, 16)
            self.dma_val[queue][slot] += 16
        ev = (sem, self.dma_val[queue][slot], "dma")
        self._record(ev, reads, writes)
        if is_output:
            self.out_events.append(ev)
        return ev

    def _all_events(self):
        evs = []
        for e in ("pe", "act", "dve", "pool"):
            if self.cnt[e] > 0:
                evs.append((self.sem[e], self.cnt[e], "bar"))
        for q in self.dma_sems:
            for s, v in zip(self.dma_sems[q], self.dma_val[q]):
                if v > 0:
                    evs.append((s, v, "dma"))
        return evs

    def barrier(self):
        evs = self._all_events()
        for e in ("pe", "act", "dve", "pool", "sp"):
            self._emit_waits(e, evs)
        self.last_w.clear()
        self.readers.clear()

    def finish(self):
        self._emit_waits("sp", list(self.out_events) + self._all_events())
        self.es.close()


class Rot:
    def __init__(self, alloc, name, n, shape, dt):
        self.tiles = [alloc.sbuf(f"{name}{i}", shape, dt) for i in range(n)]
        self.keys = [f"{name}#{id(self)}#{i}" for i in range(n)]
        self.i = -1

    def next(self):
        self.i = (self.i + 1) % len(self.tiles)
        return self.tiles[self.i], self.keys[self.i]


class PsRot:
    def __init__(self, c, banks):
        self.c = c
        self.banks = list(banks)
        self.i = -1

    def next(self):
        self.i = (self.i + 1) % len(self.banks)
        b = self.banks[self.i]
        return self.c.ps[b], f"ps{b}"


def build(n_layers=DEPTH, debug=False, stop_after=None):
    nc = bass.Bass("TRN2", target_bir_lowering=False)

    def din(name, shape, dt=F32):
        return nc.dram_tensor(name, list(shape), dt, kind="ExternalInput").ap()

    x = din("x", [NB, S, D])
    ctxin = din("ctx", [NB, C, D])
    c3 = din("c3", [3, D])
    w_ada = din("w_ada", [DEPTH, D, 6 * D])
    b_ada = din("b_ada", [DEPTH, 6 * D])
    wqkv_a = din("wqkv_a", [2, D, 1536])
    wo_a = din("wo_a", [2, D, D])
    sink_a = din("sink_a", [2, 16])
    wqkv_b = din("wqkv_b", [2, D, 3072])
    wo_b = din("wo_b", [2, D, D])
    lambda_b = din("lambda_b", [2, 4, 64])
    subln_b = din("subln_b", [2, 128])
    ln_attn_g = din("ln_attn_g", [DEPTH, D])
    ln_attn_b = din("ln_attn_b", [DEPTH, D])
    ln_ffn_g = din("ln_ffn_g", [DEPTH, D])
    ln_ffn_b = din("ln_ffn_b", [DEPTH, D])
    w_router = din("w_router", [D, E])
    router_bias = din("router_bias", [E])
    w_gate = din("w_gate", [DEPTH, E, D, DE])
    w_up = din("w_up", [DEPTH, E, D, DE])
    w_down = din("w_down", [DEPTH, E, DE, D])
    k_ident = din("k_ident", [128, 128])
    k_rt = din("k_rt", [128, 128])
    k_cos = din("k_cos", [128, S])
    k_sin = din("k_sin", [128, S])
    k_maskp = din("k_maskp", [128, 512])
    k_maskn = din("k_maskn", [128, 512])

    out = nc.dram_tensor("out", [NB, S, D], F32, kind="ExternalOutput").ap()
    skind = "ExternalOutput" if debug else "Internal"
    xa = nc.dram_tensor("xa", [NB, TOK, D], F32, kind=skind).ap()
    xb = nc.dram_tensor("xb", [NB, TOK, D], F32, kind=skind).ap()
    osc = nc.dram_tensor("osc", [NB, TOK, D], BF16, kind="Internal").ap()
    gsc = nc.dram_tensor("gsc", [DEPTH, 3, 2, 128, D], F32, kind="Internal").ap()
    if debug:
        dbg_gates = nc.dram_tensor("dbg_gates", [NB, TOK, E], F32, kind="ExternalOutput").ap()
        dbg_mod = nc.dram_tensor("dbg_mod", [128, DEPTH * 4 * 8 * 3], F32, kind="ExternalOutput").ap()
        dbg_o = nc.dram_tensor("dbg_o", [NB, TOK, D], F32, kind="ExternalOutput").ap()

    c = Ctx(nc)
    ps = c.ps

    identf = c.sbuf("identf", [128, 128], F32)
    identb = c.sbuf("identb", [128, 128], BF16)
    rtb = c.sbuf("rtb", [128, 128], BF16)
    modS = c.sbuf("modS", [128, DEPTH * 4 * 8 * 3], F32)
    h2T = c.sbuf("h2T", [128, 8, TOK], BF16)
    gates = c.sbuf("gates", [128, NT, E], F32)
    wrb = c.sbuf("wrb", [128, 8, E], BF16)
    rbias = c.sbuf("rbias", [128, E], F32)

    def mod_ap(l, kind, j, cls):
        o = ((l * 4 + kind) * 8 + j) * 3 + cls
        return modS[:, o:o + 1]

    c.dma("sp", identf[:], k_ident[:, :], [], ["identf"])
    c.dma("pool", identb[:], k_ident[:, :], [], ["identb"])
    c.dma("pool", rtb[:], k_rt[:, :], [], ["rtb"])
    c.dma("pool", wrb[:], w_router.rearrange("(k p) e -> p k e", p=128), [], ["wrb"])
    c.dma("sp", rbias[:], router_bias.partition_broadcast(128), [], ["rbias"])

    def ada_prologue():
        with c.phase("ada") as ph:
            c3s = ph.sbuf("c3s", [3, D], F32)
            silT = ph.sbuf("silT", [128, 8, 3], F32)
            silTb = ph.sbuf("silTb", [128, 8, 3], BF16)
            silbc = [ph.sbuf(f"silbc{i}", [128, 8, 128], BF16) for i in range(3)]
            lnrows = ph.sbuf("lnrows", [64, 128], F32)
            lnT = ph.sbuf("lnT", [128, 64], F32)
            c.dma("sp", c3s[:], c3[:, :], [], ["c3s"])
            c.dma("sp", [lnrows[0:32, :], lnrows[32:64, :]],
                  [ln_attn_g.rearrange("l (j p) -> (l j) p", p=128),
                   ln_attn_b.rearrange("l (j p) -> (l j) p", p=128)], [], ["lnrows"])

            def e_ct(pe):
                for k in range(8):
                    ins = pe.matmul(ps[0][:, k * 3:(k + 1) * 3], c3s[0:3, k * 128:(k + 1) * 128],
                                    identf[0:3, 0:3], start=(k == 0), stop=True, skip_group_check=True)
                return ins
            c.op("pe", ["c3s", "identf"], ["ps0"], e_ct)
            c.op("act", ["ps0"], ["silT"],
                 lambda a: a.activation(out=silT[:].rearrange("p k c -> p (k c)"), in_=ps[0][:, 0:24], func=AF.Silu))
            c.op("dve", ["silT"], ["silTb"], lambda v: v.tensor_copy(out=silTb[:], in_=silT[:]))
            for cls in range(3):
                c.op("dve", ["silT"], [f"silbc{cls}"],
                     lambda v, cls=cls: v.tensor_copy(out=silbc[cls][:],
                                                      in_=silT[:, :, cls:cls + 1].to_broadcast([128, 8, 128])))
            c.op("pe", ["lnrows", "identf"], ["ps1"],
                 lambda pe: pe.matmul(ps[1][:, 0:64], lnrows[0:64, :], identf[0:64, 0:64], start=True, stop=True))
            c.op("dve", ["ps1"], ["lnT"], lambda v: v.tensor_copy(out=lnT[:], in_=ps[1][:, 0:64]))

            wab_rot = Rot(ph, "wab", 3, [128, 8, 512], BF16)
            gst_rot = Rot(ph, "gst", 2, [128, 512], F32)
            gps = PsRot(c, [4, 5, 6])
            bt = ph.sbuf("bt", [48, 128], F32)
            bT = ph.sbuf("bT", [128, 48], F32)
            gbias = ph.sbuf("gbias", [128, 2, D], F32)
            mraw = ph.sbuf("mraw", [128, 32, 3], F32)
            tmp83 = ph.sbuf("tmp83", [128, 8, 3], F32)
            for l in range(n_layers):
                c.dma("sp", bt[:], b_ada[l].rearrange("(j p) -> j p", p=128), [], ["bt"])
                c.dma("sp", [gbias[:, 0, :], gbias[:, 1, :]],
                      [b_ada[l, 2048:3072].partition_broadcast(128),
                       b_ada[l, 5120:6144].partition_broadcast(128)], [], ["gbias"])
                c.op("pe", ["bt", "identf"], ["ps2"],
                     lambda pe: pe.matmul(ps[2][:, 0:48], bt[0:48, :], identf[0:48, 0:48], start=True, stop=True))
                c.op("act", ["ps2"], ["bT"], lambda a: a.copy(out=bT[:], in_=ps[2][:, 0:48]))
                first3 = True
                for cc in range(12):
                    kind = cc // 2
                    half = cc % 2
                    if kind in (2, 5):
                        wa, wak = wab_rot.next()
                        c.dma("pool", wa[:], w_ada[l][:, cc * 512:(cc + 1) * 512].rearrange("(k p) f -> p k f", p=128),
                              [], [wak])
                    else:
                        wa, wak = wab_rot.next()
                        c.dma("pool", wa[:], w_ada[l][:, cc * 512:(cc + 1) * 512].rearrange("(k p) f -> p k f", p=128),
                              [], [wak])
                    if kind in (2, 5):
                        which = 0 if kind == 2 else 1
                        for cls in range(3):
                            pst, psk = gps.next()

                            def e_g(pe, pst=pst, cls=cls, wa=wa):
                                for k in range(8):
                                    ins = pe.matmul(pst[:, :], silbc[cls][:, k, :], wa[:, k, :],
                                                    start=(k == 0), stop=(k == 7))
                                return ins
                            c.op("pe", [wak, f"silbc{cls}"], [psk], e_g)
                            gst, gstk = gst_rot.next()
                            c.op("dve", [psk, "gbias"], [gstk],
                                 lambda v, pst=pst, gst=gst, which=which, half=half: v.tensor_tensor(
                                     out=gst[:], in0=pst[:, :], in1=gbias[:, which, half * 512:(half + 1) * 512],
                                     op=ALU.add))
                            c.dma("sp", gsc[l, cls, which][:, half * 512:(half + 1) * 512], gst[:],
                                  [gstk], [("gsc", l, cls, which, half)])
                    else:
                        ks = {1: 0, 0: 1, 4: 2, 3: 3}[kind]

                        def e_m(pe, wa=wa, ks=ks, half=half, first=first3):
                            for fs in range(4):
                                slot = ks * 8 + half * 4 + fs
                                for k in range(8):
                                    ins = pe.matmul(ps[3][:, slot * 3:(slot + 1) * 3],
                                                    wa[:, k, fs * 128:(fs + 1) * 128], silTb[:, k, :],
                                                    start=(first and fs == 0 and k == 0), stop=(k == 7),
                                                    skip_group_check=True)
                            return ins
                        c.op("pe", [wak, "silTb"], ["ps3"], e_m)
                        first3 = False
                ps3v = ps[3][:, 0:96].rearrange("p (s c) -> p s c", c=3)
                for ks, a0 in ((0, 8), (1, 0), (2, 32), (3, 24)):
                    c.op("dve", ["ps3", "bT"], ["mraw"],
                         lambda v, ks=ks, a0=a0: v.tensor_tensor(
                             out=mraw[:, ks * 8:(ks + 1) * 8, :], in0=ps3v[:, ks * 8:(ks + 1) * 8, :],
                             in1=bT[:, a0:a0 + 8].unsqueeze(2).to_broadcast([128, 8, 3]), op=ALU.add))
                base = l * 96
                mv = modS[:, base:base + 96].rearrange("p (s c) -> p s c", c=3)
                lng = lnT[:, l * 8:(l + 1) * 8].unsqueeze(2).to_broadcast([128, 8, 3])
                lnb = lnT[:, 32 + l * 8:32 + (l + 1) * 8].unsqueeze(2).to_broadcast([128, 8, 3])
                c.op("dve", ["mraw"], ["modS"], lambda v: v.tensor_scalar_add(out=mv[:, 0:8, :], in0=mraw[:, 0:8, :], scalar1=1.0))
                c.op("dve", ["mraw"], ["modS"], lambda v: v.tensor_copy(out=mv[:, 8:16, :], in_=mraw[:, 8:16, :]))
                c.op("dve", ["mraw"], ["tmp83"], lambda v: v.tensor_scalar_add(out=tmp83[:], in0=mraw[:, 16:24, :], scalar1=1.0))
                c.op("dve", ["tmp83", "lnT"], ["modS"], lambda v: v.tensor_tensor(out=mv[:, 16:24, :], in0=tmp83[:], in1=lng, op=ALU.mult))
                c.op("dve", ["tmp83", "lnT"], ["tmp83"], lambda v: v.tensor_tensor(out=tmp83[:], in0=tmp83[:], in1=lnb, op=ALU.mult))
                c.op("dve", ["tmp83", "mraw"], ["modS"], lambda v: v.tensor_tensor(out=mv[:, 24:32, :], in0=tmp83[:], in1=mraw[:, 24:32, :], op=ALU.add))
            if debug:
                c.dma("sp", dbg_mod[:, :], modS[:], ["modS"], ["dbg_mod"], is_output=True)

    def xin_ap(l, b, t):
        if l == 0:
            if t < NTL:
                return x[b, t * 128:(t + 1) * 128, :], ("x", b, t)
            return ctxin[b, (t - NTL) * 128:(t - NTL + 1) * 128, :], ("ctx", b, t)
        return xa[b, t * 128:(t + 1) * 128, :], ("xa", b, t)

    def cls_of(b, t):
        return b if t < NTL else 2

    def make_hT(ph_rots, l, b, tiles, kind_s, kind_b, dst, dst_key, dst_col0, src_loader):
        tps = ph_rots["tps"]
        n = len(tiles)
        srcs = [src_loader(t) for t in tiles]
        for j in range(8):
            pst, psk = tps.next()
            pv = pst[:].bitcast(BF16)

            def e_t(pe, j=j, pv=pv):
                for i in range(n):
                    ins = pe.transpose(out=pv[:, i * 128:(i + 1) * 128], in_=srcs[i][0][:, j * 128:(j + 1) * 128],
                                       identity=identb[:])
                return ins
            c.op("pe", [s[1] for s in srcs] + ["identb"], [psk], e_t)
            cls = cls_of(b, tiles[0])
            c.op("act", [psk, "modS"], [dst_key],
                 lambda a, j=j, pv=pv, cls=cls: a.activation(
                     out=dst[:, j, dst_col0:dst_col0 + n * 128], in_=pv[:, 0:n * 128], func=AF.Identity,
                     bias=mod_ap(l, kind_b, j, cls), scale=mod_ap(l, kind_s, j, cls)))

    def ln_stats(ph_rots, r, rk):
        st, stk = ph_rots["stats"].next()
        c.op("dve", [rk], [stk], lambda v: v.bn_stats(out=st[:, 0:6], in_=r[:, 0:512]))
        c.op("dve", [rk], [stk], lambda v: v.bn_stats(out=st[:, 6:12], in_=r[:, 512:1024]))
        c.op("dve", [stk], [stk], lambda v: v.bn_aggr(out=st[:, 12:14], in_=st[:, 0:12]))
        c.op("dve", [stk], [stk], lambda v: v.tensor_scalar_add(out=st[:, 14:15], in0=st[:, 13:14], scalar1=LN_EPS))
        c.op("act", [stk], [stk], lambda a: a.activation(out=st[:, 14:15], in_=st[:, 14:15], func=AF.Ln))
        c.op("act", [stk], [stk], lambda a: a.activation(out=st[:, 14:15], in_=st[:, 14:15], func=AF.Exp, scale=-0.5))
        c.op("dve", [stk], [stk], lambda v: v.scalar_tensor_tensor(out=st[:, 15:16], in0=st[:, 12:13], scalar=-1.0,
                                                                   in1=st[:, 14:15], op0=ALU.mult, op1=ALU.mult))
        return st, stk

    def ln_apply(ph_rots, stt, r, rk, gb, gbk, xo, xok, xnb=None, xnbk=None):
        st, stk = stt
        xn, xnk = ph_rots["xn"].next()
        c.op("act", [rk, stk], [xnk], lambda a: a.activation(out=xn[:], in_=r[:], func=AF.Identity,
                                                             bias=st[:, 15:16], scale=st[:, 14:15]))
        if xnb is not None:
            c.op("act", [rk, stk], [xnbk], lambda a: a.activation(out=xnb[:], in_=r[:], func=AF.Identity,
                                                                  bias=st[:, 15:16], scale=st[:, 14:15]))
        c.op("pool", [xnk, gbk], [xnk], lambda g: g.tensor_tensor(out=xn[:], in0=xn[:], in1=gb[:, 0, :], op=ALU.mult))
        c.op("pool", [xnk, gbk], [xok], lambda g: g.tensor_tensor(out=xo[:], in0=xn[:], in1=gb[:, 1, :], op=ALU.add))

    def layer_norm(ph_rots, r, rk, gb, gbk, xo, xok, xnb=None, xnbk=None):
        stt = ln_stats(ph_rots, r, rk)
        ln_apply(ph_rots, stt, r, rk, gb, gbk, xo, xok, xnb, xnbk)

    def qkv_project(ph, rots, l, b, last, wq, wqk, nq, wk, wkk, nk, wv, wvk, vcols, QT, KT, VA, cosT, sinT):
        xf_rot, xb_rot, hT_rot, raw_rot, t1_rot, t2_rot = (rots[k] for k in ("xf", "xbf", "hT", "raw", "t1", "t2"))
        mm = rots["mm"]
        vheads = VA.shape[2]
        vd = VA.shape[3] - 1
        groups = [list(range(g * 4, g * 4 + 4)) for g in range(4)] + [[16, 17]]
        for tiles in groups:
            is_ctx = tiles[0] >= NTL
            T = len(tiles) * 128
            col0 = tiles[0] * 128
            loaded = {}

            def loader(t):
                if t in loaded:
                    return loaded[t]
                xf, xfk = xf_rot.next()
                ap, dk = xin_ap(l, b, t)
                c.dma("sp", xf[:], ap, [dk], [xfk])
                xbt, xbk = xb_rot.next()
                c.op("pool", [xfk], [xbk], lambda g, xf=xf, xbt=xbt: g.tensor_copy(out=xbt[:], in_=xf[:]))
                loaded[t] = (xbt, xbk)
                return loaded[t]
            hT, hTk = hT_rot.next()
            make_hT(rots, l, b, tiles, 0, 1, hT, hTk, 0, loader)
            jobs = []
            if not (is_ctx and last):
                jobs += [("q", i) for i in range(nq)]
            jobs += [("k", i) for i in range(nk)]
            def emit_proj(kind, i):
                w, wkey = (wq, wqk) if kind == "q" else (wk, wkk)
                dstT, dkey = (QT, "QT") if kind == "q" else (KT, "KT")
                pst, psk = mm.next()

                def e_p(pe):
                    for k in range(8):
                        ins = pe.matmul(pst[:, 0:T], w[:, k, i * 128:(i + 1) * 128], hT[:, k, 0:T],
                                        start=(k == 0), stop=(k == 7))
                    return ins
                c.op("pe", [wkey, hTk], [psk], e_p)
                if is_ctx:
                    c.op("act", [psk], [(dkey, i, tiles[0])],
                         lambda a: a.copy(out=dstT[:, i, col0:col0 + T], in_=pst[:, 0:T]))
                    return None
                raw, rawk = raw_rot.next()
                c.op("act", [psk], [rawk], lambda a: a.copy(out=raw[:, 0:T], in_=pst[:, 0:T]))
                return (dstT, dkey, i, raw, rawk)

            def emit_rope(st):
                dstT, dkey, i, raw, rawk = st
                pst2, psk2 = mm.next()
                c.op("pe", [rawk, "rtb"], [psk2],
                     lambda pe: pe.matmul(pst2[:, 0:T], rtb[:], raw[:, 0:T], start=True, stop=True))
                t1, t1k = t1_rot.next()
                t2, t2k = t2_rot.next()
                c.op("dve", [rawk, "cos"], [t1k],
                     lambda g: g.tensor_tensor(out=t1[:, 0:T], in0=raw[:, 0:T], in1=cosT[:, col0:col0 + T], op=ALU.mult))
                c.op("dve", [psk2, "sin"], [t2k],
                     lambda v: v.tensor_tensor(out=t2[:, 0:T], in0=pst2[:, 0:T], in1=sinT[:, col0:col0 + T], op=ALU.mult))
                c.op("pool", [t1k, t2k], [(dkey, i, tiles[0])],
                     lambda g: g.tensor_tensor(out=dstT[:, i, col0:col0 + T], in0=t1[:, 0:T], in1=t2[:, 0:T], op=ALU.add))

            prev = None
            for job in jobs + [None]:
                cur = emit_proj(*job) if job is not None else None
                if prev is not None:
                    emit_rope(prev)
                prev = cur
            for si, t in enumerate(tiles):
                pst, psk = mm.next()

                def e_v(pe, pst=pst, hT=hT, si=si):
                    for k in range(8):
                        ins = pe.matmul(pst[:, 0:vcols], hT[:, k, si * 128:(si + 1) * 128], wv[:, k, 0:vcols],
                                        start=(k == 0), stop=(k == 7))
                    return ins
                c.op("pe", [wvk, hTk], [psk], e_v)
                c.op("act", [psk], [("VA", t)],
                     lambda a, pst=pst, t=t: a.copy(out=VA[:, t, :, 0:vd],
                                                    in_=pst[:, 0:vcols].rearrange("p (h d) -> p h d", d=vd)))

    def std_rots(ph, raw_cols=512):
        return {
            "xf": Rot(ph, "xf", 3, [128, D], F32),
            "xbf": Rot(ph, "xbf", 5, [128, D], BF16),
            "hT": Rot(ph, "hT", 2, [128, 8, 512], BF16),
            "raw": Rot(ph, "raw", 3, [128, 512], BF16),
            "t1": Rot(ph, "t1", 2, [128, 512], F32),
            "t2": Rot(ph, "t2", 2, [128, 512], F32),
            "tps": PsRot(c, [0, 1]),
            "mm": PsRot(c, [2, 3, 4, 5]),
        }

    def phase_attn_A(l, b, last):
        jj = l // 2
        with c.phase(f"attnA_{l}_{b}") as ph:
            rots = std_rots(ph)
            wq = ph.sbuf("wq", [128, 8, 1024], BF16)
            wk = ph.sbuf("wk", [128, 8, 4, 2, 64], BF16)
            wv = ph.sbuf("wv", [128, 8, 256], BF16)
            cosT = ph.sbuf("cosT", [128, S], F32)
            sinT = ph.sbuf("sinT", [128, S], F32)
            maskp = ph.sbuf("maskp", [128, 512], BF16)
            maskn = ph.sbuf("maskn", [128, 512], BF16)
            esink = ph.sbuf("esink", [128, 16], F32)
            QT = ph.sbuf("QT", [128, 8, TOK], BF16)
            KT = ph.sbuf("KT", [128, 4, TOK], BF16)
            VA = ph.sbuf("VA", [128, NT, 4, 65], BF16)
            wsrc = wqkv_a[jj]
            for k in range(8):
                c.dma("pool", wq[:, k, :], wsrc[k * 128:(k + 1) * 128, 0:1024], [], ["wq"])
            ksrc = wsrc[:, 1024:1280].rearrange("(k p) (g d) -> p k g d", p=128, d=64)
            for k in range(8):
                c.dma("pool", [wk[:, k, :, 0, :], wk[:, k, :, 1, :]], [ksrc[:, k], ksrc[:, k]], [], ["wk"])
            c.dma("pool", wv[:], wsrc[:, 1280:1536].rearrange("(k p) f -> p k f", p=128), [], ["wv"])
            c.dma("sp", cosT[:], k_cos[:, :], [], ["cos"])
            c.dma("sp", sinT[:], k_sin[:, :], [], ["sin"])
            c.dma("pool", maskp[:], k_maskp[:, :], [], ["maskp"])
            c.dma("pool", maskn[:], k_maskn[:, :], [], ["maskn"])
            c.dma("sp", esink[:], sink_a[jj].partition_broadcast(128), [], ["esink"])
            c.op("act", ["esink"], ["esink"], lambda a: a.activation(out=esink[:], in_=esink[:], func=AF.Exp))
            c.op("pool", [], [("VA", t) for t in range(NT)], lambda g: g.memset(VA[:, :, :, 64:65], 1.0))
            wkv = wk[:].rearrange("p k g r d -> p k (g r d)")
            qkv_project(ph, rots, l, b, last, wq, "wq", 8, wkv, "wk", 4, wv, "wv", 256, QT, KT, VA, cosT, sinT)

            if stop_after is not None and stop_after[0] == "qkv":
                return
            spsx = PsRot(c, [0, 2, 6])
            spsy = PsRot(c, [1, 3, 7])
            ops_ = PsRot(c, [4, 5])
            pT_rot = Rot(ph, "pT", 4, [128, 512], BF16)
            ot_rot = Rot(ph, "ot", 2, [128, 16, 64], BF16)
            dn_rot = Rot(ph, "dn", 2, [128, 8], F32)
            qtiles = list(range(NTL)) + ([] if last else [16, 17])
            pcol = {0: 0, 2: 128, 1: 256, 3: 384}
            LA = 2
            its = []
            for qt in qtiles:
                if qt < NTL:
                    kts = ([(qt - 1, maskp, "maskp")] if qt > 0 else []) + [(qt, None, None)] + \
                          ([(qt + 1, maskn, "maskn")] if qt < NTL - 1 else []) + [(16, None, None), (17, None, None)]
                else:
                    kts = [(16, None, None), (17, None, None)]
                for g in range(4):
                    for ki, (kt, msk, mskk) in enumerate(kts):
                        its.append((qt, g, ki, len(kts), kt, msk, mskk))
            state = {}

            def emit_s(it):
                qt, g, ki, nk, kt, msk, mskk = it
                sx, sxk = spsx.next()
                sy, syk = spsy.next()

                def e_s(pe):
                    if msk is not None:
                        pe.matmul(sx[:, 0:256], identb[:], msk[:, 0:256], start=True, stop=False, skip_group_check=True)
                        pe.matmul(sy[:, 0:256], identb[:], msk[:, 0:256], start=True, stop=False, skip_group_check=True)
                    for hd in range(4):
                        cch = 2 * g + hd // 2
                        base = 64 * (hd % 2)
                        dst = sx if hd % 2 == 0 else sy
                        o = 128 * (hd // 2)
                        ins = pe.matmul(dst[:, o:o + 128],
                                        KT[base:base + 64, g, kt * 128:(kt + 1) * 128],
                                        QT[base:base + 64, cch, qt * 128:(qt + 1) * 128],
                                        start=(msk is None and hd < 2), stop=(hd >= 2), skip_group_check=True)
                    return ins
                rk = [("KT", g, (kt // 4) * 4 if kt < 16 else 16), ("QT", 2 * g, (qt // 4) * 4 if qt < 16 else 16),
                      ("QT", 2 * g + 1, (qt // 4) * 4 if qt < 16 else 16), "identb"] + ([mskk] if mskk else [])
                c.op("pe", rk, [sxk, syk], e_s)
                pT, pTk = pT_rot.next()
                bx = int(sxk[2:])
                sxy = c.psall[:, bx * 512:(bx + 2) * 512].rearrange("p (m n) -> p m n", m=2)
                c.op("act", [sxk, syk], [(pTk, 0), (pTk, 1)],
                     lambda a: a.activation(out=pT[:].rearrange("p (m n) -> p m n", m=2), in_=sxy[:, :, 0:256], func=AF.Exp, scale=SCALE))
                return (pT, pTk)

            def emit_pv(it, pt):
                qt, g, ki, nk, kt, msk, mskk = it
                pT, pTk = pt
                if g == 0 and ki == 0:
                    state["ot"] = ot_rot.next()
                if ki == 0:
                    state["op"] = ops_.next()
                ot, otk = state["ot"]
                opst, opsk = state["op"]

                def e_o(pe):
                    for hd in range(4):
                        ins = pe.matmul(opst[:, hd * 128:hd * 128 + 65], pT[:, pcol[hd]:pcol[hd] + 128],
                                        VA[:, kt, g, :], start=(ki == 0 and hd == 0), stop=(ki == nk - 1),
                                        skip_group_check=True)
                    return ins
                c.op("pe", [(pTk, 0), (pTk, 1), ("VA", kt)], [opsk], e_o)
                if ki != nk - 1:
                    return
                dn, dnk = dn_rot.next()
                ov = opst[:, :].rearrange("p (h d) -> p h d", d=128)
                c.op("dve", [opsk, "esink"], [dnk],
                     lambda v: v.tensor_tensor(out=dn[:, 0:4], in0=ov[:, :, 64], in1=esink[:, 4 * g:4 * g + 4], op=ALU.add))
                c.op("dve", [dnk], [dnk], lambda v: v.reciprocal(out=dn[:, 4:8], in_=dn[:, 0:4]))
                c.op("dve", [opsk, dnk], [otk],
                     lambda v: v.tensor_tensor(
                         out=ot[:, 4 * g:4 * g + 4, :], in0=ov[:, :, 0:64],
                         in1=dn[:, 4:8].unsqueeze(2).to_broadcast([128, 4, 64]), op=ALU.mult))
                if g == 3:
                    c.dma("pool", osc[b, qt * 128:(qt + 1) * 128, :], ot[:].rearrange("p h d -> p (h d)"), [otk], [("osc", b, qt)])

            pend = []
            for idx in range(len(its) + LA):
                if idx < len(its):
                    pend.append(emit_s(its[idx]))
                if idx - LA >= 0:
                    emit_pv(its[idx - LA], pend[idx - LA])

    def phase_attn_B(l, b, last, hh):
        jj = l // 2
        lam_init = 0.8 - 0.6 * math.exp(-0.3 * l)
        with c.phase(f"attnB_{l}_{b}_{hh}") as ph:
            rots = std_rots(ph)
            wq = ph.sbuf("wq", [128, 8, 512], BF16)
            wk = ph.sbuf("wk", [128, 8, 512], BF16)
            wv = ph.sbuf("wv", [128, 8, 512], BF16)
            cosT = ph.sbuf("cosT", [128, S], F32)
            sinT = ph.sbuf("sinT", [128, S], F32)
            QT = ph.sbuf("QT", [128, 4, TOK], BF16)
            KT = ph.sbuf("KT", [128, 4, TOK], BF16)
            VA = ph.sbuf("VA", [128, NT, 4, 129], BF16)
            lamt = ph.sbuf("lamt", [128, 4, 64], F32)
            lsm = ph.sbuf("lsm", [128, 8], F32)
            gsub = ph.sbuf("gsub", [128, 128], F32)
            wsrc = wqkv_b[jj]
            for wt, key, c0 in ((wq, "wq", hh * 512), (wk, "wk", 1024 + hh * 512), (wv, "wv", 2048 + hh * 512)):
                for k in range(8):
                    c.dma("pool", wt[:, k, :], wsrc[k * 128:(k + 1) * 128, c0:c0 + 512], [], [key])
            c.dma("sp", cosT[:], k_cos[:, :], [], ["cos"])
            c.dma("sp", sinT[:], k_sin[:, :], [], ["sin"])
            c.dma("sp", lamt[:].rearrange("p a d -> p (a d)"),
                  lambda_b[jj].rearrange("a d -> (a d)").partition_broadcast(128), [], ["lamt"])
            c.dma("sp", gsub[:], subln_b[jj].partition_broadcast(128), [], ["gsub"])
            c.op("dve", ["lamt"], ["lamt"], lambda v: v.tensor_tensor(out=lamt[:, 0, :], in0=lamt[:, 0, :], in1=lamt[:, 1, :], op=ALU.mult))
            c.op("dve", ["lamt"], ["lamt"], lambda v: v.tensor_tensor(out=lamt[:, 2, :], in0=lamt[:, 2, :], in1=lamt[:, 3, :], op=ALU.mult))
            c.op("dve", ["lamt"], ["lsm"], lambda v: v.reduce_sum(out=lsm[:, 0:1], in_=lamt[:, 0, :], axis=AX.X))
            c.op("dve", ["lamt"], ["lsm"], lambda v: v.reduce_sum(out=lsm[:, 1:2], in_=lamt[:, 2, :], axis=AX.X))
            c.op("act", ["lsm"], ["lsm"], lambda a: a.activation(out=lsm[:, 2:4], in_=lsm[:, 0:2], func=AF.Exp))
            c.op("dve", ["lsm"], ["lsm"], lambda v: v.tensor_tensor(out=lsm[:, 4:5], in0=lsm[:, 3:4], in1=lsm[:, 2:3], op=ALU.subtract))
            c.op("dve", ["lsm"], ["lsm"], lambda v: v.tensor_scalar_add(out=lsm[:, 4:5], in0=lsm[:, 4:5], scalar1=-lam_init))
            c.op("dve", ["gsub"], ["gsub"], lambda v: v.tensor_scalar_mul(out=gsub[:], in0=gsub[:], scalar1=(1.0 - lam_init)))
            c.op("pool", [], [("VA", t) for t in range(NT)], lambda g: g.memset(VA[:, :, :, 128:129], 1.0))
            qkv_project(ph, rots, l, b, last, wq, "wq", 4, wk, "wk", 4, wv, "wv", 512, QT, KT, VA, cosT, sinT)

            spsx = PsRot(c, [0, 2])
            spsy = PsRot(c, [1, 3])
            pT_rot = Rot(ph, "pT", 4, [128, 2, 512], BF16)
            ot_rot = Rot(ph, "ot", 2, [128, 4, 512], BF16)
            sm_rot = Rot(ph, "sm", 2, [128, 16], F32)
            of_rot = Rot(ph, "of", 2, [128, 4, 128], F32)
            jk_rot = Rot(ph, "jk", 2, [128, 128], F32)
            qgroups = [list(range(g * 4, g * 4 + 4)) for g in range(4)] + ([] if last else [[16, 17]])
            LA = 2
            for qg in qgroups:
                nq = len(qg)
                Tq = nq * 128
                q0 = qg[0] * 128
                kts = list(range(NT)) if qg[0] < NTL else [16, 17]
                nk = len(kts)
                ot, otk = ot_rot.next()
                gq = qg[0]
                its = [(h, ki, kt) for h in range(4) for ki, kt in enumerate(kts)]
                pend = []

                def emit_s(it):
                    h, ki, kt = it
                    sx, sxk = spsx.next()
                    sy, syk = spsy.next()

                    def e_s(pe):
                        pe.matmul(sx[:, 0:Tq], KT[0:64, h, kt * 128:(kt + 1) * 128], QT[0:64, h, q0:q0 + Tq], start=True, stop=True)
                        return pe.matmul(sy[:, 0:Tq], KT[64:128, h, kt * 128:(kt + 1) * 128], QT[64:128, h, q0:q0 + Tq], start=True, stop=True)
                    c.op("pe", [("KT", h, (kt // 4) * 4 if kt < 16 else 16), ("QT", h, gq)], [sxk, syk], e_s)
                    pT, pTk = pT_rot.next()
                    bx = int(sxk[2:])
                    sxy = c.psall[:, bx * 512:(bx + 2) * 512].rearrange("p (m n) -> p m n", m=2)
                    c.op("act", [sxk, syk], [(pTk, 0), (pTk, 1)],
                         lambda a: a.activation(out=pT[:, :, 0:Tq], in_=sxy[:, :, 0:Tq], func=AF.Exp, scale=SCALE))
                    return (pT, pTk)

                def emit_pv(it, pt):
                    h, ki, kt = it
                    pT, pTk = pt

                    def e_o(pe):
                        for qi in range(nq):
                            for m in range(2):
                                ins = pe.matmul(ps[4 + qi][:, m * 256:m * 256 + 129], pT[:, m, qi * 128:(qi + 1) * 128], VA[:, kt, h, :],
                                                start=(ki == 0 and m == 0), stop=(ki == nk - 1), skip_group_check=True)
                        return ins
                    c.op("pe", [(pTk, 0), (pTk, 1), ("VA", kt)], [f"ps{4 + qi}" for qi in range(nq)], e_o)
                    if ki != nk - 1:
                        return
                    sm, smk = sm_rot.next()
                    of, ofk = of_rot.next()
                    for qi in range(nq):
                        pk = f"ps{4 + qi}"
                        pb = ps[4 + qi]
                        c.op("dve", [pk], [smk], lambda v, qi=qi, pb=pb: v.reciprocal(out=sm[:, qi:qi + 1], in_=pb[:, 128:129]))
                        c.op("dve", [pk, smk], [smk], lambda v, qi=qi, pb=pb: v.reciprocal(out=sm[:, 4 + qi:5 + qi], in_=pb[:, 384:385]))
                        c.op("dve", ["lsm", smk], [smk],
                             lambda v, qi=qi: v.tensor_scalar_mul(out=sm[:, 4 + qi:5 + qi], in0=sm[:, 4 + qi:5 + qi], scalar1=lsm[:, 4:5]))
                        c.op("dve", [pk, smk], [ofk],
                             lambda v, qi=qi, pb=pb: v.tensor_scalar_mul(out=of[:, qi, :], in0=pb[:, 0:128], scalar1=sm[:, qi:qi + 1]))
                        c.op("dve", [pk, smk, ofk], [ofk],
                             lambda v, qi=qi, pb=pb: v.scalar_tensor_tensor(out=of[:, qi, :], in0=pb[:, 256:384], scalar=sm[:, 4 + qi:5 + qi], in1=of[:, qi, :], op0=ALU.mult, op1=ALU.add))
                        jk, jkk = jk_rot.next()
                        c.op("act", [ofk], [jkk, smk],
                             lambda a, qi=qi, jk=jk: a.activation(out=jk[:], in_=of[:, qi, :], func=AF.Square, accum_out=sm[:, 8 + qi:9 + qi]))
                    c.op("dve", [smk], [smk], lambda v: v.tensor_scalar(out=sm[:, 8:8 + nq], in0=sm[:, 8:8 + nq], scalar1=1.0 / 128.0, scalar2=SUBLN_EPS, op0=ALU.mult, op1=ALU.add))
                    c.op("act", [smk], [smk], lambda a: a.activation(out=sm[:, 12:12 + nq], in_=sm[:, 8:8 + nq], func=AF.Ln))
                    c.op("act", [smk], [smk], lambda a: a.activation(out=sm[:, 12:12 + nq], in_=sm[:, 12:12 + nq], func=AF.Exp, scale=-0.5))
                    for qi in range(nq):
                        c.op("dve", [ofk, smk, "gsub"], [otk],
                             lambda v, qi=qi: v.scalar_tensor_tensor(
                                 out=ot[:, qi, h * 128:(h + 1) * 128], in0=of[:, qi, :], scalar=sm[:, 12 + qi:13 + qi], in1=gsub[:], op0=ALU.mult, op1=ALU.mult))

                for idx in range(len(its) + LA):
                    if idx < len(its):
                        pend.append(emit_s(its[idx]))
                    if idx - LA >= 0:
                        emit_pv(its[idx - LA], pend[idx - LA])
                for qi, t in enumerate(qg):
                    c.dma("pool", osc[b, t * 128:(t + 1) * 128, hh * 512:(hh + 1) * 512], ot[:, qi, :], [otk], [("osc", b, t, hh)])

    def phase_oproj(l, b, last):
        typ_a = (l % 2 == 0)
        jj = l // 2
        with c.phase(f"oproj_{l}_{b}") as ph:
            wo = ph.sbuf("wo", [128, 8, D], BF16)
            gb1 = ph.sbuf("gb1", [128, 2, D], F32)
            g1l = ph.sbuf("g1l", [128, D], F32)
            g1c = ph.sbuf("g1c", [128, D], F32)
            wsrc = (wo_a if typ_a else wo_b)[jj]
            for k in range(8):
                c.dma("pool", wo[:, k, :], wsrc[k * 128:(k + 1) * 128, :], [], ["wo"])
            c.dma("sp", [gb1[:, 0, :], gb1[:, 1, :]],
                  [ln_attn_g[l].partition_broadcast(128), ln_attn_b[l].partition_broadcast(128)], [], ["gb1"])
            c.dma("sp", g1l[:], gsc[l, b, 0], [("gsc", l, b, 0, 0), ("gsc", l, b, 0, 1)], ["g1l"])
            c.dma("sp", g1c[:], gsc[l, 2, 0], [("gsc", l, 2, 0, 0), ("gsc", l, 2, 0, 1)], ["g1c"])
            rots = {"stats": Rot(ph, "st", 4, [128, 16], F32), "xn": Rot(ph, "xn", 2, [128, D], F32),
                    "tps": PsRot(c, [0, 1])}
            ob_rot = Rot(ph, "ob", 3, [128, D], BF16)
            xf_rot = Rot(ph, "xf", 4, [128, D], F32)
            oT_rot = Rot(ph, "oT", 2, [128, 8, 128], BF16)
            r_rot = Rot(ph, "r", 3, [128, D], F32)
            x1_rot = Rot(ph, "x1", 2, [128, D], F32)
            xnb_rot = Rot(ph, "xnb", 5, [128, D], BF16)
            sg_rot = Rot(ph, "sg", 2, [128, 64], F32)
            alps = PsRot(c, [2, 3, 4, 5])
            rps = PsRot(c, [6, 7])
            tiles = list(range(NTL)) + ([] if last else [16, 17])
            loads = {}
            st1 = {}
            st1a = {}
            fr = {}

            def load(t):
                ob, obk = ob_rot.next()
                c.dma("sp", ob[:], osc[b, t * 128:(t + 1) * 128, :], [], [obk])
                xf, xfk = xf_rot.next()
                ap, dk = xin_ap(l, b, t)
                c.dma("sp", xf[:], ap, [dk], [xfk])
                loads[t] = (ob, obk, xf, xfk)

            def stage1(t):
                g1 = g1l if t < NTL else g1c
                g1k = "g1l" if t < NTL else "g1c"
                ob, obk, xf, xfk = loads.pop(t)
                pst, psk = rots["tps"].next()
                pv = pst[:].bitcast(BF16)

                def e_t(pe):
                    for j in range(8):
                        ins = pe.transpose(out=pv[:, j * 128:(j + 1) * 128], in_=ob[:, j * 128:(j + 1) * 128], identity=identb[:])
                    return ins
                c.op("pe", [obk, "identb"], [psk], e_t)
                oT, oTk = oT_rot.next()
                c.op("act", [psk], [oTk], lambda a: a.copy(out=oT[:].rearrange("p j t -> p (j t)"), in_=pv[:, :]))
                aps = []
                for half in range(2):
                    apst, apsk = alps.next()

                    def e_a(pe, apst=apst, half=half):
                        for k in range(8):
                            ins = pe.matmul(apst[:, :], oT[:, k, :], wo[:, k, half * 512:(half + 1) * 512], start=(k == 0), stop=(k == 7))
                        return ins
                    c.op("pe", [oTk, "wo"], [apsk], e_a)
                    aps.append((apst, apsk))
                fr[t] = (aps, xf, xfk, g1, g1k)

            def stage1back(t):
                aps, xf, xfk, g1, g1k = fr.pop(t)
                r, rk = r_rot.next()
                for half in range(2):
                    apst, apsk = aps[half]
                    c.op("dve", [apsk, g1k], [(rk, half)],
                         lambda v, apst=apst, half=half: v.tensor_tensor(out=r[:, half * 512:(half + 1) * 512], in0=apst[:, :], in1=g1[:, half * 512:(half + 1) * 512], op=ALU.mult))
                c.op("dve", [(rk, 0), (rk, 1), xfk], [rk],
                     lambda g: g.scalar_tensor_tensor(out=r[:], in0=xf[:], scalar=ALPHA, in1=r[:], op0=ALU.mult, op1=ALU.add))
                st1a[t] = (ln_stats(rots, r, rk), r, rk)

            def stage1b(t):
                stt, r, rk = st1a.pop(t)
                x1, x1k = x1_rot.next()
                xnb, xnbk = xnb_rot.next()
                ln_apply(rots, stt, r, rk, gb1, "gb1", x1, x1k, xnb, xnbk)
                c.dma("pool", xb[b, t * 128:(t + 1) * 128, :], x1[:], [x1k], [("xb", b, t)])
                st1[t] = (xnb, xnbk)

            h2ps = PsRot(c, [6, 7])

            def stage2(t0):
                cls = cls_of(b, t0)
                pair = [st1.pop(t0), st1.pop(t0 + 1)]
                for jh in range(2):
                    pst, psk = h2ps.next()
                    pv = pst[:].bitcast(BF16)

                    def e_t2(pe):
                        for jj_ in range(4):
                            j = jh * 4 + jj_
                            for i in range(2):
                                ins = pe.transpose(out=pv[:, jj_ * 256 + i * 128:jj_ * 256 + (i + 1) * 128],
                                                   in_=pair[i][0][:, j * 128:(j + 1) * 128], identity=identb[:])
                        return ins
                    c.op("pe", [pair[0][1], pair[1][1], "identb"], [psk], e_t2)
                    for jj_ in range(4):
                        j = jh * 4 + jj_
                        c.op("act", [psk, "modS"], [("h2T", t0, j)],
                             lambda a, j=j, jj_=jj_, pv=pv: a.activation(
                                 out=h2T[:, j, t0 * 128:(t0 + 2) * 128], in_=pv[:, jj_ * 256:(jj_ + 1) * 256], func=AF.Identity,
                                 bias=mod_ap(l, 3, j, cls), scale=mod_ap(l, 2, j, cls)))

            n = len(tiles)
            for i in range(min(2, n)):
                load(tiles[i])
            for i in range(n + 1):
                if i < n:
                    stage1(tiles[i])
                    if i + 2 < n:
                        load(tiles[i + 2])
                if i >= 1:
                    stage1back(tiles[i - 1])
                    stage1b(tiles[i - 1])
                    if (i - 1) % 2 == 1 and i - 1 >= 3:
                        stage2(tiles[i - 4])
            for t0 in sorted(k for k in list(st1.keys()) if k % 2 == 0):
                stage2(t0)
            nt = n
            rp = ps[2]

            def e_r(pe):
                for ti, t in enumerate(tiles):
                    for k in range(8):
                        ins = pe.matmul(rp[:, ti * E:(ti + 1) * E], h2T[:, k, t * 128:(t + 1) * 128], wrb[:, k, :],
                                        start=(ti == 0 and k == 0), stop=(k == 7), skip_group_check=True)
                return ins
            c.op("pe", [("h2T", t0, j) for t0 in tiles[::2] for j in range(8)] + ["wrb"], ["ps2"], e_r)
            router_topk(ph, rp, "ps2", tiles)
            if debug:
                c.dma("sp", dbg_gates[b].rearrange("(t p) e -> p t e", p=128), gates[:], [("gates", t) for t in tiles], [("dbg_gates", b)], is_output=True)

    def router_topk(ph, rp, rpk, tiles):
        G = len(tiles)
        W = G * E
        sgs = ph.sbuf("rt_s", [128, NT * E], F32)
        sgb = ph.sbuf("rt_b", [128, NT * E], F32)
        scr = ph.sbuf("rt_x", [128, 8, NT * 4], F32)
        red = ph.sbuf("rt_r", [128, 2, NT], F32)
        k = "rt"
        s_ = sgs[:, 0:W]
        b_all = sgb[:, 0:W]
        Bv = b_all.rearrange("p (g i) -> p g i", i=4)
        n4 = G * 4

        def sc(i):
            return scr[:, i, 0:n4]
        c.op("act", [rpk], [k], lambda a: a.activation(out=s_, in_=rp[:, 0:W], func=AF.Exp, scale=-1.0))
        c.op("dve", [k], [k], lambda v: v.tensor_scalar_add(out=s_, in0=s_, scalar1=1.0))
        c.op("dve", [k], [k], lambda v: v.reciprocal(out=s_, in_=s_))
        c.op("dve", [k, "rbias"], [k], lambda v: v.tensor_tensor(
            out=b_all.rearrange("p (t e) -> p t e", e=E), in0=s_.rearrange("p (t e) -> p t e", e=E),
            in1=rbias[:].unsqueeze(1).to_broadcast([128, G, E]), op=ALU.add))
        a0, a1, a2, a3 = (Bv[:, :, i] for i in range(4))
        P_, Q_, R_, S_, M1, M2, GS = sc(0), sc(1), sc(2), sc(3), sc(4), sc(5), sc(6)
        steps = [
            (P_, a0, a1, ALU.max), (Q_, a0, a1, ALU.min), (R_, a2, a3, ALU.max), (S_, a2, a3, ALU.min),
            (M1, P_, R_, ALU.max),
            (M2, P_, R_, ALU.min),
            (Q_, Q_, S_, ALU.max),
            (M2, M2, Q_, ALU.max),
            (GS, M1, M2, ALU.add),
        ]
        for (o, i0, i1, op_) in steps:
            c.op("dve", [k], [k], lambda v, o=o, i0=i0, i1=i1, op_=op_: v.tensor_tensor(out=o, in0=i0, in1=i1, op=op_))
        gs3 = GS.rearrange("p (t g) -> p t g", g=4)
        c.op("dve", [k], [k], lambda v: v.tensor_reduce(out=red[:, 0, 0:G], in_=gs3, axis=AX.X, op=ALU.max))
        c.op("dve", [k], [k], lambda v: v.tensor_tensor(out=gs3, in0=gs3, in1=red[:, 0, 0:G].unsqueeze(2).to_broadcast([128, G, 4]), op=ALU.is_ge))
        c.op("dve", [k], [k], lambda v: v.tensor_tensor(out=Bv, in0=Bv, in1=M2.unsqueeze(2).to_broadcast([128, n4, 4]), op=ALU.is_ge))
        c.op("dve", [k], [k], lambda v: v.tensor_tensor(out=Bv, in0=Bv, in1=GS.unsqueeze(2).to_broadcast([128, n4, 4]), op=ALU.mult))
        c.op("dve", [k], [k], lambda v: v.tensor_tensor(out=b_all, in0=b_all, in1=s_, op=ALU.mult))
        b3 = b_all.rearrange("p (t e) -> p t e", e=E)
        c.op("dve", [k], [k], lambda v: v.tensor_reduce(out=red[:, 0, 0:G], in_=b3, axis=AX.X, op=ALU.add))
        c.op("dve", [k], [k], lambda v: v.reciprocal(out=red[:, 1, 0:G], in_=red[:, 0, 0:G]))
        t0 = tiles[0]
        c.op("dve", [k], [("gates", t) for t in tiles],
             lambda v: v.tensor_tensor(out=gates[:, t0:t0 + G, :], in0=b3,
                                       in1=red[:, 1, 0:G].unsqueeze(2).to_broadcast([128, G, E]), op=ALU.mult))

    def phase_moe(l, b, last):
        ntile = NTL if last else NT
        ntok = ntile * 128
        with c.phase() as pho:
            acc = pho.sbuf("acc", [128, NT, D], F32)
            with c.phase(f"moe_{l}_{b}") as ph:
                wg_rot = Rot(ph, "wg", 2, [128, 8, DE], BF16)
                wu_rot = Rot(ph, "wu", 2, [128, 8, DE], BF16)
                wd_rot = Rot(ph, "wd", 2, [128, 4, D], BF16)
                sg_rot = Rot(ph, "sgl", 2, [128, 512], BF16)
                act_rot = Rot(ph, "act", 2, [128, 4, 512], BF16)
                gps = PsRot(c, [0, 1])
                ups = PsRot(c, [2, 3])
                yps = PsRot(c, [4, 5, 6, 7])
                ttiles = []
                t0 = 0
                while t0 < ntile:
                    n = min(4, ntile - t0)
                    ttiles.append((t0, n))
                    t0 += n
                for e in range(E):
                    wg, wgk = wg_rot.next()
                    wu, wuk = wu_rot.next()
                    wd, wdk = wd_rot.next()
                    c.dma("pool", wg[:], w_gate[l, e].rearrange("(k p) f -> p k f", p=128), [], [wgk])
                    c.dma("pool", wu[:], w_up[l, e].rearrange("(k p) f -> p k f", p=128), [], [wuk])
                    c.dma("pool", wd[:], w_down[l, e].rearrange("(k p) f -> p k f", p=128), [], [wdk])
                    for (t0, n) in ttiles:
                        T = n * 128
                        c0 = t0 * 128
                        hk = [("h2T", t) for t in range(t0, t0 + n)]
                        at, atk = act_rot.next()
                        for fc in range(4):
                            gp, gpk = gps.next()
                            up, upk = ups.next()

                            def e_g(pe, gp=gp, wg=wg, fc=fc, c0=c0, T=T):
                                for k in range(8):
                                    ins = pe.matmul(gp[:, 0:T], wg[:, k, fc * 128:(fc + 1) * 128], h2T[:, k, c0:c0 + T], start=(k == 0), stop=(k == 7))
                                return ins

                            def e_u(pe, up=up, wu=wu, fc=fc, c0=c0, T=T):
                                for k in range(8):
                                    ins = pe.matmul(up[:, 0:T], wu[:, k, fc * 128:(fc + 1) * 128], h2T[:, k, c0:c0 + T], start=(k == 0), stop=(k == 7))
                                return ins
                            c.op("pe", [wgk] + hk, [gpk], e_g)
                            c.op("pe", [wuk] + hk, [upk], e_u)
                            sgl, sglk = sg_rot.next()
                            c.op("act", [gpk], [sglk], lambda a, gp=gp, sgl=sgl, T=T: a.activation(out=sgl[:, 0:T], in_=gp[:, 0:T], func=AF.Silu))
                            c.op("dve", [upk, sglk], [(atk, fc)],
                                 lambda v, up=up, sgl=sgl, at=at, fc=fc, T=T: v.tensor_tensor(out=at[:, fc, 0:T], in0=up[:, 0:T], in1=sgl[:, 0:T], op=ALU.mult))
                        for si in range(n):
                            t = t0 + si
                            for half in range(2):
                                yp, ypk = yps.next()

                                def e_y(pe, yp=yp, at=at, si=si, half=half, wd=wd):
                                    for fc in range(4):
                                        ins = pe.matmul(yp[:, :], at[:, fc, si * 128:(si + 1) * 128], wd[:, fc, half * 512:(half + 1) * 512], start=(fc == 0), stop=(fc == 3))
                                    return ins
                                c.op("pe", [(atk, fc) for fc in range(4)] + [wdk], [ypk], e_y)
                                av = acc[:, t, half * 512:(half + 1) * 512]
                                if e == 0:
                                    c.op("dve", [ypk, ("gates", t)], [("acc", t, half)],
                                         lambda v, yp=yp, av=av, t=t, e=e: v.tensor_scalar_mul(out=av, in0=yp[:, :], scalar1=gates[:, t, e:e + 1]))
                                else:
                                    c.op("dve", [ypk, ("gates", t)], [("acc", t, half)],
                                         lambda v, yp=yp, av=av, t=t, e=e: v.scalar_tensor_tensor(out=av, in0=yp[:, :], scalar=gates[:, t, e:e + 1], in1=av, op0=ALU.mult, op1=ALU.add))
            with c.phase(f"ln2_{l}_{b}") as ph:
                gb2 = ph.sbuf("gb2", [128, 2, D], F32)
                g2l = ph.sbuf("g2l", [128, D], F32)
                g2c = ph.sbuf("g2c", [128, D], F32)
                c.dma("sp", [gb2[:, 0, :], gb2[:, 1, :]],
                      [ln_ffn_g[l].partition_broadcast(128), ln_ffn_b[l].partition_broadcast(128)], [], ["gb2"])
                c.dma("sp", g2l[:], gsc[l, b, 1], [], ["g2l"])
                c.dma("sp", g2c[:], gsc[l, 2, 1], [], ["g2c"])
                rots = {"stats": Rot(ph, "st", 4, [128, 16], F32), "xn": Rot(ph, "xn", 2, [128, D], F32)}
                xf_rot = Rot(ph, "xf", 3, [128, D], F32)
                x2_rot = Rot(ph, "x2", 2, [128, D], F32)
                lds = {}

                def load2(t):
                    xf, xfk = xf_rot.next()
                    c.dma("sp", xf[:], xb[b, t * 128:(t + 1) * 128, :], [], [xfk])
                    lds[t] = (xf, xfk)
                for t in range(min(2, ntile)):
                    load2(t)
                pend2 = {}

                def s2a(t):
                    g2 = g2l if t < NTL else g2c
                    g2k = "g2l" if t < NTL else "g2c"
                    xf, xfk = lds.pop(t)
                    rk = ("accr", t)
                    c.op("dve", [g2k], [rk], lambda v: v.tensor_tensor(out=acc[:, t, :], in0=acc[:, t, :], in1=g2[:], op=ALU.mult))
                    c.op("dve", [xfk], [rk], lambda g: g.scalar_tensor_tensor(out=acc[:, t, :], in0=xf[:], scalar=ALPHA, in1=acc[:, t, :], op0=ALU.mult, op1=ALU.add))
                    if t + 2 < ntile:
                        load2(t + 2)
                    pend2[t] = (ln_stats(rots, acc[:, t, :], rk), rk)

                def s2b(t):
                    stt, rk = pend2.pop(t)
                    x2, x2k = x2_rot.next()
                    ln_apply(rots, stt, acc[:, t, :], rk, gb2, "gb2", x2, x2k)
                    if last:
                        c.dma("pool", out[b, t * 128:(t + 1) * 128, :], x2[:], [x2k], [("out", b, t)], is_output=True)
                    else:
                        c.dma("pool", xa[b, t * 128:(t + 1) * 128, :], x2[:], [x2k], [("xa", b, t)], is_output=debug)

                for t in range(ntile + 1):
                    if t < ntile:
                        s2a(t)
                    if t >= 1:
                        s2b(t - 1)

    ada_prologue()
    done = stop_after is not None and stop_after[0] == "ada"
    if done:
        n_layers = 0
    for l in range(n_layers):
        last = (l == DEPTH - 1)
        for b in range(NB):
            if l % 2 == 0:
                phase_attn_A(l, b, last)
            else:
                for hh in range(2):
                    phase_attn_B(l, b, last, hh)
            if stop_after is not None and stop_after[0] in ("attn", "qkv") and stop_after[1:] == (l, b):
                done = True
                break
            phase_oproj(l, b, last)
            if stop_after == ("oproj", l, b):
                done = True
                break
            phase_moe(l, b, last)
            if stop_after == ("moe", l, b):
                done = True
                break
        if done:
            break
    c.finish()
    return nc


def _consts():
    ident = np.eye(128, dtype=np.float32)
    R = np.zeros((64, 64), np.float32)
    for d in range(64):
        if (d % 32) < 16:
            R[d, d + 16] = -1.0
        else:
            R[d, d - 16] = 1.0
    RT = np.zeros((128, 128), np.float32)
    RT[:64, :64] = R.T
    RT[64:, 64:] = R.T
    t = np.arange(S)
    rows = (t // 64).astype(np.float64)
    cols = (t % 64).astype(np.float64)
    inv = 10000.0 ** (-np.arange(16, dtype=np.float64) / 16.0)
    cos = np.zeros((128, S), np.float32)
    sin = np.zeros((128, S), np.float32)
    for p in range(128):
        d = p % 64
        pos = rows if d < 32 else cols
        ang = pos * np.float64(np.float32(inv[d % 16]))
        ang = (pos.astype(np.float32) * np.float32(inv[d % 16])).astype(np.float64)
        cos[p] = np.cos(ang)
        sin[p] = np.sin(ang)
    kk = np.arange(128)[:, None]
    qq = np.arange(128)[None, :]
    mp = np.where(kk >= qq, 0.0, NEG).astype(np.float32)
    mn = np.where(kk <= qq, 0.0, NEG).astype(np.float32)
    return {"k_ident": ident, "k_rt": RT, "k_cos": cos, "k_sin": sin,
            "k_maskp": np.tile(mp, (1, 4)), "k_maskn": np.tile(mn, (1, 4))}


_NC_CACHE = {}


def make_in_maps(inputs):
    consts = _consts()
    shared = {k: np.ascontiguousarray(inputs[k], dtype=np.float32) for k in (
        "w_ada", "b_ada", "wqkv_a", "wo_a", "sink_a", "wqkv_b", "wo_b", "lambda_b", "subln_b",
        "ln_attn_g", "ln_attn_b", "ln_ffn_g", "ln_ffn_b", "w_router", "router_bias", "w_gate", "w_up", "w_down")}
    shared.update(consts)
    in_maps = []
    for i in range(NCORES):
        m = dict(shared)
        m["x"] = np.ascontiguousarray(inputs["x"][NB * i:NB * (i + 1)], dtype=np.float32)
        m["ctx"] = np.ascontiguousarray(inputs["ctx"][NB * i:NB * (i + 1)], dtype=np.float32)
        m["c3"] = np.ascontiguousarray(
            np.concatenate([inputs["c"][NB * i:NB * (i + 1)], inputs["c_ctx"][None, :]], axis=0), dtype=np.float32)
        in_maps.append(m)
    return in_maps


def kernel(**inputs):
    if "nc" not in _NC_CACHE:
        _NC_CACHE["nc"] = build()
    nc = _NC_CACHE["nc"]
    in_maps = make_in_maps(inputs)
    res = run_bass_kernel_spmd(nc, in_maps, core_ids=list(range(NCORES)))
    return np.concatenate([np.asarray(r["out"]) for r in res.results], axis=0).astype(np.float32)
```

```python
import math
from contextlib import ExitStack, contextmanager

import numpy as np
import concourse.bass as bass
import concourse.mybir as mybir
from concourse.bass_utils import run_bass_kernel_spmd

F32 = mybir.dt.float32
BF16 = mybir.dt.bfloat16
AF = mybir.ActivationFunctionType
ALU = mybir.AluOpType
AX = mybir.AxisListType

DEPTH = 4
D = 1024
S = 2048
C = 256
TOK = S + C
NT = TOK // 128
NTL = S // 128
NB = 2
NCORES = 8
E = 16
DE = 512
ALPHA = (2 * DEPTH) ** 0.25
LN_EPS = 1e-6
SUBLN_EPS = 1e-5
SCALE = 0.125
NEG = -30000.0

SEM_ROLL = 30000
SAME_ENGINE_SYNC = ("act", "dve", "pool")
import os
DBG_CUT = int(os.environ.get("DBG_CUT", "9"))
SCOPES = bool(int(os.environ.get("DBG_SCOPES", "0")))
N_DMA_SLOTS = {"sp": 16, "pool": 12}


class Ctx:
    def __init__(self, nc, same_engine_sync=SAME_ENGINE_SYNC):
        self.nc = nc
        self.es = ExitStack()
        self.eng = {"pe": nc.tensor, "act": nc.scalar, "dve": nc.vector,
                    "pool": nc.gpsimd, "sp": nc.sync}
        self.same_engine_sync = set(same_engine_sync)
        self.sem = {}
        self.cnt = {}
        self.nsem = 0
        self.uid = 0
        for e in ("pe", "act", "dve", "pool"):
            self._new_sem(e)
        self.known = {e: {} for e in self.eng}
        self.last_w = {}
        self.readers = {}
        self.dma_sems = {}
        self.dma_val = {}
        self.dma_i = {}
        for q, n in N_DMA_SLOTS.items():
            self.dma_sems[q] = [self.es.enter_context(nc.semaphore(f"dq_{q}_{i}")) for i in range(n)]
            self.dma_val[q] = [0] * n
            self.dma_i[q] = 0
        self.out_events = []
        self.psall = self.es.enter_context(nc.psum_tensor("psall", [128, 8 * 512], F32))
        self.ps = [self.psall[:, i * 512:(i + 1) * 512] for i in range(8)]

    def _new_sem(self, e):
        self.nsem += 1
        self.sem[e] = self.es.enter_context(self.nc.semaphore(f"s_{e}_{self.nsem}"))
        self.cnt[e] = 0

    def sbuf(self, name, shape, dt):
        self.uid += 1
        return self.es.enter_context(self.nc.sbuf_tensor(f"{name}_{self.uid}", list(shape), dt))

    @contextmanager
    def phase(self, name=None):
        st = ExitStack()
        ctx = self
        if name is not None and SCOPES:
            st.enter_context(self.nc.named_scope(name))

        class PH:
            def sbuf(self_, name, shape, dt):
                ctx.uid += 1
                return st.enter_context(ctx.nc.sbuf_tensor(f"{name}_{ctx.uid}", list(shape), dt))

        try:
            yield PH()
        finally:
            self.barrier()
            st.close()

    def _deps(self, reads, writes):
        evs = []
        for k in list(reads) + list(writes):
            ev = self.last_w.get(k)
            if ev is not None:
                evs.append(ev)
        for k in writes:
            evs.extend(self.readers.get(k, ()))
        return evs

    def _emit_waits(self, engine, evs):
        best = {}
        for (sem, val, src) in evs:
            if src == engine and engine not in self.same_engine_sync:
                continue
            key = id(sem)
            if self.known[engine].get(key, 0) >= val:
                continue
            if key not in best or best[key][1] < val:
                best[key] = (sem, val)
        for key, (sem, val) in best.items():
            self.eng[engine].wait_ge(sem, val)
            self.known[engine][key] = val

    def _record(self, ev, reads, writes):
        for k in writes:
            self.last_w[k] = ev
            self.readers[k] = []
        for k in reads:
            self.readers.setdefault(k, []).append(ev)

    @staticmethod
    def _excl(reads, writes):
        r = [k for k in reads if not (isinstance(k, str) and k.startswith("ps"))]
        w = list(writes) + [k for k in reads if isinstance(k, str) and k.startswith("ps")]
        return r, w

    def op(self, engine, reads, writes, emit):
        reads, writes = self._excl(reads, writes)
        self._emit_waits(engine, self._deps(reads, writes))
        ins = emit(self.eng[engine])
        if self.cnt[engine] >= SEM_ROLL:
            self._new_sem(engine)
        self.cnt[engine] += 1
        ins.then_inc(self.sem[engine], 1)
        ev = (self.sem[engine], self.cnt[engine], engine)
        self._record(ev, reads, writes)
        return ev

    def dma(self, queue, out_ap, in_ap, reads, writes, is_output=False):
        slot = self.dma_i[queue] % len(self.dma_sems[queue])
        self.dma_i[queue] += 1
        sem = self.dma_sems[queue][slot]
        evs = self._deps(reads, writes)
        if self.dma_val[queue][slot] > 0:
            evs.append((sem, self.dma_val[queue][slot], "dma"))
        self._emit_waits(queue, evs)
        outs = out_ap if isinstance(out_ap, (list, tuple)) else [out_ap]
        ins_ = in_ap if isinstance(in_ap, (list, tuple)) else [in_ap]
        for o, i in zip(outs, ins_):
            self.eng[queue].dma_start(out=o, in_=i).then_inc(sem, 16)
            self.dma_val[queue][slot] += 16
        ev = (sem, self.dma_val[queue][slot], "dma")
        self._record(ev, reads, writes)
        if is_output:
            self.out_events.append(ev)
        return ev

    def _all_events(self):
        evs = []
        for e in ("pe", "act", "dve", "pool"):
            if self.cnt[e] > 0:
                evs.append((self.sem[e], self.cnt[e], "bar"))
        for q in self.dma_sems:
            for s, v in zip(self.dma_sems[q], self.dma_val[q]):
                if v > 0:
                    evs.append((s, v, "dma"))
        return evs

    def barrier(self):
        evs = self._all_events()
        for e in ("pe", "act", "dve", "pool", "sp"):
            self._emit_waits(e, evs)
        self.last_w.clear()
        self.readers.clear()

    def finish(self):
        self._emit_waits("sp", list(self.out_events) + self._all_events())
        self.es.close()


class Rot:
    def __init__(self, alloc, name, n, shape, dt):
        self.tiles = [alloc.sbuf(f"{name}{i}", shape, dt) for i in range(n)]
        self.keys = [f"{name}#{id(self)}#{i}" for i in range(n)]
        self.i = -1

    def next(self):
        self.i = (self.i + 1) % len(self.tiles)
        return self.tiles[self.i], self.keys[self.i]


class PsRot:
    def __init__(self, c, banks):
        self.c = c
        self.banks = list(banks)
        self.i = -1

    def next(self):
        self.i = (self.i + 1) % len(self.banks)
        b = self.banks[self.i]
        return self.c.ps[b], f"ps{b}"


def build(n_layers=DEPTH, debug=False, stop_after=None):
    nc = bass.Bass("TRN2", target_bir_lowering=False)

    def din(name, shape, dt=F32):
        return nc.dram_tensor(name, list(shape), dt, kind="ExternalInput").ap()

    x = din("x", [NB, S, D])
    ctxin = din("ctx", [NB, C, D])
    c3 = din("c3", [3, D])
    w_ada = din("w_ada", [DEPTH, D, 6 * D])
    b_ada = din("b_ada", [DEPTH, 6 * D])
    wqkv_a = din("wqkv_a", [2, D, 1536])
    wo_a = din("wo_a", [2, D, D])
    sink_a = din("sink_a", [2, 16])
    wqkv_b = din("wqkv_b", [2, D, 3072])
    wo_b = din("wo_b", [2, D, D])
    lambda_b = din("lambda_b", [2, 4, 64])
    subln_b = din("subln_b", [2, 128])
    ln_attn_g = din("ln_attn_g", [DEPTH, D])
    ln_attn_b = din("ln_attn_b", [DEPTH, D])
    ln_ffn_g = din("ln_ffn_g", [DEPTH, D])
    ln_ffn_b = din("ln_ffn_b", [DEPTH, D])
    w_router = din("w_router", [D, E])
    router_bias = din("router_bias", [E])
    w_gate = din("w_gate", [DEPTH, E, D, DE])
    w_up = din("w_up", [DEPTH, E, D, DE])
    w_down = din("w_down", [DEPTH, E, DE, D])
    k_ident = din("k_ident", [128, 128])
    k_rt = din("k_rt", [128, 128])
    k_cos = din("k_cos", [128, S])
    k_sin = din("k_sin", [128, S])
    k_maskp = din("k_maskp", [128, 512])
    k_maskn = din("k_maskn", [128, 512])

    out = nc.dram_tensor("out", [NB, S, D], F32, kind="ExternalOutput").ap()
    skind = "ExternalOutput" if debug else "Internal"
    xa = nc.dram_tensor("xa", [NB, TOK, D], F32, kind=skind).ap()
    xb = nc.dram_tensor("xb", [NB, TOK, D], F32, kind=skind).ap()
    osc = nc.dram_tensor("osc", [NB, TOK, D], BF16, kind="Internal").ap()
    gsc = nc.dram_tensor("gsc", [DEPTH, 3, 2, 128, D], F32, kind="Internal").ap()
    if debug:
        dbg_gates = nc.dram_tensor("dbg_gates", [NB, TOK, E], F32, kind="ExternalOutput").ap()
        dbg_mod = nc.dram_tensor("dbg_mod", [128, DEPTH * 4 * 8 * 3], F32, kind="ExternalOutput").ap()
        dbg_o = nc.dram_tensor("dbg_o", [NB, TOK, D], F32, kind="ExternalOutput").ap()

    c = Ctx(nc)
    ps = c.ps

    identf = c.sbuf("identf", [128, 128], F32)
    identb = c.sbuf("identb", [128, 128], BF16)
    rtb = c.sbuf("rtb", [128, 128], BF16)
    modS = c.sbuf("modS", [128, DEPTH * 4 * 8 * 3], F32)
    h2T = c.sbuf("h2T", [128, 8, TOK], BF16)
    gates = c.sbuf("gates", [128, NT, E], F32)
    wrb = c.sbuf("wrb", [128, 8, E], BF16)
    rbias = c.sbuf("rbias", [128, E], F32)

    def mod_ap(l, kind, j, cls):
        o = ((l * 4 + kind) * 8 + j) * 3 + cls
        return modS[:, o:o + 1]

    c.dma("sp", identf[:], k_ident[:, :], [], ["identf"])
    c.dma("pool", identb[:], k_ident[:, :], [], ["identb"])
    c.dma("pool", rtb[:], k_rt[:, :], [], ["rtb"])
    c.dma("pool", wrb[:], w_router.rearrange("(k p) e -> p k e", p=128), [], ["wrb"])
    c.dma("sp", rbias[:], router_bias.partition_broadcast(128), [], ["rbias"])

    def ada_prologue():
        with c.phase("ada") as ph:
            c3s = ph.sbuf("c3s", [3, D], F32)
            silT = ph.sbuf("silT", [128, 8, 3], F32)
            silTb = ph.sbuf("silTb", [128, 8, 3], BF16)
            silbc = [ph.sbuf(f"silbc{i}", [128, 8, 128], BF16) for i in range(3)]
            lnrows = ph.sbuf("lnrows", [64, 128], F32)
            lnT = ph.sbuf("lnT", [128, 64], F32)
            c.dma("sp", c3s[:], c3[:, :], [], ["c3s"])
            c.dma("sp", [lnrows[0:32, :], lnrows[32:64, :]],
                  [ln_attn_g.rearrange("l (j p) -> (l j) p", p=128),
                   ln_attn_b.rearrange("l (j p) -> (l j) p", p=128)], [], ["lnrows"])

            def e_ct(pe):
                for k in range(8):
                    ins = pe.matmul(ps[0][:, k * 3:(k + 1) * 3], c3s[0:3, k * 128:(k + 1) * 128],
                                    identf[0:3, 0:3], start=(k == 0), stop=True, skip_group_check=True)
                return ins
            c.op("pe", ["c3s", "identf"], ["ps0"], e_ct)
            c.op("act", ["ps0"], ["silT"],
                 lambda a: a.activation(out=silT[:].rearrange("p k c -> p (k c)"), in_=ps[0][:, 0:24], func=AF.Silu))
            c.op("dve", ["silT"], ["silTb"], lambda v: v.tensor_copy(out=silTb[:], in_=silT[:]))
            for cls in range(3):
                c.op("dve", ["silT"], [f"silbc{cls}"],
                     lambda v, cls=cls: v.tensor_copy(out=silbc[cls][:],
                                                      in_=silT[:, :, cls:cls + 1].to_broadcast([128, 8, 128])))
            c.op("pe", ["lnrows", "identf"], ["ps1"],
                 lambda pe: pe.matmul(ps[1][:, 0:64], lnrows[0:64, :], identf[0:64, 0:64], start=True, stop=True))
            c.op("dve", ["ps1"], ["lnT"], lambda v: v.tensor_copy(out=lnT[:], in_=ps[1][:, 0:64]))

            wab_rot = Rot(ph, "wab", 3, [128, 8, 512], BF16)
            gst_rot = Rot(ph, "gst", 2, [128, 512], F32)
            gps = PsRot(c, [4, 5, 6])
            bt = ph.sbuf("bt", [48, 128], F32)
            bT = ph.sbuf("bT", [128, 48], F32)
            gbias = ph.sbuf("gbias", [128, 2, D], F32)
            mraw = ph.sbuf("mraw", [128, 32, 3], F32)
            tmp83 = ph.sbuf("tmp83", [128, 8, 3], F32)
            for l in range(n_layers):
                c.dma("sp", bt[:], b_ada[l].rearrange("(j p) -> j p", p=128), [], ["bt"])
                c.dma("sp", [gbias[:, 0, :], gbias[:, 1, :]],
                      [b_ada[l, 2048:3072].partition_broadcast(128),
                       b_ada[l, 5120:6144].partition_broadcast(128)], [], ["gbias"])
                c.op("pe", ["bt", "identf"], ["ps2"],
                     lambda pe: pe.matmul(ps[2][:, 0:48], bt[0:48, :], identf[0:48, 0:48], start=True, stop=True))
                c.op("act", ["ps2"], ["bT"], lambda a: a.copy(out=bT[:], in_=ps[2][:, 0:48]))
                first3 = True
                for cc in range(12):
                    kind = cc // 2
                    half = cc % 2
                    if kind in (2, 5):
                        wa, wak = wab_rot.next()
                        c.dma("pool", wa[:], w_ada[l][:, cc * 512:(cc + 1) * 512].rearrange("(k p) f -> p k f", p=128),
                              [], [wak])
                    else:
                        wa, wak = wab_rot.next()
                        c.dma("pool", wa[:], w_ada[l][:, cc * 512:(cc + 1) * 512].rearrange("(k p) f -> p k f", p=128),
                              [], [wak])
                    if kind in (2, 5):
                        which = 0 if kind == 2 else 1
                        for cls in range(3):
                            pst, psk = gps.next()

                            def e_g(pe, pst=pst, cls=cls, wa=wa):
                                for k in range(8):
                                    ins = pe.matmul(pst[:, :], silbc[cls][:, k, :], wa[:, k, :],
                                                    start=(k == 0), stop=(k == 7))
                                return ins
                            c.op("pe", [wak, f"silbc{cls}"], [psk], e_g)
                            gst, gstk = gst_rot.next()
                            c.op("dve", [psk, "gbias"], [gstk],
                                 lambda v, pst=pst, gst=gst, which=which, half=half: v.tensor_tensor(
                                     out=gst[:], in0=pst[:, :], in1=gbias[:, which, half * 512:(half + 1) * 512],
                                     op=ALU.add))
                            c.dma("sp", gsc[l, cls, which][:, half * 512:(half + 1) * 512], gst[:],
                                  [gstk], [("gsc", l, cls, which, half)])
                    else:
                        ks = {1: 0, 0: 1, 4: 2, 3: 3}[kind]

                        def e_m(pe, wa=wa, ks=ks, half=half, first=first3):
                            for fs in range(4):
                                slot = ks * 8 + half * 4 + fs
                                for k in range(8):
                                    ins = pe.matmul(ps[3][:, slot * 3:(slot + 1) * 3],
                                                    wa[:, k, fs * 128:(fs + 1) * 128], silTb[:, k, :],
                                                    start=(first and fs == 0 and k == 0), stop=(k == 7),
                                                    skip_group_check=True)
                            return ins
                        c.op("pe", [wak, "silTb"], ["ps3"], e_m)
                        first3 = False
                ps3v = ps[3][:, 0:96].rearrange("p (s c) -> p s c", c=3)
                for ks, a0 in ((0, 8), (1, 0), (2, 32), (3, 24)):
                    c.op("dve", ["ps3", "bT"], ["mraw"],
                         lambda v, ks=ks, a0=a0: v.tensor_tensor(
                             out=mraw[:, ks * 8:(ks + 1) * 8, :], in0=ps3v[:, ks * 8:(ks + 1) * 8, :],
                             in1=bT[:, a0:a0 + 8].unsqueeze(2).to_broadcast([128, 8, 3]), op=ALU.add))
                base = l * 96
                mv = modS[:, base:base + 96].rearrange("p (s c) -> p s c", c=3)
                lng = lnT[:, l * 8:(l + 1) * 8].unsqueeze(2).to_broadcast([128, 8, 3])
                lnb = lnT[:, 32 + l * 8:32 + (l + 1) * 8].unsqueeze(2).to_broadcast([128, 8, 3])
                c.op("dve", ["mraw"], ["modS"], lambda v: v.tensor_scalar_add(out=mv[:, 0:8, :], in0=mraw[:, 0:8, :], scalar1=1.0))
                c.op("dve", ["mraw"], ["modS"], lambda v: v.tensor_copy(out=mv[:, 8:16, :], in_=mraw[:, 8:16, :]))
                c.op("dve", ["mraw"], ["tmp83"], lambda v: v.tensor_scalar_add(out=tmp83[:], in0=mraw[:, 16:24, :], scalar1=1.0))
                c.op("dve", ["tmp83", "lnT"], ["modS"], lambda v: v.tensor_tensor(out=mv[:, 16:24, :], in0=tmp83[:], in1=lng, op=ALU.mult))
                c.op("dve", ["tmp83", "lnT"], ["tmp83"], lambda v: v.tensor_tensor(out=tmp83[:], in0=tmp83[:], in1=lnb, op=ALU.mult))
                c.op("dve", ["tmp83", "mraw"], ["modS"], lambda v: v.tensor_tensor(out=mv[:, 24:32, :], in0=tmp83[:], in1=mraw[:, 24:32, :], op=ALU.add))
            if debug:
                c.dma("sp", dbg_mod[:, :], modS[:], ["modS"], ["dbg_mod"], is_output=True)

    def xin_ap(l, b, t):
        if l == 0:
            if t < NTL:
                return x[b, t * 128:(t + 1) * 128, :], ("x", b, t)
            return ctxin[b, (t - NTL) * 128:(t - NTL + 1) * 128, :], ("ctx", b, t)
        return xa[b, t * 128:(t + 1) * 128, :], ("xa", b, t)

    def cls_of(b, t):
        return b if t < NTL else 2

    def make_hT(ph_rots, l, b, tiles, kind_s, kind_b, dst, dst_key, dst_col0, src_loader):
        tps = ph_rots["tps"]
        n = len(tiles)
        srcs = [src_loader(t) for t in tiles]
        for j in range(8):
            pst, psk = tps.next()
            pv = pst[:].bitcast(BF16)

            def e_t(pe, j=j, pv=pv):
                for i in range(n):
                    ins = pe.transpose(out=pv[:, i * 128:(i + 1) * 128], in_=srcs[i][0][:, j * 128:(j + 1) * 128],
                                       identity=identb[:])
                return ins
            c.op("pe", [s[1] for s in srcs] + ["identb"], [psk], e_t)
            cls = cls_of(b, tiles[0])
            c.op("act", [psk, "modS"], [dst_key],
                 lambda a, j=j, pv=pv, cls=cls: a.activation(
                     out=dst[:, j, dst_col0:dst_col0 + n * 128], in_=pv[:, 0:n * 128], func=AF.Identity,
                     bias=mod_ap(l, kind_b, j, cls), scale=mod_ap(l, kind_s, j, cls)))

    def ln_stats(ph_rots, r, rk):
        st, stk = ph_rots["stats"].next()
        c.op("dve", [rk], [stk], lambda v: v.bn_stats(out=st[:, 0:6], in_=r[:, 0:512]))
        c.op("dve", [rk], [stk], lambda v: v.bn_stats(out=st[:, 6:12], in_=r[:, 512:1024]))
        c.op("dve", [stk], [stk], lambda v: v.bn_aggr(out=st[:, 12:14], in_=st[:, 0:12]))
        c.op("dve", [stk], [stk], lambda v: v.tensor_scalar_add(out=st[:, 14:15], in0=st[:, 13:14], scalar1=LN_EPS))
        c.op("act", [stk], [stk], lambda a: a.activation(out=st[:, 14:15], in_=st[:, 14:15], func=AF.Ln))
        c.op("act", [stk], [stk], lambda a: a.activation(out=st[:, 14:15], in_=st[:, 14:15], func=AF.Exp, scale=-0.5))
        c.op("dve", [stk], [stk], lambda v: v.scalar_tensor_tensor(out=st[:, 15:16], in0=st[:, 12:13], scalar=-1.0,
                                                                   in1=st[:, 14:15], op0=ALU.mult, op1=ALU.mult))
        return st, stk

    def ln_apply(ph_rots, stt, r, rk, gb, gbk, xo, xok, xnb=None, xnbk=None):
        st, stk = stt
        xn, xnk = ph_rots["xn"].next()
        c.op("act", [rk, stk], [xnk], lambda a: a.activation(out=xn[:], in_=r[:], func=AF.Identity,
                                                             bias=st[:, 15:16], scale=st[:, 14:15]))
        if xnb is not None:
            c.op("act", [rk, stk], [xnbk], lambda a: a.activation(out=xnb[:], in_=r[:], func=AF.Identity,
                                                                  bias=st[:, 15:16], scale=st[:, 14:15]))
        c.op("pool", [xnk, gbk], [xnk], lambda g: g.tensor_tensor(out=xn[:], in0=xn[:], in1=gb[:, 0, :], op=ALU.mult))
        c.op("pool", [xnk, gbk], [xok], lambda g: g.tensor_tensor(out=xo[:], in0=xn[:], in1=gb[:, 1, :], op=ALU.add))

    def layer_norm(ph_rots, r, rk, gb, gbk, xo, xok, xnb=None, xnbk=None):
        stt = ln_stats(ph_rots, r, rk)
        ln_apply(ph_rots, stt, r, rk, gb, gbk, xo, xok, xnb, xnbk)

    def qkv_project(ph, rots, l, b, last, wq, wqk, nq, wk, wkk, nk, wv, wvk, vcols, QT, KT, VA, cosT, sinT):
        xb_rot, hT_rot, raw_rot, t1_rot, t2_rot = (rots[k] for k in ("xbf", "hT", "raw", "t1", "t2"))
        mm = rots["mm"]
        vheads = VA.shape[2]
        vd = VA.shape[3] - 1
        groups = [list(range(g * 4, g * 4 + 4)) for g in range(4)] + [[16, 17]]
        loaded = {}

        def loader(t):
            if t in loaded:
                return loaded[t]
            xbt, xbk = xb_rot.next()
            ap, dk = xin_ap(l, b, t)
            c.dma("pool", xbt[:], ap, [dk], [xbk])
            loaded[t] = (xbt, xbk)
            return loaded[t]
        for t in groups[0]:
            loader(t)
        for gi, tiles in enumerate(groups):
            is_ctx = tiles[0] >= NTL
            T = len(tiles) * 128
            col0 = tiles[0] * 128
            hT, hTk = hT_rot.next()
            make_hT(rots, l, b, tiles, 0, 1, hT, hTk, 0, loader)
            if gi + 1 < len(groups):
                for t in groups[gi + 1]:
                    loader(t)
            jobs = []
            if not (is_ctx and last):
                jobs += [("q", i) for i in range(nq)]
            jobs += [("k", i) for i in range(nk)]
            def emit_proj(kind, i):
                w, wkey = (wq, wqk) if kind == "q" else (wk, wkk)
                dstT, dkey = (QT, "QT") if kind == "q" else (KT, "KT")
                pst, psk = mm.next()

                def e_p(pe):
                    for k in range(8):
                        ins = pe.matmul(pst[:, 0:T], w[:, k, i * 128:(i + 1) * 128], hT[:, k, 0:T],
                                        start=(k == 0), stop=(k == 7))
                    return ins
                c.op("pe", [wkey, hTk], [psk], e_p)
                if is_ctx:
                    c.op("act", [psk], [(dkey, i, tiles[0])],
                         lambda a: a.copy(out=dstT[:, i, col0:col0 + T], in_=pst[:, 0:T]))
                    return None
                raw, rawk = raw_rot.next()
                c.op("act", [psk], [rawk], lambda a: a.copy(out=raw[:, 0:T], in_=pst[:, 0:T]))
                return (dstT, dkey, i, raw, rawk)

            def emit_rope(st):
                dstT, dkey, i, raw, rawk = st
                pst2, psk2 = mm.next()
                c.op("pe", [rawk, "rtb"], [psk2],
                     lambda pe: pe.matmul(pst2[:, 0:T], rtb[:], raw[:, 0:T], start=True, stop=True))
                t1, t1k = t1_rot.next()
                t2, t2k = t2_rot.next()
                c.op("dve", [rawk, "cos"], [t1k],
                     lambda g: g.tensor_tensor(out=t1[:, 0:T], in0=raw[:, 0:T], in1=cosT[:, col0:col0 + T], op=ALU.mult))
                c.op("dve", [psk2, "sin"], [t2k],
                     lambda v: v.tensor_tensor(out=t2[:, 0:T], in0=pst2[:, 0:T], in1=sinT[:, col0:col0 + T], op=ALU.mult))
                c.op("pool", [t1k, t2k], [(dkey, i, tiles[0])],
                     lambda g: g.tensor_tensor(out=dstT[:, i, col0:col0 + T], in0=t1[:, 0:T], in1=t2[:, 0:T], op=ALU.add))

            prev = None
            for job in jobs + [None]:
                cur = emit_proj(*job) if job is not None else None
                if prev is not None:
                    emit_rope(prev)
                prev = cur
            for si, t in enumerate(tiles):
                pst, psk = mm.next()

                def e_v(pe, pst=pst, hT=hT, si=si):
                    for k in range(8):
                        ins = pe.matmul(pst[:, 0:vcols], hT[:, k, si * 128:(si + 1) * 128], wv[:, k, 0:vcols],
                                        start=(k == 0), stop=(k == 7))
                    return ins
                c.op("pe", [wvk, hTk], [psk], e_v)
                c.op("act", [psk], [("VA", t)],
                     lambda a, pst=pst, t=t: a.copy(out=VA[:, t, :, 0:vd],
                                                    in_=pst[:, 0:vcols].rearrange("p (h d) -> p h d", d=vd)))

    def std_rots(ph, raw_cols=512):
        return {
            "xbf": Rot(ph, "xbf", 8, [128, D], BF16),
            "hT": Rot(ph, "hT", 2, [128, 8, 512], BF16),
            "raw": Rot(ph, "raw", 3, [128, 512], BF16),
            "t1": Rot(ph, "t1", 2, [128, 512], F32),
            "t2": Rot(ph, "t2", 2, [128, 512], F32),
            "tps": PsRot(c, [0, 1]),
            "mm": PsRot(c, [2, 3, 4, 5]),
        }

    def phase_attn_A(l, b, last):
        jj = l // 2
        with c.phase(f"attnA_{l}_{b}") as ph:
            rots = std_rots(ph)
            wq = ph.sbuf("wq", [128, 8, 1024], BF16)
            wk = ph.sbuf("wk", [128, 8, 4, 2, 64], BF16)
            wv = ph.sbuf("wv", [128, 8, 256], BF16)
            cosT = ph.sbuf("cosT", [128, S], F32)
            sinT = ph.sbuf("sinT", [128, S], F32)
            maskp = ph.sbuf("maskp", [128, 512], BF16)
            maskn = ph.sbuf("maskn", [128, 512], BF16)
            esink = ph.sbuf("esink", [128, 16], F32)
            QT = ph.sbuf("QT", [128, 8, TOK], BF16)
            KT = ph.sbuf("KT", [128, 4, TOK], BF16)
            VA = ph.sbuf("VA", [128, NT, 4, 65], BF16)
            wsrc = wqkv_a[jj]
            for k in range(8):
                c.dma("pool", wq[:, k, :], wsrc[k * 128:(k + 1) * 128, 0:1024], [], ["wq"])
            ksrc = wsrc[:, 1024:1280].rearrange("(k p) (g d) -> p k g d", p=128, d=64)
            for k in range(8):
                c.dma("pool", [wk[:, k, :, 0, :], wk[:, k, :, 1, :]], [ksrc[:, k], ksrc[:, k]], [], ["wk"])
            c.dma("pool", wv[:], wsrc[:, 1280:1536].rearrange("(k p) f -> p k f", p=128), [], ["wv"])
            c.dma("sp", cosT[:], k_cos[:, :], [], ["cos"])
            c.dma("sp", sinT[:], k_sin[:, :], [], ["sin"])
            c.dma("pool", maskp[:], k_maskp[:, :], [], ["maskp"])
            c.dma("pool", maskn[:], k_maskn[:, :], [], ["maskn"])
            c.dma("sp", esink[:], sink_a[jj].partition_broadcast(128), [], ["esink"])
            c.op("act", ["esink"], ["esink"], lambda a: a.activation(out=esink[:], in_=esink[:], func=AF.Exp))
            c.op("pool", [], [("VA", t) for t in range(NT)], lambda g: g.memset(VA[:, :, :, 64:65], 1.0))
            wkv = wk[:].rearrange("p k g r d -> p k (g r d)")
            qkv_project(ph, rots, l, b, last, wq, "wq", 8, wkv, "wk", 4, wv, "wv", 256, QT, KT, VA, cosT, sinT)

            if stop_after is not None and stop_after[0] == "qkv":
                return
            spsx = PsRot(c, [0, 2, 6])
            spsy = PsRot(c, [1, 3, 7])
            ops_ = PsRot(c, [4, 5])
            pT_rot = Rot(ph, "pT", 4, [128, 512], BF16)
            ot_rot = Rot(ph, "ot", 2, [128, 16, 64], BF16)
            dn_rot = Rot(ph, "dn", 2, [128, 8], F32)
            qtiles = list(range(NTL)) + ([] if last else [16, 17])
            pcol = {0: 0, 2: 128, 1: 256, 3: 384}
            LA = 2
            its = []
            for qt in qtiles:
                if qt < NTL:
                    kts = ([(qt - 1, maskp, "maskp")] if qt > 0 else []) + [(qt, None, None)] + \
                          ([(qt + 1, maskn, "maskn")] if qt < NTL - 1 else []) + [(16, None, None), (17, None, None)]
                else:
                    kts = [(16, None, None), (17, None, None)]
                for g in range(4):
                    for ki, (kt, msk, mskk) in enumerate(kts):
                        its.append((qt, g, ki, len(kts), kt, msk, mskk))
            state = {}

            def emit_s(it):
                qt, g, ki, nk, kt, msk, mskk = it
                sx, sxk = spsx.next()
                sy, syk = spsy.next()

                def e_s(pe):
                    if msk is not None:
                        pe.matmul(sx[:, 0:256], identb[:], msk[:, 0:256], start=True, stop=False, skip_group_check=True)
                        pe.matmul(sy[:, 0:256], identb[:], msk[:, 0:256], start=True, stop=False, skip_group_check=True)
                    for hd in range(4):
                        cch = 2 * g + hd // 2
                        base = 64 * (hd % 2)
                        dst = sx if hd % 2 == 0 else sy
                        o = 128 * (hd // 2)
                        ins = pe.matmul(dst[:, o:o + 128],
                                        KT[base:base + 64, g, kt * 128:(kt + 1) * 128],
                                        QT[base:base + 64, cch, qt * 128:(qt + 1) * 128],
                                        start=(msk is None and hd < 2), stop=(hd >= 2), skip_group_check=True)
                    return ins
                rk = [("KT", g, (kt // 4) * 4 if kt < 16 else 16), ("QT", 2 * g, (qt // 4) * 4 if qt < 16 else 16),
                      ("QT", 2 * g + 1, (qt // 4) * 4 if qt < 16 else 16), "identb"] + ([mskk] if mskk else [])
                c.op("pe", rk, [sxk, syk], e_s)
                pT, pTk = pT_rot.next()
                bx = int(sxk[2:])
                sxy = c.psall[:, bx * 512:(bx + 2) * 512].rearrange("p (m n) -> p m n", m=2)
                c.op("act", [sxk, syk], [(pTk, 0), (pTk, 1)],
                     lambda a: a.activation(out=pT[:].rearrange("p (m n) -> p m n", m=2), in_=sxy[:, :, 0:256], func=AF.Exp, scale=SCALE))
                return (pT, pTk)

            def emit_pv(it, pt):
                qt, g, ki, nk, kt, msk, mskk = it
                pT, pTk = pt
                if g == 0 and ki == 0:
                    state["ot"] = ot_rot.next()
                if ki == 0:
                    state["op"] = ops_.next()
                ot, otk = state["ot"]
                opst, opsk = state["op"]

                def e_o(pe):
                    for hd in range(4):
                        ins = pe.matmul(opst[:, hd * 128:hd * 128 + 65], pT[:, pcol[hd]:pcol[hd] + 128],
                                        VA[:, kt, g, :], start=(ki == 0 and hd == 0), stop=(ki == nk - 1),
                                        skip_group_check=True)
                    return ins
                c.op("pe", [(pTk, 0), (pTk, 1), ("VA", kt)], [opsk], e_o)
                if ki != nk - 1:
                    return
                dn, dnk = dn_rot.next()
                ov = opst[:, :].rearrange("p (h d) -> p h d", d=128)
                c.op("dve", [opsk, "esink"], [dnk],
                     lambda v: v.tensor_tensor(out=dn[:, 0:4], in0=ov[:, :, 64], in1=esink[:, 4 * g:4 * g + 4], op=ALU.add))
                c.op("dve", [dnk], [dnk], lambda v: v.reciprocal(out=dn[:, 4:8], in_=dn[:, 0:4]))
                c.op("dve", [opsk, dnk], [otk],
                     lambda v: v.tensor_tensor(
                         out=ot[:, 4 * g:4 * g + 4, :], in0=ov[:, :, 0:64],
                         in1=dn[:, 4:8].unsqueeze(2).to_broadcast([128, 4, 64]), op=ALU.mult))
                if g == 3:
                    c.dma("pool", osc[b, qt * 128:(qt + 1) * 128, :], ot[:].rearrange("p h d -> p (h d)"), [otk], [("osc", b, qt)])

            pend = []
            for idx in range(len(its) + LA):
                if idx < len(its):
                    pend.append(emit_s(its[idx]))
                if idx - LA >= 0:
                    emit_pv(its[idx - LA], pend[idx - LA])

    def phase_attn_B(l, b, last, hh):
        jj = l // 2
        lam_init = 0.8 - 0.6 * math.exp(-0.3 * l)
        with c.phase(f"attnB_{l}_{b}_{hh}") as ph:
            rots = std_rots(ph)
            wq = ph.sbuf("wq", [128, 8, 512], BF16)
            wk = ph.sbuf("wk", [128, 8, 512], BF16)
            wv = ph.sbuf("wv", [128, 8, 512], BF16)
            cosT = ph.sbuf("cosT", [128, S], F32)
            sinT = ph.sbuf("sinT", [128, S], F32)
            QT = ph.sbuf("QT", [128, 4, TOK], BF16)
            KT = ph.sbuf("KT", [128, 4, TOK], BF16)
            VA = ph.sbuf("VA", [128, NT, 4, 129], BF16)
            lamt = ph.sbuf("lamt", [128, 4, 64], F32)
            lsm = ph.sbuf("lsm", [128, 8], F32)
            gsub = ph.sbuf("gsub", [128, 128], F32)
            wsrc = wqkv_b[jj]
            for wt, key, c0 in ((wq, "wq", hh * 512), (wk, "wk", 1024 + hh * 512), (wv, "wv", 2048 + hh * 512)):
                for k in range(8):
                    c.dma("pool", wt[:, k, :], wsrc[k * 128:(k + 1) * 128, c0:c0 + 512], [], [key])
            c.dma("sp", cosT[:], k_cos[:, :], [], ["cos"])
            c.dma("sp", sinT[:], k_sin[:, :], [], ["sin"])
            c.dma("sp", lamt[:].rearrange("p a d -> p (a d)"),
                  lambda_b[jj].rearrange("a d -> (a d)").partition_broadcast(128), [], ["lamt"])
            c.dma("sp", gsub[:], subln_b[jj].partition_broadcast(128), [], ["gsub"])
            c.op("dve", ["lamt"], ["lamt"], lambda v: v.tensor_tensor(out=lamt[:, 0, :], in0=lamt[:, 0, :], in1=lamt[:, 1, :], op=ALU.mult))
            c.op("dve", ["lamt"], ["lamt"], lambda v: v.tensor_tensor(out=lamt[:, 2, :], in0=lamt[:, 2, :], in1=lamt[:, 3, :], op=ALU.mult))
            c.op("dve", ["lamt"], ["lsm"], lambda v: v.reduce_sum(out=lsm[:, 0:1], in_=lamt[:, 0, :], axis=AX.X))
            c.op("dve", ["lamt"], ["lsm"], lambda v: v.reduce_sum(out=lsm[:, 1:2], in_=lamt[:, 2, :], axis=AX.X))
            c.op("act", ["lsm"], ["lsm"], lambda a: a.activation(out=lsm[:, 2:4], in_=lsm[:, 0:2], func=AF.Exp))
            c.op("dve", ["lsm"], ["lsm"], lambda v: v.tensor_tensor(out=lsm[:, 4:5], in0=lsm[:, 3:4], in1=lsm[:, 2:3], op=ALU.subtract))
            c.op("dve", ["lsm"], ["lsm"], lambda v: v.tensor_scalar_add(out=lsm[:, 4:5], in0=lsm[:, 4:5], scalar1=-lam_init))
            c.op("dve", ["gsub"], ["gsub"], lambda v: v.tensor_scalar_mul(out=gsub[:], in0=gsub[:], scalar1=(1.0 - lam_init)))
            c.op("pool", [], [("VA", t) for t in range(NT)], lambda g: g.memset(VA[:, :, :, 128:129], 1.0))
            qkv_project(ph, rots, l, b, last, wq, "wq", 4, wk, "wk", 4, wv, "wv", 512, QT, KT, VA, cosT, sinT)

            spsx = PsRot(c, [0, 2])
            spsy = PsRot(c, [1, 3])
            pT_rot = Rot(ph, "pT", 4, [128, 2, 512], BF16)
            ot_rot = Rot(ph, "ot", 2, [128, 4, 512], BF16)
            sm_rot = Rot(ph, "sm", 2, [128, 16], F32)
            of_rot = Rot(ph, "of", 2, [128, 4, 128], F32)
            jk_rot = Rot(ph, "jk", 2, [128, 128], F32)
            qgroups = [list(range(g * 4, g * 4 + 4)) for g in range(4)] + ([] if last else [[16, 17]])
            LA = 2
            for qg in qgroups:
                nq = len(qg)
                Tq = nq * 128
                q0 = qg[0] * 128
                kts = list(range(NT)) if qg[0] < NTL else [16, 17]
                nk = len(kts)
                ot, otk = ot_rot.next()
                gq = qg[0]
                its = [(h, ki, kt) for h in range(4) for ki, kt in enumerate(kts)]
                pend = []

                def emit_s(it):
                    h, ki, kt = it
                    sx, sxk = spsx.next()
                    sy, syk = spsy.next()

                    def e_s(pe):
                        pe.matmul(sx[:, 0:Tq], KT[0:64, h, kt * 128:(kt + 1) * 128], QT[0:64, h, q0:q0 + Tq], start=True, stop=True)
                        return pe.matmul(sy[:, 0:Tq], KT[64:128, h, kt * 128:(kt + 1) * 128], QT[64:128, h, q0:q0 + Tq], start=True, stop=True)
                    c.op("pe", [("KT", h, (kt // 4) * 4 if kt < 16 else 16), ("QT", h, gq)], [sxk, syk], e_s)
                    pT, pTk = pT_rot.next()
                    bx = int(sxk[2:])
                    sxy = c.psall[:, bx * 512:(bx + 2) * 512].rearrange("p (m n) -> p m n", m=2)
                    c.op("act", [sxk, syk], [(pTk, 0), (pTk, 1)],
                         lambda a: a.activation(out=pT[:, :, 0:Tq], in_=sxy[:, :, 0:Tq], func=AF.Exp, scale=SCALE))
                    return (pT, pTk)

                def emit_pv(it, pt):
                    h, ki, kt = it
                    pT, pTk = pt

                    def e_o(pe):
                        for qi in range(nq):
                            for m in range(2):
                                ins = pe.matmul(ps[4 + qi][:, m * 256:m * 256 + 129], pT[:, m, qi * 128:(qi + 1) * 128], VA[:, kt, h, :],
                                                start=(ki == 0 and m == 0), stop=(ki == nk - 1), skip_group_check=True)
                        return ins
                    c.op("pe", [(pTk, 0), (pTk, 1), ("VA", kt)], [f"ps{4 + qi}" for qi in range(nq)], e_o)
                    if ki != nk - 1:
                        return
                    sm, smk = sm_rot.next()
                    of, ofk = of_rot.next()
                    for qi in range(nq):
                        pk = f"ps{4 + qi}"
                        pb = ps[4 + qi]
                        c.op("dve", [pk], [smk], lambda v, qi=qi, pb=pb: v.reciprocal(out=sm[:, qi:qi + 1], in_=pb[:, 128:129]))
                        c.op("dve", [pk, smk], [smk], lambda v, qi=qi, pb=pb: v.reciprocal(out=sm[:, 4 + qi:5 + qi], in_=pb[:, 384:385]))
                        c.op("dve", ["lsm", smk], [smk],
                             lambda v, qi=qi: v.tensor_scalar_mul(out=sm[:, 4 + qi:5 + qi], in0=sm[:, 4 + qi:5 + qi], scalar1=lsm[:, 4:5]))
                        c.op("dve", [pk, smk], [ofk],
                             lambda v, qi=qi, pb=pb: v.tensor_scalar_mul(out=of[:, qi, :], in0=pb[:, 0:128], scalar1=sm[:, qi:qi + 1]))
                        c.op("dve", [pk, smk, ofk], [ofk],
                             lambda v, qi=qi, pb=pb: v.scalar_tensor_tensor(out=of[:, qi, :], in0=pb[:, 256:384], scalar=sm[:, 4 + qi:5 + qi], in1=of[:, qi, :], op0=ALU.mult, op1=ALU.add))
                        jk, jkk = jk_rot.next()
                        c.op("act", [ofk], [jkk, smk],
                             lambda a, qi=qi, jk=jk: a.activation(out=jk[:], in_=of[:, qi, :], func=AF.Square, accum_out=sm[:, 8 + qi:9 + qi]))
                    c.op("dve", [smk], [smk], lambda v: v.tensor_scalar(out=sm[:, 8:8 + nq], in0=sm[:, 8:8 + nq], scalar1=1.0 / 128.0, scalar2=SUBLN_EPS, op0=ALU.mult, op1=ALU.add))
                    c.op("act", [smk], [smk], lambda a: a.activation(out=sm[:, 12:12 + nq], in_=sm[:, 8:8 + nq], func=AF.Ln))
                    c.op("act", [smk], [smk], lambda a: a.activation(out=sm[:, 12:12 + nq], in_=sm[:, 12:12 + nq], func=AF.Exp, scale=-0.5))
                    for qi in range(nq):
                        c.op("dve", [ofk, smk, "gsub"], [otk],
                             lambda v, qi=qi: v.scalar_tensor_tensor(
                                 out=ot[:, qi, h * 128:(h + 1) * 128], in0=of[:, qi, :], scalar=sm[:, 12 + qi:13 + qi], in1=gsub[:], op0=ALU.mult, op1=ALU.mult))

                for idx in range(len(its) + LA):
                    if idx < len(its):
                        pend.append(emit_s(its[idx]))
                    if idx - LA >= 0:
                        emit_pv(its[idx - LA], pend[idx - LA])
                for qi, t in enumerate(qg):
                    c.dma("pool", osc[b, t * 128:(t + 1) * 128, hh * 512:(hh + 1) * 512], ot[:, qi, :], [otk], [("osc", b, t, hh)])

    def phase_oproj(l, b, last):
        typ_a = (l % 2 == 0)
        jj = l // 2
        with c.phase(f"oproj_{l}_{b}") as ph:
            wo = ph.sbuf("wo", [128, 8, D], BF16)
            gb1 = ph.sbuf("gb1", [128, 2, D], F32)
            g1l = ph.sbuf("g1l", [128, D], F32)
            g1c = ph.sbuf("g1c", [128, D], F32)
            wsrc = (wo_a if typ_a else wo_b)[jj]
            for k in range(8):
                c.dma("pool", wo[:, k, :], wsrc[k * 128:(k + 1) * 128, :], [], ["wo"])
            c.dma("sp", [gb1[:, 0, :], gb1[:, 1, :]],
                  [ln_attn_g[l].partition_broadcast(128), ln_attn_b[l].partition_broadcast(128)], [], ["gb1"])
            c.dma("sp", g1l[:], gsc[l, b, 0], [("gsc", l, b, 0, 0), ("gsc", l, b, 0, 1)], ["g1l"])
            c.dma("sp", g1c[:], gsc[l, 2, 0], [("gsc", l, 2, 0, 0), ("gsc", l, 2, 0, 1)], ["g1c"])
            rots = {"stats": Rot(ph, "st", 4, [128, 16], F32), "xn": Rot(ph, "xn", 2, [128, D], F32),
                    "tps": PsRot(c, [0, 1])}
            ob_rot = Rot(ph, "ob", 3, [128, D], BF16)
            xf_rot = Rot(ph, "xf", 4, [128, D], F32)
            oT_rot = Rot(ph, "oT", 2, [128, 8, 128], BF16)
            r_rot = Rot(ph, "r", 3, [128, D], F32)
            x1_rot = Rot(ph, "x1", 2, [128, D], F32)
            xnb_rot = Rot(ph, "xnb", 5, [128, D], BF16)
            sg_rot = Rot(ph, "sg", 2, [128, 64], F32)
            alps = PsRot(c, [2, 3, 4, 5])
            rps = PsRot(c, [6, 7])
            tiles = list(range(NTL)) + ([] if last else [16, 17])
            loads = {}
            st1 = {}
            st1a = {}
            fr = {}

            def load(t):
                ob, obk = ob_rot.next()
                c.dma("sp", ob[:], osc[b, t * 128:(t + 1) * 128, :], [], [obk])
                xf, xfk = xf_rot.next()
                ap, dk = xin_ap(l, b, t)
                c.dma("sp", xf[:], ap, [dk], [xfk])
                loads[t] = (ob, obk, xf, xfk)

            def stage1(t):
                g1 = g1l if t < NTL else g1c
                g1k = "g1l" if t < NTL else "g1c"
                ob, obk, xf, xfk = loads.pop(t)
                pst, psk = rots["tps"].next()
                pv = pst[:].bitcast(BF16)

                def e_t(pe):
                    for j in range(8):
                        ins = pe.transpose(out=pv[:, j * 128:(j + 1) * 128], in_=ob[:, j * 128:(j + 1) * 128], identity=identb[:])
                    return ins
                c.op("pe", [obk, "identb"], [psk], e_t)
                oT, oTk = oT_rot.next()
                c.op("act", [psk], [oTk], lambda a: a.copy(out=oT[:].rearrange("p j t -> p (j t)"), in_=pv[:, :]))
                aps = []
                for half in range(2):
                    apst, apsk = alps.next()

                    def e_a(pe, apst=apst, half=half):
                        for k in range(8):
                            ins = pe.matmul(apst[:, :], oT[:, k, :], wo[:, k, half * 512:(half + 1) * 512], start=(k == 0), stop=(k == 7))
                        return ins
                    c.op("pe", [oTk, "wo"], [apsk], e_a)
                    aps.append((apst, apsk))
                fr[t] = (aps, xf, xfk, g1, g1k)

            def stage1back(t):
                aps, xf, xfk, g1, g1k = fr.pop(t)
                r, rk = r_rot.next()
                for half in range(2):
                    apst, apsk = aps[half]
                    c.op("dve", [apsk, g1k], [(rk, half)],
                         lambda v, apst=apst, half=half: v.tensor_tensor(out=r[:, half * 512:(half + 1) * 512], in0=apst[:, :], in1=g1[:, half * 512:(half + 1) * 512], op=ALU.mult))
                c.op("dve", [(rk, 0), (rk, 1), xfk], [rk],
                     lambda g: g.scalar_tensor_tensor(out=r[:], in0=xf[:], scalar=ALPHA, in1=r[:], op0=ALU.mult, op1=ALU.add))
                st1a[t] = (ln_stats(rots, r, rk), r, rk)

            def stage1b(t):
                stt, r, rk = st1a.pop(t)
                x1, x1k = x1_rot.next()
                xnb, xnbk = xnb_rot.next()
                ln_apply(rots, stt, r, rk, gb1, "gb1", x1, x1k, xnb, xnbk)
                c.dma("pool", xb[b, t * 128:(t + 1) * 128, :], x1[:], [x1k], [("xb", b, t)])
                st1[t] = (xnb, xnbk)

            h2ps = PsRot(c, [6, 7])

            def stage2(t0):
                cls = cls_of(b, t0)
                pair = [st1.pop(t0), st1.pop(t0 + 1)]
                for jh in range(2):
                    pst, psk = h2ps.next()
                    pv = pst[:].bitcast(BF16)

                    def e_t2(pe):
                        for jj_ in range(4):
                            j = jh * 4 + jj_
                            for i in range(2):
                                ins = pe.transpose(out=pv[:, jj_ * 256 + i * 128:jj_ * 256 + (i + 1) * 128],
                                                   in_=pair[i][0][:, j * 128:(j + 1) * 128], identity=identb[:])
                        return ins
                    c.op("pe", [pair[0][1], pair[1][1], "identb"], [psk], e_t2)
                    for jj_ in range(4):
                        j = jh * 4 + jj_
                        c.op("act", [psk, "modS"], [("h2T", t0, j)],
                             lambda a, j=j, jj_=jj_, pv=pv: a.activation(
                                 out=h2T[:, j, t0 * 128:(t0 + 2) * 128], in_=pv[:, jj_ * 256:(jj_ + 1) * 256], func=AF.Identity,
                                 bias=mod_ap(l, 3, j, cls), scale=mod_ap(l, 2, j, cls)))

            n = len(tiles)
            for i in range(min(2, n)):
                load(tiles[i])
            for i in range(n + 1):
                if i < n:
                    stage1(tiles[i])
                    if i + 2 < n:
                        load(tiles[i + 2])
                if i >= 1:
                    stage1back(tiles[i - 1])
                    stage1b(tiles[i - 1])
                    if (i - 1) % 2 == 1 and i - 1 >= 3:
                        stage2(tiles[i - 4])
            for t0 in sorted(k for k in list(st1.keys()) if k % 2 == 0):
                stage2(t0)
            nt = n
            rp = ps[2]

            def e_r(pe):
                for ti, t in enumerate(tiles):
                    for k in range(8):
                        ins = pe.matmul(rp[:, ti * E:(ti + 1) * E], h2T[:, k, t * 128:(t + 1) * 128], wrb[:, k, :],
                                        start=(ti == 0 and k == 0), stop=(k == 7), skip_group_check=True)
                return ins
            c.op("pe", [("h2T", t0, j) for t0 in tiles[::2] for j in range(8)] + ["wrb"], ["ps2"], e_r)
            router_topk(ph, rp, "ps2", tiles)
            if debug:
                c.dma("sp", dbg_gates[b].rearrange("(t p) e -> p t e", p=128), gates[:], [("gates", t) for t in tiles], [("dbg_gates", b)], is_output=True)

    def router_topk(ph, rp, rpk, tiles):
        G = len(tiles)
        W = G * E
        sgs = ph.sbuf("rt_s", [128, NT * E], F32)
        sgb = ph.sbuf("rt_b", [128, NT * E], F32)
        scr = ph.sbuf("rt_x", [128, 8, NT * 4], F32)
        red = ph.sbuf("rt_r", [128, 2, NT], F32)
        k = "rt"
        s_ = sgs[:, 0:W]
        b_all = sgb[:, 0:W]
        Bv = b_all.rearrange("p (g i) -> p g i", i=4)
        n4 = G * 4

        def sc(i):
            return scr[:, i, 0:n4]
        c.op("act", [rpk], [k], lambda a: a.activation(out=s_, in_=rp[:, 0:W], func=AF.Exp, scale=-1.0))
        c.op("dve", [k], [k], lambda v: v.tensor_scalar_add(out=s_, in0=s_, scalar1=1.0))
        c.op("dve", [k], [k], lambda v: v.reciprocal(out=s_, in_=s_))
        c.op("dve", [k, "rbias"], [k], lambda v: v.tensor_tensor(
            out=b_all.rearrange("p (t e) -> p t e", e=E), in0=s_.rearrange("p (t e) -> p t e", e=E),
            in1=rbias[:].unsqueeze(1).to_broadcast([128, G, E]), op=ALU.add))
        a0, a1, a2, a3 = (Bv[:, :, i] for i in range(4))
        P_, Q_, R_, S_, M1, M2, GS = sc(0), sc(1), sc(2), sc(3), sc(4), sc(5), sc(6)
        steps = [
            (P_, a0, a1, ALU.max), (Q_, a0, a1, ALU.min), (R_, a2, a3, ALU.max), (S_, a2, a3, ALU.min),
            (M1, P_, R_, ALU.max),
            (M2, P_, R_, ALU.min),
            (Q_, Q_, S_, ALU.max),
            (M2, M2, Q_, ALU.max),
            (GS, M1, M2, ALU.add),
        ]
        for (o, i0, i1, op_) in steps:
            c.op("dve", [k], [k], lambda v, o=o, i0=i0, i1=i1, op_=op_: v.tensor_tensor(out=o, in0=i0, in1=i1, op=op_))
        gs3 = GS.rearrange("p (t g) -> p t g", g=4)
        c.op("dve", [k], [k], lambda v: v.tensor_reduce(out=red[:, 0, 0:G], in_=gs3, axis=AX.X, op=ALU.max))
        c.op("dve", [k], [k], lambda v: v.tensor_tensor(out=gs3, in0=gs3, in1=red[:, 0, 0:G].unsqueeze(2).to_broadcast([128, G, 4]), op=ALU.is_ge))
        c.op("dve", [k], [k], lambda v: v.tensor_tensor(out=Bv, in0=Bv, in1=M2.unsqueeze(2).to_broadcast([128, n4, 4]), op=ALU.is_ge))
        c.op("dve", [k], [k], lambda v: v.tensor_tensor(out=Bv, in0=Bv, in1=GS.unsqueeze(2).to_broadcast([128, n4, 4]), op=ALU.mult))
        c.op("dve", [k], [k], lambda v: v.tensor_tensor(out=b_all, in0=b_all, in1=s_, op=ALU.mult))
        b3 = b_all.rearrange("p (t e) -> p t e", e=E)
        c.op("dve", [k], [k], lambda v: v.tensor_reduce(out=red[:, 0, 0:G], in_=b3, axis=AX.X, op=ALU.add))
        c.op("dve", [k], [k], lambda v: v.reciprocal(out=red[:, 1, 0:G], in_=red[:, 0, 0:G]))
        t0 = tiles[0]
        c.op("dve", [k], [("gates", t) for t in tiles],
             lambda v: v.tensor_tensor(out=gates[:, t0:t0 + G, :], in0=b3,
                                       in1=red[:, 1, 0:G].unsqueeze(2).to_broadcast([128, G, E]), op=ALU.mult))

    def phase_moe(l, b, last):
        ntile = NTL if last else NT
        ntok = ntile * 128
        with c.phase() as pho:
            acc = pho.sbuf("acc", [128, NT, D], F32)
            with c.phase(f"moe_{l}_{b}") as ph:
                wg_rot = Rot(ph, "wg", 2, [128, 8, DE], BF16)
                wu_rot = Rot(ph, "wu", 2, [128, 8, DE], BF16)
                wd_rot = Rot(ph, "wd", 2, [128, 4, D], BF16)
                sg_rot = Rot(ph, "sgl", 2, [128, 512], BF16)
                act_rot = Rot(ph, "act", 2, [128, 4, 512], BF16)
                gps = PsRot(c, [0, 1])
                ups = PsRot(c, [2, 3])
                yps = PsRot(c, [4, 5, 6, 7])
                ttiles = []
                t0 = 0
                while t0 < ntile:
                    n = min(4, ntile - t0)
                    ttiles.append((t0, n))
                    t0 += n
                for e in range(E):
                    wg, wgk = wg_rot.next()
                    wu, wuk = wu_rot.next()
                    wd, wdk = wd_rot.next()
                    c.dma("pool", wg[:], w_gate[l, e].rearrange("(k p) f -> p k f", p=128), [], [wgk])
                    c.dma("pool", wu[:], w_up[l, e].rearrange("(k p) f -> p k f", p=128), [], [wuk])
                    c.dma("pool", wd[:], w_down[l, e].rearrange("(k p) f -> p k f", p=128), [], [wdk])
                    for (t0, n) in ttiles:
                        T = n * 128
                        c0 = t0 * 128
                        hk = [("h2T", t) for t in range(t0, t0 + n)]
                        at, atk = act_rot.next()
                        for fc in range(4):
                            gp, gpk = gps.next()
                            up, upk = ups.next()

                            def e_g(pe, gp=gp, wg=wg, fc=fc, c0=c0, T=T):
                                for k in range(8):
                                    ins = pe.matmul(gp[:, 0:T], wg[:, k, fc * 128:(fc + 1) * 128], h2T[:, k, c0:c0 + T], start=(k == 0), stop=(k == 7))
                                return ins

                            def e_u(pe, up=up, wu=wu, fc=fc, c0=c0, T=T):
                                for k in range(8):
                                    ins = pe.matmul(up[:, 0:T], wu[:, k, fc * 128:(fc + 1) * 128], h2T[:, k, c0:c0 + T], start=(k == 0), stop=(k == 7))
                                return ins
                            c.op("pe", [wgk] + hk, [gpk], e_g)
                            c.op("pe", [wuk] + hk, [upk], e_u)
                            sgl, sglk = sg_rot.next()
                            c.op("act", [gpk], [sglk], lambda a, gp=gp, sgl=sgl, T=T: a.activation(out=sgl[:, 0:T], in_=gp[:, 0:T], func=AF.Silu))
                            c.op("dve", [upk, sglk], [(atk, fc)],
                                 lambda v, up=up, sgl=sgl, at=at, fc=fc, T=T: v.tensor_tensor(out=at[:, fc, 0:T], in0=up[:, 0:T], in1=sgl[:, 0:T], op=ALU.mult))
                        for si in range(n):
                            t = t0 + si
                            for half in range(2):
                                yp, ypk = yps.next()

                                def e_y(pe, yp=yp, at=at, si=si, half=half, wd=wd):
                                    for fc in range(4):
                                        ins = pe.matmul(yp[:, :], at[:, fc, si * 128:(si + 1) * 128], wd[:, fc, half * 512:(half + 1) * 512], start=(fc == 0), stop=(fc == 3))
                                    return ins
                                c.op("pe", [(atk, fc) for fc in range(4)] + [wdk], [ypk], e_y)
                                av = acc[:, t, half * 512:(half + 1) * 512]
                                if e == 0:
                                    c.op("dve", [ypk, ("gates", t)], [("acc", t, half)],
                                         lambda v, yp=yp, av=av, t=t, e=e: v.tensor_scalar_mul(out=av, in0=yp[:, :], scalar1=gates[:, t, e:e + 1]))
                                else:
                                    c.op("dve", [ypk, ("gates", t)], [("acc", t, half)],
                                         lambda v, yp=yp, av=av, t=t, e=e: v.scalar_tensor_tensor(out=av, in0=yp[:, :], scalar=gates[:, t, e:e + 1], in1=av, op0=ALU.mult, op1=ALU.add))
            with c.phase(f"ln2_{l}_{b}") as ph:
                gb2 = ph.sbuf("gb2", [128, 2, D], F32)
                g2l = ph.sbuf("g2l", [128, D], F32)
                g2c = ph.sbuf("g2c", [128, D], F32)
                c.dma("sp", [gb2[:, 0, :], gb2[:, 1, :]],
                      [ln_ffn_g[l].partition_broadcast(128), ln_ffn_b[l].partition_broadcast(128)], [], ["gb2"])
                c.dma("sp", g2l[:], gsc[l, b, 1], [], ["g2l"])
                c.dma("sp", g2c[:], gsc[l, 2, 1], [], ["g2c"])
                rots = {"stats": Rot(ph, "st", 4, [128, 16], F32), "xn": Rot(ph, "xn", 2, [128, D], F32)}
                xf_rot = Rot(ph, "xf", 3, [128, D], F32)
                x2_rot = Rot(ph, "x2", 2, [128, D], F32)
                lds = {}

                def load2(t):
                    xf, xfk = xf_rot.next()
                    c.dma("sp", xf[:], xb[b, t * 128:(t + 1) * 128, :], [], [xfk])
                    lds[t] = (xf, xfk)
                for t in range(min(2, ntile)):
                    load2(t)
                pend2 = {}

                def s2a(t):
                    g2 = g2l if t < NTL else g2c
                    g2k = "g2l" if t < NTL else "g2c"
                    xf, xfk = lds.pop(t)
                    rk = ("accr", t)
                    c.op("dve", [g2k], [rk], lambda v: v.tensor_tensor(out=acc[:, t, :], in0=acc[:, t, :], in1=g2[:], op=ALU.mult))
                    c.op("dve", [xfk], [rk], lambda g: g.scalar_tensor_tensor(out=acc[:, t, :], in0=xf[:], scalar=ALPHA, in1=acc[:, t, :], op0=ALU.mult, op1=ALU.add))
                    if t + 2 < ntile:
                        load2(t + 2)
                    pend2[t] = (ln_stats(rots, acc[:, t, :], rk), rk)

                def s2b(t):
                    stt, rk = pend2.pop(t)
                    x2, x2k = x2_rot.next()
                    ln_apply(rots, stt, acc[:, t, :], rk, gb2, "gb2", x2, x2k)
                    if last:
                        c.dma("pool", out[b, t * 128:(t + 1) * 128, :], x2[:], [x2k], [("out", b, t)], is_output=True)
                    else:
                        c.dma("pool", xa[b, t * 128:(t + 1) * 128, :], x2[:], [x2k], [("xa", b, t)], is_output=debug)

                for t in range(ntile + 1):
                    if t < ntile:
                        s2a(t)
                    if t >= 1:
                        s2b(t - 1)

    ada_prologue()
    done = stop_after is not None and stop_after[0] == "ada"
    if done:
        n_layers = 0
    for l in range(n_layers):
        last = (l == DEPTH - 1)
        for b in range(NB):
            if l % 2 == 0:
                phase_attn_A(l, b, last)
            else:
                for hh in range(2):
                    phase_attn_B(l, b, last, hh)
            if stop_after is not None and stop_after[0] in ("attn", "qkv") and stop_after[1:] == (l, b):
                done = True
                break
            phase_oproj(l, b, last)
            if stop_after == ("oproj", l, b):
                done = True
                break
            phase_moe(l, b, last)
            if stop_after == ("moe", l, b):
                done = True
                break
        if done:
            break
    c.finish()
    return nc


def _consts():
    ident = np.eye(128, dtype=np.float32)
    R = np.zeros((64, 64), np.float32)
    for d in range(64):
        if (d % 32) < 16:
            R[d, d + 16] = -1.0
        else:
            R[d, d - 16] = 1.0
    RT = np.zeros((128, 128), np.float32)
    RT[:64, :64] = R.T
    RT[64:, 64:] = R.T
    t = np.arange(S)
    rows = (t // 64).astype(np.float64)
    cols = (t % 64).astype(np.float64)
    inv = 10000.0 ** (-np.arange(16, dtype=np.float64) / 16.0)
    cos = np.zeros((128, S), np.float32)
    sin = np.zeros((128, S), np.float32)
    for p in range(128):
        d = p % 64
        pos = rows if d < 32 else cols
        ang = pos * np.float64(np.float32(inv[d % 16]))
        ang = (pos.astype(np.float32) * np.float32(inv[d % 16])).astype(np.float64)
        cos[p] = np.cos(ang)
        sin[p] = np.sin(ang)
    kk = np.arange(128)[:, None]
    qq = np.arange(128)[None, :]
    mp = np.where(kk >= qq, 0.0, NEG).astype(np.float32)
    mn = np.where(kk <= qq, 0.0, NEG).astype(np.float32)
    return {"k_ident": ident, "k_rt": RT, "k_cos": cos, "k_sin": sin,
            "k_maskp": np.tile(mp, (1, 4)), "k_maskn": np.tile(mn, (1, 4))}


_NC_CACHE = {}


def make_in_maps(inputs):
    consts = _consts()
    shared = {k: np.ascontiguousarray(inputs[k], dtype=np.float32) for k in (
        "w_ada", "b_ada", "wqkv_a", "wo_a", "sink_a", "wqkv_b", "wo_b", "lambda_b", "subln_b",
        "ln_attn_g", "ln_attn_b", "ln_ffn_g", "ln_ffn_b", "w_router", "router_bias", "w_gate", "w_up", "w_down")}
    shared.update(consts)
    in_maps = []
    for i in range(NCORES):
        m = dict(shared)
        m["x"] = np.ascontiguousarray(inputs["x"][NB * i:NB * (i + 1)], dtype=np.float32)
        m["ctx"] = np.ascontiguousarray(inputs["ctx"][NB * i:NB * (i + 1)], dtype=np.float32)
        m["c3"] = np.ascontiguousarray(
            np.concatenate([inputs["c"][NB * i:NB * (i + 1)], inputs["c_ctx"][None, :]], axis=0), dtype=np.float32)
        in_maps.append(m)
    return in_maps


def kernel(**inputs):
    if "nc" not in _NC_CACHE:
        _NC_CACHE["nc"] = build()
    nc = _NC_CACHE["nc"]
    in_maps = make_in_maps(inputs)
    res = run_bass_kernel_spmd(nc, in_maps, core_ids=list(range(NCORES)))
    return np.concatenate([np.asarray(r["out"]) for r in res.results], axis=0).astype(np.float32)
```
